# Optimizing a Trainium2 kernel written in Bass

```python
import math
import jax, jax.numpy as jnp
from jax import lax
import numpy as np

D_MODEL = 1024
BATCH = 8
SEQ = 8192
DEPTH = 4

HEAD_DIM = 64
A_WIDTH = D_MODEL // 2
A_HEADS = A_WIDTH // HEAD_DIM
B_WIDTH = D_MODEL // 4
B_HEADS = B_WIDTH // HEAD_DIM
C_WIDTH = D_MODEL // 4
MIX_WIDTH = A_WIDTH + B_WIDTH + C_WIDTH
IN_WIDTH = 3 * A_WIDTH + 3 * B_WIDTH + C_WIDTH
DILATED_CONFIGS = ((128, 1), (512, 4), (2048, 16))
GRID_W = 64
NA_ROWS = 8
NA_COLS = 16
NA_QC = 16
NA_KC = NA_QC + NA_COLS
S5_GROUP = 16
S5_GROUPS = C_WIDTH // S5_GROUP
S5_STATE = 64
S5_DT_MIN = 1e-3
S5_DT_MAX = 1e-1
N_EXPERTS = 16
EC_CAPACITY = 2
EXPERT_FF = 2 * D_MODEL
EPS = 1e-6
NEG_INF = -1e30

kernel_name = "hybrid_dilated_natten_s5_ec_encoder"


def rms_norm(x, g):
    xf = x.astype(jnp.float32)
    y = xf * lax.rsqrt(jnp.mean(xf * xf, axis=-1, keepdims=True) + EPS)
    return y.astype(x.dtype) * g


def split_heads(t, n):
    b, s, _ = t.shape
    return t.reshape(b, s, n, HEAD_DIM).transpose(0, 2, 1, 3)


def merge_heads(t):
    b, h, s, d = t.shape
    return t.transpose(0, 2, 1, 3).reshape(b, s, h * d)


def alibi_slopes(n):
    return np.array([2.0 ** (-8.0 * (i + 1) / n) for i in range(n)], dtype=np.float32)


def dilated_branch(q, k, v, window, dil, slopes):
    b, h, s, dh = q.shape
    half = window // (2 * dil)
    L = s // dil
    nb = -(-L // half)
    lp = nb * half

    def classes(t):
        return t.reshape(b, h, L, dil, dh).transpose(0, 1, 3, 2, 4)

    qb = jnp.pad(classes(q), ((0, 0), (0, 0), (0, 0), (0, lp - L), (0, 0))).reshape(b, h, dil, nb, half, dh)

    def band(t):
        t = jnp.pad(classes(t), ((0, 0), (0, 0), (0, 0), (half, lp - L + half), (0, 0)))
        t = t.reshape(b, h, dil, nb + 2, half, dh)
        return jnp.concatenate([t[:, :, :, :-2], t[:, :, :, 1:-1], t[:, :, :, 2:]], axis=4)

    kb, vb = band(k), band(v)
    rel = np.arange(3 * half)[None, :] - half - np.arange(half)[:, None]
    kpos = np.arange(nb)[:, None, None] * half + np.arange(half)[None, :, None] + rel[None]
    valid = (np.abs(rel) <= half)[None] & (kpos >= 0) & (kpos < L)
    bias = -(slopes[:, None, None] * (dil * np.abs(rel)).astype(np.float32)[None])
    sc = jnp.einsum('bhrnqd,bhrnkd->bhrnqk', qb, kb).astype(jnp.float32) * (HEAD_DIM ** -0.5)
    sc = jnp.where(valid, sc + bias[:, None, None], NEG_INF)
    m = jnp.max(sc, axis=-1, keepdims=True)
    p = jnp.exp(sc - m)
    den = jnp.sum(p, axis=-1, keepdims=True)
    o = (jnp.einsum('bhrnqk,bhrnkd->bhrnqd', p.astype(v.dtype), vb).astype(jnp.float32) / den).astype(v.dtype)
    lse = (m + jnp.log(den))[..., 0]
    o = o.reshape(b, h, dil, lp, dh)[:, :, :, :L].transpose(0, 1, 3, 2, 4).reshape(b, h, s, dh)
    lse = lse.reshape(b, h, dil, lp)[..., :L].transpose(0, 1, 3, 2).reshape(b, h, s)
    return o, lse


def dilated_attention(q, k, v):
    slopes = alibi_slopes(q.shape[1])
    outs, lses = [], []
    for window, dil in DILATED_CONFIGS:
        o, lse = dilated_branch(q, k, v, window, dil, slopes)
        outs.append(o)
        lses.append(lse)
    w = jax.nn.softmax(jnp.stack(lses, axis=0), axis=0)
    return jnp.sum(w[..., None].astype(q.dtype) * jnp.stack(outs, axis=0), axis=0)


def neighborhood_attention(q, k, v, rpb):
    b, h, s, dh = q.shape
    rows = s // GRID_W
    kr = min(NA_ROWS, rows)
    ncb = GRID_W // NA_QC
    kk = kr * NA_KC
    r = np.arange(rows)
    rs = np.clip(r - NA_ROWS // 2, 0, rows - kr)
    key_row = rs[:, None] + np.arange(kr)[None]
    kcs = np.clip(np.arange(ncb) * NA_QC - NA_COLS // 2, 0, GRID_W - NA_KC)
    key_col = kcs[:, None] + np.arange(NA_KC)[None]
    idx = (key_row[:, None, :, None] * GRID_W + key_col[None, :, None, :]).reshape(-1)
    kg = jnp.take(k, jnp.asarray(idx, dtype=jnp.int32), axis=2).reshape(b, h, rows, ncb, kk, dh)
    vg = jnp.take(v, jnp.asarray(idx, dtype=jnp.int32), axis=2).reshape(b, h, rows, ncb, kk, dh)
    qb = q.reshape(b, h, rows, ncb, NA_QC, dh)
    qcol = np.arange(ncb)[:, None] * NA_QC + np.arange(NA_QC)[None]
    cs = np.clip(qcol - NA_COLS // 2, 0, GRID_W - NA_COLS)
    kcol = np.broadcast_to(key_col[:, None, :], (ncb, kr, NA_KC)).reshape(ncb, kk)
    valid = (kcol[:, None, :] >= cs[:, :, None]) & (kcol[:, None, :] < cs[:, :, None] + NA_COLS)
    krow = np.broadcast_to(key_row[:, :, None], (rows, kr, NA_KC)).reshape(rows, kk)
    dr_idx = (krow - r[:, None] + NA_ROWS - 1)[:, None, None, :]
    dc_idx = np.clip(kcol[:, None, :] - qcol[:, :, None] + NA_COLS - 1, 0, 2 * NA_COLS - 2)[None]
    bias = rpb.astype(jnp.float32)[:, dr_idx, dc_idx]
    sc = jnp.einsum('bhrjqd,bhrjkd->bhrjqk', qb, kg).astype(jnp.float32) * (HEAD_DIM ** -0.5) + bias
    p = jax.nn.softmax(jnp.where(valid, sc, NEG_INF), axis=-1)
    o = jnp.einsum('bhrjqk,bhrjkd->bhrjqd', p.astype(v.dtype), vg)
    return o.reshape(b, h, s, dh)


def complex_affine_combine(e1, e2):
    a1r, a1i, b1r, b1i = e1
    a2r, a2i, b2r, b2i = e2
    return (a2r * a1r - a2i * a1i,
            a2r * a1i + a2i * a1r,
            a2r * b1r - a2i * b1i + b2r,
            a2r * b1i + a2i * b1r + b2i)


def s5_scan(u, a_re, a_im, log_dt, b_re, b_im, c_re, c_im):
    s = u.shape[1]
    dt = jnp.exp(log_dt.astype(jnp.float32))[:, None]
    a = jnp.minimum(a_re.astype(jnp.float32), -1e-4)
    w = a_im.astype(jnp.float32)
    mag = jnp.exp(a * dt)
    abar_r, abar_i = mag * jnp.cos(w * dt), mag * jnp.sin(w * dt)
    den = a * a + w * w
    zr = abar_r - 1.0
    gr = (zr * a + abar_i * w) / den
    gi = (abar_i * a - zr * w) / den
    br, bi = b_re.astype(jnp.float32), b_im.astype(jnp.float32)
    bbar_r = gr[..., None] * br - gi[..., None] * bi
    bbar_i = gr[..., None] * bi + gi[..., None] * br
    bu_r = jnp.einsum('bsgc,gpc->bsgp', u, bbar_r)
    bu_i = jnp.einsum('bsgc,gpc->bsgp', u, bbar_i)
    ar_seq = jnp.broadcast_to(abar_r, (1, s) + abar_r.shape)
    ai_seq = jnp.broadcast_to(abar_i, (1, s) + abar_i.shape)
    _, _, xr, xi = lax.associative_scan(complex_affine_combine, (ar_seq, ai_seq, bu_r, bu_i), axis=1)
    return (jnp.einsum('bsgp,gcp->bsgc', xr, c_re.astype(jnp.float32))
            - jnp.einsum('bsgp,gcp->bsgc', xi, c_im.astype(jnp.float32)))


def s5_mixer(u, a_re, a_im, log_dt, b_re, b_im, c_re, c_im, d_skip, w_glu):
    bsz, s, _ = u.shape
    uf = u.astype(jnp.float32).reshape(bsz, s, S5_GROUPS, S5_GROUP)
    y_f = s5_scan(uf, a_re[0], a_im[0], log_dt[0], b_re[0], b_im[0], c_re[0], c_im[0])
    y_b = jnp.flip(s5_scan(jnp.flip(uf, axis=1), a_re[1], a_im[1], log_dt[1],
                           b_re[1], b_im[1], c_re[1], c_im[1]), axis=1)
    y = (y_f + y_b).reshape(bsz, s, C_WIDTH) + d_skip.astype(jnp.float32) * uf.reshape(bsz, s, C_WIDTH)
    g = jax.nn.gelu(y).astype(u.dtype)
    return g * jax.nn.sigmoid(g @ w_glu)


def ec_moe(h, w_router, w_gate, w_up, w_down):
    b, s, d = h.shape
    cap = EC_CAPACITY * s // N_EXPERTS
    aff = jax.nn.softmax(jnp.einsum('bsd,de->bse', h, w_router).astype(jnp.float32), axis=-1)
    gate, idx = lax.top_k(aff.transpose(0, 2, 1), cap)
    xe = jax.vmap(lambda hb, ib: hb[ib])(h, idx)
    hid = jax.nn.silu(jnp.einsum('becd,edf->becf', xe, w_gate)) * jnp.einsum('becd,edf->becf', xe, w_up)
    ye = jnp.einsum('becf,efd->becd', hid, w_down) * gate[..., None].astype(h.dtype)
    return jax.vmap(lambda ib, yb: jnp.zeros((s, d), yb.dtype).at[ib.reshape(-1)].add(yb.reshape(-1, d)))(idx, ye)


def setup_inputs(seed: int = 0) -> dict:
    key = jax.random.key(seed)
    ks = jax.random.split(key, 26)

    def nrm(k, shape, scale):
        return scale * jax.random.normal(k, shape, jnp.float32)

    def gain(k, shape):
        return 1.0 + 0.02 * jax.random.normal(k, shape, jnp.float32)

    sh5 = (DEPTH, 2, S5_GROUPS, S5_STATE)
    return {
        "x": nrm(ks[0], (BATCH, SEQ, D_MODEL), 1.0),
        "attn_norm": gain(ks[1], (DEPTH, D_MODEL)),
        "w_in": nrm(ks[2], (DEPTH, D_MODEL, IN_WIDTH), D_MODEL ** -0.5),
        "q_norm_a": gain(ks[3], (DEPTH, HEAD_DIM)),
        "k_norm_a": gain(ks[4], (DEPTH, HEAD_DIM)),
        "q_norm_b": gain(ks[5], (DEPTH, HEAD_DIM)),
        "k_norm_b": gain(ks[6], (DEPTH, HEAD_DIM)),
        "rel_pos_bias": nrm(ks[7], (DEPTH, B_HEADS, 2 * NA_ROWS - 1, 2 * NA_COLS - 1), 0.1),
        "s5_a_re": -0.5 * jnp.exp(nrm(ks[8], sh5, 0.05)),
        "s5_a_im": math.pi * jnp.arange(S5_STATE, dtype=jnp.float32) + nrm(ks[9], sh5, 0.01),
        "s5_log_dt": jax.random.uniform(ks[10], (DEPTH, 2, S5_GROUPS), jnp.float32,
                                        math.log(S5_DT_MIN), math.log(S5_DT_MAX)),
        "s5_b_re": nrm(ks[11], (DEPTH, 2, S5_GROUPS, S5_STATE, S5_GROUP), (2 * S5_GROUP) ** -0.5),
        "s5_b_im": nrm(ks[12], (DEPTH, 2, S5_GROUPS, S5_STATE, S5_GROUP), (2 * S5_GROUP) ** -0.5),
        "s5_c_re": nrm(ks[13], (DEPTH, 2, S5_GROUPS, S5_GROUP, S5_STATE), 0.5),
        "s5_c_im": nrm(ks[14], (DEPTH, 2, S5_GROUPS, S5_GROUP, S5_STATE), 0.5),
        "s5_d": nrm(ks[15], (DEPTH, C_WIDTH), 0.5),
        "w_glu": nrm(ks[16], (DEPTH, C_WIDTH, C_WIDTH), C_WIDTH ** -0.5),
        "out_norm_a": gain(ks[17], (DEPTH, A_WIDTH)),
        "out_norm_b": gain(ks[18], (DEPTH, B_WIDTH)),
        "out_norm_c": gain(ks[19], (DEPTH, C_WIDTH)),
        "w_out": nrm(ks[20], (DEPTH, MIX_WIDTH, D_MODEL), MIX_WIDTH ** -0.5),
        "ffn_norm": gain(ks[21], (DEPTH, D_MODEL)),
        "w_router": nrm(ks[22], (DEPTH, D_MODEL, N_EXPERTS), D_MODEL ** -0.5),
        "w_gate": nrm(ks[23], (DEPTH, N_EXPERTS, D_MODEL, EXPERT_FF), D_MODEL ** -0.5),
        "w_up": nrm(ks[24], (DEPTH, N_EXPERTS, D_MODEL, EXPERT_FF), D_MODEL ** -0.5),
        "w_down": nrm(ks[25], (DEPTH, N_EXPERTS, EXPERT_FF, D_MODEL), EXPERT_FF ** -0.5),
    }


def reference(x, attn_norm, w_in, q_norm_a, k_norm_a, q_norm_b, k_norm_b, rel_pos_bias,
              s5_a_re, s5_a_im, s5_log_dt, s5_b_re, s5_b_im, s5_c_re, s5_c_im, s5_d, w_glu,
              out_norm_a, out_norm_b, out_norm_c, w_out, ffn_norm, w_router, w_gate, w_up, w_down):
    splits = np.cumsum([A_WIDTH, A_WIDTH, A_WIDTH, B_WIDTH, B_WIDTH, B_WIDTH]).tolist()
    for l in range(DEPTH):
        h = rms_norm(x, attn_norm[l])
        proj = h @ w_in[l]
        qa, ka, va, qb, kb, vb, u = jnp.split(proj, splits, axis=-1)
        qa = rms_norm(split_heads(qa, A_HEADS), q_norm_a[l])
        ka = rms_norm(split_heads(ka, A_HEADS), k_norm_a[l])
        oa = merge_heads(dilated_attention(qa, ka, split_heads(va, A_HEADS)))
        qb = rms_norm(split_heads(qb, B_HEADS), q_norm_b[l])
        kb = rms_norm(split_heads(kb, B_HEADS), k_norm_b[l])
        ob = merge_heads(neighborhood_attention(qb, kb, split_heads(vb, B_HEADS), rel_pos_bias[l]))
        oc = s5_mixer(u, s5_a_re[l], s5_a_im[l], s5_log_dt[l], s5_b_re[l], s5_b_im[l],
                      s5_c_re[l], s5_c_im[l], s5_d[l], w_glu[l])
        mix = jnp.concatenate([rms_norm(oa, out_norm_a[l]), rms_norm(ob, out_norm_b[l]),
                               rms_norm(oc, out_norm_c[l])], axis=-1)
        x = x + mix @ w_out[l]
        x = x + ec_moe(rms_norm(x, ffn_norm[l]), w_router[l], w_gate[l], w_up[l], w_down[l])
    return x
```

```python
import math
import numpy as np
import ml_dtypes
from contextlib import ExitStack
import concourse.bass as bass
import concourse.mybir as mybir
from concourse.bass_utils import run_bass_kernel_spmd

F32 = mybir.dt.float32
BF16 = mybir.dt.bfloat16
I32 = mybir.dt.int32
AF = mybir.ActivationFunctionType
ALU = mybir.AluOpType
AX = mybir.AxisListType

S = 8192
D = 1024
DEPTH = 4
NT = S // 128
EPS = 1e-6
PADR = 1024
NEGB = -30000.0
NEXP = 16
CAP = 1024
ROWS = CAP + 128
SEG = 2048


class Buf:
    __slots__ = ("name", "lw", "rd")

    def __init__(self, name=""):
        self.name = name
        self.lw = None
        self.rd = {}


class Prog:
    ENGS = ("pe", "act", "dve", "pool", "sp")

    def __init__(self, nc, es, ndma_sems=16):
        self.nc = nc
        self.es = es
        self.q = {e: [] for e in self.ENGS}
        self.seq = {}
        self.sem = {}
        self.cur = {}
        self.epoch = 0
        for e in ("pe", "act", "dve", "pool"):
            self.cur[e] = e + "#0"
            self.sem[self.cur[e]] = es.enter_context(nc.semaphore("prog_" + e + "_0"))
            self.seq[self.cur[e]] = 0
        self.waited = {e: {} for e in self.ENGS}
        self.dpool = {}
        self.dval = {}
        self.drr = {}
        for qn in ("sp", "pool", "act"):
            self.dpool[qn] = [es.enter_context(nc.semaphore("dq_%s_%d" % (qn, i))) for i in range(ndma_sems)]
            for i in range(ndma_sems):
                self.dval[(qn, i)] = 0
            self.drr[qn] = 0
        self.ninstr = 0

    def new_epoch(self):
        self.epoch += 1
        for e in ("pe", "act", "dve", "pool"):
            k = "%s#%d" % (e, self.epoch)
            self.cur[e] = k
            self.sem[k] = self.es.enter_context(self.nc.semaphore("prog_%s_%d" % (e, self.epoch)))
            self.seq[k] = 0

    def _semobj(self, key):
        if isinstance(key, str):
            return self.sem[key]
        return self.dpool[key[0]][key[1]]

    def _wait(self, eng, key, val):
        if val <= 0:
            return
        w = self.waited[eng]
        if w.get(key, 0) >= val:
            return
        w[key] = val
        self.q[eng].append(("w", key, val))

    def _deps(self, eng, reads, writes):
        deps = {}
        for b in reads:
            if b.lw is not None and deps.get(b.lw[0], 0) < b.lw[1]:
                deps[b.lw[0]] = b.lw[1]
        for b in writes:
            if b.lw is not None and deps.get(b.lw[0], 0) < b.lw[1]:
                deps[b.lw[0]] = b.lw[1]
            for k, v in b.rd.items():
                if deps.get(k, 0) < v:
                    deps[k] = v
        for k, v in deps.items():
            if eng == "pe" and isinstance(k, str) and k.startswith("pe#"):
                continue
            self._wait(eng, k, v)

    def op(self, eng, fn, reads=(), writes=()):
        self._deps(eng, reads, writes)
        key = self.cur[eng]
        self.seq[key] += 1
        v = self.seq[key]
        self.q[eng].append(("i", fn, key, 1))
        for b in writes:
            b.lw = (key, v)
            b.rd = {}
        for b in reads:
            if b.rd.get(key, 0) < v:
                b.rd[key] = v
        self.ninstr += 1

    def dma(self, qn, fn, reads=(), writes=()):
        self._deps(qn, reads, writes)
        i = self.drr[qn]
        self.drr[qn] = (i + 1) % len(self.dpool[qn])
        key = (qn, i)
        self._wait(qn, key, self.dval[key])
        self.dval[key] += 16
        v = self.dval[key]
        self.q[qn].append(("i", fn, key, 16))
        for b in writes:
            b.lw = (key, v)
            b.rd = {}
        for b in reads:
            b.rd[key] = v
        self.ninstr += 1

    def barrier(self):
        for eng in self.ENGS:
            for e in ("pe", "act", "dve", "pool"):
                self._wait(eng, self.cur[e], self.seq[self.cur[e]])
            for key, v in self.dval.items():
                self._wait(eng, key, v)

    def finish_wait_all(self, eng="sp"):
        for e in ("pe", "act", "dve", "pool"):
            self._wait(eng, self.cur[e], self.seq[self.cur[e]])
        for key, v in self.dval.items():
            self._wait(eng, key, v)

    def emit(self):
        nc = self.nc
        engmap = {"pe": "tensor", "act": "scalar", "dve": "vector", "pool": "gpsimd", "sp": "sync"}
        with nc.Block() as block:
            for e in self.ENGS:
                items = self.q[e]

                def body(engobj, items=items):
                    for it in items:
                        if it[0] == "w":
                            engobj.wait_ge(self._semobj(it[1]), it[2])
                        else:
                            it[1](engobj).then_inc(self._semobj(it[2]), it[3])
                getattr(block, engmap[e])(body)
        self.q = {e: [] for e in self.ENGS}


class Rot:
    def __init__(self, tiles):
        self.t = [(t, Buf()) for t in tiles]
        self.i = 0

    def next(self):
        r = self.t[self.i]
        self.i = (self.i + 1) % len(self.t)
        return r


def build_program(depth=DEPTH, stop_after=None, dbg=(), only=None, feed=(), nl=DEPTH):
    nc = bass.Bass("TRN2", target_bir_lowering=False)

    def din(name, shape, dt=F32):
        return nc.dram_tensor(name, list(shape), dt, kind="ExternalInput").ap()

    def dscr(name, shape, dt):
        kind = "ExternalOutput" if name in dbg else ("ExternalInput" if name in feed else "Internal")
        return nc.dram_tensor(name, list(shape), dt, kind=kind).ap()

    L = nl
    x_in = din("x", [S, D])
    OUT = nc.dram_tensor("out", [S, D], F32, kind="ExternalOutput").ap()
    w_in = din("w_in", [L, D, 2560])
    w_out = din("w_out", [L, D, D])
    w_glu = din("w_glu", [L, 256, 256])
    w_router = din("w_router", [L, D, NEXP])
    if only is None or "F" in only:
        w_gate = [din("w_gate%d" % i, [NEXP, D, 2048]) for i in range(L)]
        w_up = [din("w_up%d" % i, [NEXP, D, 2048]) for i in range(L)]
        w_down = [din("w_down%d" % i, [NEXP, 2048, D]) for i in range(L)]
    gcol_d = din("gcol", [128, L, 3, 8])
    gqk_d = din("gqk", [128, L, 4])
    gffn_d = din("gffn", [L, 128, D])
    nab_d = din("nab", [L, 7, 4, 128, 128])
    dbias_d = din("dbias", [3, 8, 128, 256], BF16)
    namask_d = din("namask", [21, 128, 128], BF16)
    cmat_d = din("cmat", [6, 128, 128], BF16)
    identf_d = din("identf", [128, 128])
    s5lam_d = din("s5lam", [128, L, 3, 16])
    s5bT_d = din("s5bT", [128, L, 4, 5, 64])
    s5cT_d = din("s5cT", [128, L, 2, 16, 16])
    s5d_d = din("s5dd", [128, L, 2])
    maskB_d = din("maskB", [128, 8])
    maskC_d = din("maskC", [128, 4, 8])
    ecst_d = din("ecst", [128, 18])

    QTa = dscr("QTa", [512, S], BF16)
    KTa = dscr("KTa", [512, S], BF16)
    Va = dscr("Va", [S + 2 * PADR, 8 * 128], BF16)
    QTb = dscr("QTb", [256, S], BF16)
    KTb = dscr("KTb", [256, S], BF16)
    Vb = dscr("Vb", [S, 4 * 128], BF16)
    UT = dscr("UT", [256, S], BF16)
    ACCA = dscr("ACCA", [3, S, 8 * 65], F32)
    ACCB = dscr("ACCB", [S, 4 * 65], F32)
    MIXC = dscr("MIXC", [256, S], BF16)
    HF = dscr("HF", [S, D], BF16)
    LIST = dscr("LIST", [NEXP * ROWS, 2], F32)
    AFFD = dscr("AFFD", [S, NEXP], F32)
    AFFTD = dscr("AFFTD", [NEXP, S], F32)

    es = ExitStack()
    with es:
        P = Prog(nc, es)

        uid = [0]

        def sbuf(stack, name, shape, dt):
            uid[0] += 1
            return stack.enter_context(nc.sbuf_tensor("s%d_%s" % (uid[0], name), list(shape), dt))

        def psum(stack, name, shape=(128, 512), dt=F32):
            uid[0] += 1
            return stack.enter_context(nc.psum_tensor("p%d_%s" % (uid[0], name), list(shape), dt))

        cmat = sbuf(es, "cmat", [128, 6, 128], BF16)
        identf = sbuf(es, "identf", [128, 128], F32)
        gcol = sbuf(es, "gcol", [128, L, 3, 8], F32)
        gqk = sbuf(es, "gqk", [128, L, 4], F32)
        epsT = sbuf(es, "epsT", [128, 1], F32)
        b_const = Buf("const")
        P.dma("sp", lambda e: e.dma_start(out=cmat[:], in_=cmat_d.rearrange("k p f -> p k f")), writes=[b_const])
        P.dma("sp", lambda e: e.dma_start(out=identf[:], in_=identf_d[:, :]), writes=[b_const])
        P.dma("sp", lambda e: e.dma_start(out=gcol[:], in_=gcol_d[:, :, :, :]), writes=[b_const])
        P.dma("sp", lambda e: e.dma_start(out=gqk[:], in_=gqk_d[:, :, :]), writes=[b_const])
        P.op("pool", lambda e: e.memset(epsT[:], EPS), writes=[b_const])
        ident_bf = cmat[:, 0, :]
        blockones = cmat[:, 1, :]
        ones_bf = cmat[:, 2, :]
        b_X = Buf("X")
        b_QK = Buf("QK")
        b_ACCA = Buf("ACCA")
        b_ACCB = Buf("ACCB")
        b_MIXC = Buf("MIXC")
        b_HF = Buf("HF")
        b_LIST = Buf("LIST")
        b_AFF = Buf("AFF")

        with ExitStack() as s0:
            z = sbuf(s0, "zpad", [128, 8, 1024], BF16)
            bz = Buf()
            P.op("pool", lambda e: e.memset(z[:], 0.0), writes=[bz])
            for r0 in (0, PADR + S):
                P.dma("sp", lambda e, r0=r0: e.dma_start(out=Va[r0:r0 + PADR, :].rearrange("(p a) c -> p a c", a=8), in_=z[:]),
                      reads=[bz], writes=[])
            P.barrier()

        def done(l, ph):
            return stop_after is not None and (l, ph) == tuple(stop_after)

        def phase_A(l):
            xsrc = x_in if l == 0 else OUT
            with ExitStack() as st:
                Win = sbuf(st, "Win", [128, 8, 2560], BF16)
                wst = Rot([sbuf(st, "wst%d" % i, [128, 2560], F32) for i in range(2)])
                xt = Rot([sbuf(st, "xt%d" % i, [128, 4, 1024], F32) for i in range(2)])
                hn = Rot([sbuf(st, "hn%d" % i, [128, 1024], BF16) for i in range(2)])
                hT = Rot([sbuf(st, "hT%d" % i, [128, 8, 512], BF16) for i in range(2)])
                junk = sbuf(st, "junkA", [128, 1024], BF16)
                ss = Rot([sbuf(st, "ssA%d" % i, [128, 4], F32) for i in range(2)])
                sq = Rot([sbuf(st, "sqA%d" % i, [128, 512], BF16) for i in range(2)])
                rr = Rot([sbuf(st, "rrA%d" % i, [128, 512], F32) for i in range(2)])
                qn = Rot([sbuf(st, "qnA%d" % i, [128, 512], BF16) for i in range(3)])
                vst = Rot([sbuf(st, "vstA%d" % i, [128, 8, 128], BF16) for i in range(2)])
                vbst = Rot([sbuf(st, "vbstA%d" % i, [128, 4, 128], BF16) for i in range(2)])
                pT = Rot([psum(st, "pTA%d" % i, [128, 8, 128], BF16) for i in range(2)])
                psA = Rot([psum(st, "psA%d" % i) for i in range(2)])
                ps2 = Rot([psum(st, "ps2A%d" % i) for i in range(1)])
                psv = Rot([psum(st, "psvA%d" % i) for i in range(2)])
                bWin = Buf()
                bjunk = Buf()
                for (t, b) in vst.t + vbst.t:
                    P.op("pool", lambda e, t=t: e.memset(t[:, :, 64:128], 1.0), writes=[b])
                for kc in range(8):
                    w, bw = wst.next()
                    P.dma("sp", lambda e, w=w, kc=kc: e.dma_start(out=w[:], in_=w_in[l, kc * 128:(kc + 1) * 128, :]), writes=[bw])
                    if kc % 2 == 0:
                        P.op("dve", lambda e, w=w, kc=kc: e.tensor_scalar(out=Win[:, kc, :], in0=w[:], scalar1=gcol[:, l, 0, kc:kc + 1],
                                                                           scalar2=None, op0=ALU.mult), reads=[bw, b_const], writes=[bWin])
                    else:
                        P.op("act", lambda e, w=w, kc=kc: e.activation(out=Win[:, kc, :], in_=w[:], func=AF.Copy,
                                                                        scale=gcol[:, l, 0, kc:kc + 1]), reads=[bw, b_const], writes=[bWin])
                fm_tiles = [(f0, QTa, f0, 0) for f0 in range(0, 512, 128)] + \
                           [(512 + f0, KTa, f0, 1) for f0 in range(0, 512, 128)] + \
                           [(1536 + f0, QTb, f0, 2) for f0 in range(0, 256, 128)] + \
                           [(1792 + f0, KTb, f0, 3) for f0 in range(0, 256, 128)] + \
                           [(2304 + f0, UT, f0, None) for f0 in range(0, 256, 128)]
                for blk in range(S // 512):
                    t0 = blk * 512
                    x_t, bx = xt.next()
                    P.dma("sp", lambda e, x_t=x_t, t0=t0: e.dma_start(out=x_t[:], in_=xsrc[t0:t0 + 512, :].rearrange("(j p) d -> p j d", p=128)),
                          reads=[b_X], writes=[bx])
                    s_t, bs = ss.next()
                    for j in range(4):
                        P.op("act", lambda e, x_t=x_t, s_t=s_t, j=j: e.activation(out=junk[:], in_=x_t[:, j, :], func=AF.Square,
                                                                                   accum_out=s_t[:, j:j + 1]), reads=[bx], writes=[bjunk, bs])
                    P.op("act", lambda e, s_t=s_t: e.activation(out=s_t[:], in_=s_t[:], func=AF.Sqrt, scale=1.0 / D, bias=epsT[:, 0:1]),
                         reads=[bs, b_const], writes=[bs])
                    P.op("dve", lambda e, s_t=s_t: e.reciprocal(out=s_t[:], in_=s_t[:]), reads=[bs], writes=[bs])
                    h_T, bhT = hT.next()
                    for j in range(4):
                        h_n, bhn = hn.next()
                        P.op("dve", lambda e, h_n=h_n, x_t=x_t, s_t=s_t, j=j: e.tensor_scalar(out=h_n[:], in0=x_t[:, j, :], scalar1=s_t[:, j:j + 1],
                                                                                               scalar2=None, op0=ALU.mult), reads=[bx, bs], writes=[bhn])
                        p_T, bpT = pT.next()
                        for kc in range(8):
                            P.op("pe", lambda e, p_T=p_T, h_n=h_n, kc=kc: e.transpose(out=p_T[:, kc, :], in_=h_n[:, kc * 128:(kc + 1) * 128],
                                                                                      identity=ident_bf), reads=[bhn, b_const], writes=[bpT])
                        P.op("act", lambda e, p_T=p_T, h_T=h_T, j=j: e.activation(out=h_T[:, :, j * 128:(j + 1) * 128], in_=p_T[:], func=AF.Copy),
                             reads=[bpT], writes=[bhT])
                    for (f0, dst, r0, gi) in fm_tiles:
                        ps, bps = psA.next()
                        for kc in range(8):
                            P.op("pe", lambda e, ps=ps, h_T=h_T, kc=kc, f0=f0: e.matmul(ps[:], lhsT=Win[:, kc, f0:f0 + 128], rhs=h_T[:, kc, :],
                                                                                        start=(kc == 0), stop=(kc == 7)), reads=[bWin, bhT], writes=[bps])
                        q_n, bqn = qn.next()
                        if gi is None:
                            P.op("act", lambda e, q_n=q_n, ps=ps: e.activation(out=q_n[:], in_=ps[:], func=AF.Copy), reads=[bps], writes=[bqn])
                        else:
                            s_q, bsq = sq.next()
                            P.op("act", lambda e, s_q=s_q, ps=ps: e.activation(out=s_q[:], in_=ps[:], func=AF.Square), reads=[bps], writes=[bsq])
                            p2, bp2 = ps2.next()
                            P.op("pe", lambda e, p2=p2, s_q=s_q: e.matmul(p2[:], lhsT=blockones, rhs=s_q[:], start=True, stop=True),
                                 reads=[bsq, b_const], writes=[bp2])
                            r_t, brt = rr.next()
                            P.op("act", lambda e, r_t=r_t, p2=p2: e.activation(out=r_t[:], in_=p2[:], func=AF.Sqrt, scale=1.0 / 64, bias=epsT[:, 0:1]),
                                 reads=[bp2, b_const], writes=[brt])
                            P.op("dve", lambda e, r_t=r_t: e.reciprocal(out=r_t[:], in_=r_t[:]), reads=[brt], writes=[brt])
                            P.op("dve", lambda e, q_n=q_n, ps=ps, r_t=r_t, gi=gi: e.scalar_tensor_tensor(out=q_n[:], in0=ps[:], scalar=gqk[:, l, gi:gi + 1],
                                                                                                         in1=r_t[:], op0=ALU.mult, op1=ALU.mult),
                                 reads=[bps, brt, b_const], writes=[bqn])
                        P.dma("pool", lambda e, q_n=q_n, dst=dst, r0=r0, t0=t0: e.dma_start(out=dst[r0:r0 + 128, t0:t0 + 512], in_=q_n[:]),
                              reads=[bqn], writes=[])
                    for j in range(4):
                        tk = t0 + j * 128
                        pv, bpv = psv.next()
                        for kc in range(8):
                            P.op("pe", lambda e, pv=pv, h_T=h_T, kc=kc, j=j: e.matmul(pv[:], lhsT=h_T[:, kc, j * 128:(j + 1) * 128], rhs=Win[:, kc, 1024:1536],
                                                                                      start=(kc == 0), stop=(kc == 7)), reads=[bWin, bhT], writes=[bpv])
                        v_s, bvs = vst.next()
                        P.op("act", lambda e, v_s=v_s, pv=pv: e.activation(out=v_s[:, :, 0:64], in_=pv[:].rearrange("p (h d) -> p h d", d=64), func=AF.Copy),
                             reads=[bpv], writes=[bvs])
                        P.dma("pool", lambda e, v_s=v_s, tk=tk: e.dma_start(out=Va[PADR + tk:PADR + tk + 128, :], in_=v_s[:].rearrange("p h d -> p (h d)")),
                              reads=[bvs], writes=[])
                        pw, bpw = psv.next()
                        for kc in range(8):
                            P.op("pe", lambda e, pw=pw, h_T=h_T, kc=kc, j=j: e.matmul(pw[:, 0:256], lhsT=h_T[:, kc, j * 128:(j + 1) * 128], rhs=Win[:, kc, 2048:2304],
                                                                                      start=(kc == 0), stop=(kc == 7)), reads=[bWin, bhT], writes=[bpw])
                        vb_s, bvbs = vbst.next()
                        P.op("dve", lambda e, vb_s=vb_s, pw=pw: e.tensor_copy(out=vb_s[:, :, 0:64], in_=pw[:, 0:256].rearrange("p (h d) -> p h d", d=64)),
                             reads=[bpw], writes=[bvbs])
                        P.dma("pool", lambda e, vb_s=vb_s, tk=tk: e.dma_start(out=Vb[tk:tk + 128, :], in_=vb_s[:].rearrange("p h d -> p (h d)")),
                              reads=[bvbs], writes=[])
                P.barrier()

        def phase_B(l):
            with ExitStack() as st:
                dbias = sbuf(st, "dbias", [128, 24, 256], BF16)
                bdb = Buf()
                P.dma("sp", lambda e: e.dma_start(out=dbias[:], in_=dbias_d.rearrange("c h p f -> p (c h) f")), writes=[bdb])
                QT = sbuf(st, "QTB", [128, S], BF16)
                KT = sbuf(st, "KTB", [128, S + 2 * PADR], BF16)
                bQT, bKT = Buf(), Buf()
                P.op("pool", lambda e: e.memset(KT[:, 0:PADR], 0.0), writes=[bKT])
                P.op("pool", lambda e: e.memset(KT[:, PADR + S:], 0.0), writes=[bKT])
                vt = Rot([sbuf(st, "vtB%d" % i, [128, 256], BF16) for i in range(4)])
                pt = Rot([sbuf(st, "ptB%d" % i, [128, 256], BF16) for i in range(4)])
                ost = Rot([sbuf(st, "ostB%d" % i, [128, 65], F32) for i in range(4)])
                psS = Rot([psum(st, "psSB%d" % i) for i in range(3)])
                pso = [[(psum(st, "psoB%d_%d" % (hh, par)), Buf()) for par in range(2)] for hh in range(2)]
                for hp in range(4):
                    P.dma("sp", lambda e, hp=hp: e.dma_start(out=QT[:], in_=QTa[hp * 128:(hp + 1) * 128, :]), writes=[bQT])
                    P.dma("sp", lambda e, hp=hp: e.dma_start(out=KT[:, PADR:PADR + S], in_=KTa[hp * 128:(hp + 1) * 128, :]), writes=[bKT])
                    for c, dil in enumerate((1, 4, 16)):
                        nq = S // dil // 128
                        for r in range(dil):
                            for m in range(nq + 1):
                                base = r + dil * (128 * m - 64)
                                v_t, bvt = vt.next()
                                P.dma("sp", lambda e, v_t=v_t, base=base, dil=dil, hp=hp: e.dma_start(
                                    out=v_t[:], in_=Va[PADR + base:PADR + base + 127 * dil + 1:dil, hp * 256:(hp + 1) * 256]),
                                    writes=[bvt])
                                clo = 128 if m == 0 else 0
                                chi = 128 if m == nq else 256
                                q0 = r + dil * (128 * (m - 1) + clo)
                                ncol = chi - clo
                                for hh in range(2):
                                    h = 2 * hp + hh
                                    pb = 64 * hh
                                    ps, bps = psS.next()
                                    P.op("pe", lambda e, ps=ps, pb=pb, base=base, dil=dil, q0=q0, ncol=ncol, clo=clo, chi=chi: e.matmul(
                                        ps[:, clo:chi], lhsT=KT[pb:pb + 64, PADR + base:PADR + base + 127 * dil + 1:dil],
                                        rhs=QT[pb:pb + 64, q0:q0 + (ncol - 1) * dil + 1:dil], start=True, stop=False),
                                        reads=[bKT, bQT], writes=[bps])
                                    P.op("pe", lambda e, ps=ps, c=c, h=h, clo=clo, chi=chi: e.matmul(
                                        ps[:, clo:chi], lhsT=ident_bf, rhs=dbias[:, c * 8 + h, clo:chi], start=False, stop=True),
                                        reads=[bdb, b_const], writes=[bps])
                                    p_t, bpt = pt.next()
                                    P.op("act", lambda e, p_t=p_t, ps=ps, clo=clo, chi=chi: e.activation(out=p_t[:, clo:chi], in_=ps[:, clo:chi],
                                                                                                        func=AF.Exp, scale=0.125), reads=[bps], writes=[bpt])
                                    for j in (m - 1, m):
                                        if j < 0 or j >= nq:
                                            continue
                                        co = (j - (m - 1)) * 128
                                        po, bpo = pso[hh][j % 2]
                                        first = (j == m)
                                        P.op("pe", lambda e, po=po, p_t=p_t, v_t=v_t, co=co, hh=hh, first=first: e.matmul(
                                            po[:, 0:128], lhsT=p_t[:, co:co + 128], rhs=v_t[:, hh * 128:(hh + 1) * 128], start=first, stop=(not first)),
                                            reads=[bpt, bvt], writes=[bpo])
                                        if not first:
                                            o_s, bos = ost.next()
                                            P.op("dve", lambda e, o_s=o_s, po=po: e.tensor_copy(out=o_s[:], in_=po[:, 0:65]), reads=[bpo], writes=[bos])
                                            tok0 = r + dil * 128 * j
                                            P.dma("pool", lambda e, o_s=o_s, c=c, tok0=tok0, dil=dil, h=h: e.dma_start(
                                                out=ACCA[c, tok0:tok0 + 127 * dil + 1:dil, h * 65:(h + 1) * 65], in_=o_s[:]),
                                                reads=[bos], writes=[])
                P.barrier()

        def phase_C(l):
            with ExitStack() as st:
                namask = sbuf(st, "namask", [128, 21, 128], BF16)
                nab = sbuf(st, "nab", [128, 28, 128], BF16)
                nst = Rot([sbuf(st, "nabst%d" % i, [128, 4, 128], F32) for i in range(2)])
                bnm, bnab = Buf(), Buf()
                P.dma("sp", lambda e: e.dma_start(out=namask[:], in_=namask_d.rearrange("v p f -> p v f")), writes=[bnm])
                for jj in range(7):
                    n_s, bns = nst.next()
                    P.dma("sp", lambda e, n_s=n_s, jj=jj: e.dma_start(out=n_s[:], in_=nab_d[l, jj].rearrange("h p f -> p h f")), writes=[bns])
                    P.op("act", lambda e, n_s=n_s, jj=jj: e.activation(out=nab[:, jj * 4:(jj + 1) * 4, :], in_=n_s[:], func=AF.Copy, scale=8.0),
                         reads=[bns], writes=[bnab])
                QT = sbuf(st, "QTC", [128, S], BF16)
                KT = sbuf(st, "KTC", [128, S], BF16)
                bQT, bKT = Buf(), Buf()
                vt = Rot([sbuf(st, "vtC%d" % i, [128, 256], BF16) for i in range(4)])
                pt = Rot([sbuf(st, "ptC%d" % i, [128, 128], BF16) for i in range(4)])
                ost = Rot([sbuf(st, "ostC%d" % i, [128, 65], F32) for i in range(4)])
                psS = Rot([psum(st, "psSC%d" % i) for i in range(3)])
                pso = [Rot([psum(st, "psoC%d_%d" % (hh, i)) for i in range(2)]) for hh in range(2)]
                for hp in range(2):
                    P.dma("sp", lambda e, hp=hp: e.dma_start(out=QT[:], in_=QTb[hp * 128:(hp + 1) * 128, :]), writes=[bQT])
                    P.dma("sp", lambda e, hp=hp: e.dma_start(out=KT[:], in_=KTb[hp * 128:(hp + 1) * 128, :]), writes=[bKT])
                    for a in range(64):
                        tiles = _na_block_tiles(a)
                        pos = [pso[hh].next() for hh in range(2)]
                        for ti, (kt, vi, jp) in enumerate(tiles):
                            v_t, bvt = vt.next()
                            P.dma("sp", lambda e, v_t=v_t, kt=kt, hp=hp: e.dma_start(out=v_t[:], in_=Vb[kt * 128:(kt + 1) * 128, hp * 256:(hp + 1) * 256]),
                                  writes=[bvt])
                            for hh in range(2):
                                h = 2 * hp + hh
                                pb = 64 * hh
                                ps, bps = psS.next()
                                P.op("pe", lambda e, ps=ps, pb=pb, kt=kt, a=a: e.matmul(ps[:, 0:128], lhsT=KT[pb:pb + 64, kt * 128:(kt + 1) * 128],
                                                                                         rhs=QT[pb:pb + 64, a * 128:(a + 1) * 128], start=True, stop=False),
                                     reads=[bKT, bQT], writes=[bps])
                                P.op("pe", lambda e, ps=ps, jp=jp, h=h: e.matmul(ps[:, 0:128], lhsT=ident_bf, rhs=nab[:, (jp + 3) * 4 + h, :], start=False, stop=False),
                                     reads=[bnab, b_const], writes=[bps])
                                P.op("pe", lambda e, ps=ps, vi=vi: e.matmul(ps[:, 0:128], lhsT=ident_bf, rhs=namask[:, vi, :], start=False, stop=True),
                                     reads=[bnm, b_const], writes=[bps])
                                p_t, bpt = pt.next()
                                P.op("act", lambda e, p_t=p_t, ps=ps: e.activation(out=p_t[:], in_=ps[:, 0:128], func=AF.Exp, scale=0.125),
                                     reads=[bps], writes=[bpt])
                                po, bpo = pos[hh]
                                P.op("pe", lambda e, po=po, p_t=p_t, v_t=v_t, hh=hh, ti=ti, nt=len(tiles): e.matmul(
                                    po[:, 0:128], lhsT=p_t[:], rhs=v_t[:, hh * 128:(hh + 1) * 128], start=(ti == 0), stop=(ti == nt - 1)),
                                    reads=[bpt, bvt], writes=[bpo])
                        for hh in range(2):
                            h = 2 * hp + hh
                            po, bpo = pos[hh]
                            o_s, bos = ost.next()
                            P.op("dve", lambda e, o_s=o_s, po=po: e.tensor_copy(out=o_s[:], in_=po[:, 0:65]), reads=[bpo], writes=[bos])
                            P.dma("pool", lambda e, o_s=o_s, a=a, h=h: e.dma_start(out=ACCB[a * 128:(a + 1) * 128, h * 65:(h + 1) * 65], in_=o_s[:]),
                                  reads=[bos], writes=[])
                P.barrier()

        def phase_D(l):
            PI = math.pi
            with ExitStack() as st:
                lamraw = sbuf(st, "lamraw", [128, 3, 16], F32)
                bTraw = sbuf(st, "bTraw", [128, 4, 5, 64], F32)
                cT = sbuf(st, "cTD", [128, 2, 16, 16], F32)
                maskB = sbuf(st, "maskB", [128, 8], F32)
                maskC = sbuf(st, "maskC", [128, 2, 4, 8], F32)
                dcol = sbuf(st, "dcolD", [128, 2], F32)
                wgst = sbuf(st, "wgst", [128, 2, 256], F32)
                Wglu = sbuf(st, "WgluD", [128, 2, 256], BF16)
                bprm = Buf()
                P.dma("sp", lambda e: e.dma_start(out=lamraw[:], in_=s5lam_d[:, l]), writes=[bprm])
                P.dma("sp", lambda e: e.dma_start(out=bTraw[:], in_=s5bT_d[:, l]), writes=[bprm])
                P.dma("sp", lambda e: e.dma_start(out=cT[:], in_=s5cT_d[:, l]), writes=[bprm])
                P.dma("sp", lambda e: e.dma_start(out=maskB[:], in_=maskB_d[:, :]), writes=[bprm])
                P.dma("sp", lambda e: e.dma_start(out=maskC[:, 0], in_=maskC_d[:, :, :]), writes=[bprm])
                P.dma("sp", lambda e: e.dma_start(out=dcol[:], in_=s5d_d[:, l, :]), writes=[bprm])
                P.dma("sp", lambda e: e.dma_start(out=wgst[:], in_=w_glu[l].rearrange("(k p) j -> p k j", p=128)), writes=[bprm])
                P.op("dve", lambda e: e.tensor_copy(out=Wglu[:], in_=wgst[:]), reads=[bprm], writes=[bprm])
                P.op("dve", lambda e: e.tensor_scalar(out=maskC[:, 1], in0=maskC[:, 0], scalar1=-1.0, scalar2=None, op0=ALU.mult), reads=[bprm], writes=[bprm])

                def disc(A, W, LD, F, full, tag):
                    t = {}
                    for nm in ("dt", "a", "mag", "ang", "y", "sn", "cs", "abr", "abi", "t1", "t2", "zr", "gr", "gi"):
                        t[nm] = sbuf(st, tag + nm, [128, F], F32)
                    b = bprm
                    P.op("act", lambda e: e.activation(out=t["dt"][:], in_=LD, func=AF.Exp), reads=[b], writes=[b])
                    P.op("dve", lambda e: e.tensor_scalar(out=t["a"][:], in0=A, scalar1=-1e-4, scalar2=None, op0=ALU.min), reads=[b], writes=[b])
                    P.op("dve", lambda e: e.tensor_tensor(out=t["mag"][:], in0=t["a"][:], in1=t["dt"][:], op=ALU.mult), reads=[b], writes=[b])
                    P.op("act", lambda e: e.activation(out=t["mag"][:], in_=t["mag"][:], func=AF.Exp), reads=[b], writes=[b])
                    P.op("dve", lambda e: e.tensor_tensor(out=t["ang"][:], in0=W, in1=t["dt"][:], op=ALU.mult), reads=[b], writes=[b])
                    ki = sbuf(st, tag + "ki", [128, F], I32)
                    for (dst, sh) in (("sn", 0.0), ("cs", 0.5 * PI)):
                        P.op("dve", lambda e, sh=sh: e.tensor_scalar(out=t["y"][:], in0=t["ang"][:], scalar1=sh, scalar2=None, op0=ALU.add), reads=[b], writes=[b])
                        P.op("dve", lambda e: e.tensor_scalar(out=t["t1"][:], in0=t["y"][:], scalar1=1.0 / (2.0 * PI), scalar2=None, op0=ALU.mult), reads=[b], writes=[b])
                        P.op("dve", lambda e: e.tensor_copy(out=ki[:], in_=t["t1"][:]), reads=[b], writes=[b])
                        P.op("dve", lambda e: e.tensor_copy(out=t["t1"][:], in_=ki[:]), reads=[b], writes=[b])
                        P.op("dve", lambda e: e.tensor_scalar(out=t["t1"][:], in0=t["t1"][:], scalar1=-2.0 * PI, scalar2=None, op0=ALU.mult), reads=[b], writes=[b])
                        P.op("dve", lambda e: e.tensor_tensor(out=t["y"][:], in0=t["y"][:], in1=t["t1"][:], op=ALU.add), reads=[b], writes=[b])
                        P.op("dve", lambda e: e.tensor_scalar(out=t["y"][:], in0=t["y"][:], scalar1=-PI, scalar2=PI, op0=ALU.max, op1=ALU.min), reads=[b], writes=[b])
                        P.op("act", lambda e, dst=dst: e.activation(out=t[dst][:], in_=t["y"][:], func=AF.Sin), reads=[b], writes=[b])
                    P.op("dve", lambda e: e.tensor_tensor(out=t["abr"][:], in0=t["mag"][:], in1=t["cs"][:], op=ALU.mult), reads=[b], writes=[b])
                    P.op("dve", lambda e: e.tensor_tensor(out=t["abi"][:], in0=t["mag"][:], in1=t["sn"][:], op=ALU.mult), reads=[b], writes=[b])
                    if full:
                        tt_ = lambda o, i0, i1, op: P.op("dve", lambda e: e.tensor_tensor(out=o, in0=i0, in1=i1, op=op), reads=[b], writes=[b])
                        tt_(t["t1"][:], t["a"][:], t["a"][:], ALU.mult)
                        tt_(t["t2"][:], W, W, ALU.mult)
                        tt_(t["t1"][:], t["t1"][:], t["t2"][:], ALU.add)
                        P.op("dve", lambda e: e.reciprocal(out=t["t1"][:], in_=t["t1"][:]), reads=[b], writes=[b])
                        P.op("dve", lambda e: e.tensor_scalar(out=t["zr"][:], in0=t["abr"][:], scalar1=-1.0, scalar2=None, op0=ALU.add), reads=[b], writes=[b])
                        tt_(t["gr"][:], t["zr"][:], t["a"][:], ALU.mult)
                        tt_(t["t2"][:], t["abi"][:], W, ALU.mult)
                        tt_(t["gr"][:], t["gr"][:], t["t2"][:], ALU.add)
                        tt_(t["gr"][:], t["gr"][:], t["t1"][:], ALU.mult)
                        tt_(t["gi"][:], t["abi"][:], t["a"][:], ALU.mult)
                        tt_(t["t2"][:], t["zr"][:], W, ALU.mult)
                        tt_(t["gi"][:], t["gi"][:], t["t2"][:], ALU.subtract)
                        tt_(t["gi"][:], t["gi"][:], t["t1"][:], ALU.mult)
                    return t

                NK = 11
                tl = disc(lamraw[:, 0, :], lamraw[:, 1, :], lamraw[:, 2, :], 16, False, "dl_")
                lamP = sbuf(st, "lamP", [128, 16, NK, 3], F32)
                tq = sbuf(st, "tqD", [128, 2, 16], F32)
                b = bprm
                P.op("dve", lambda e: e.tensor_copy(out=lamP[:, :, 0, 0], in_=tl["abr"][:]), reads=[b], writes=[b])
                P.op("dve", lambda e: e.tensor_copy(out=lamP[:, :, 0, 1], in_=tl["abi"][:]), reads=[b], writes=[b])
                for k in range(NK):
                    if k > 0:
                        P.op("dve", lambda e, k=k: e.tensor_tensor(out=tq[:, 0, :], in0=lamP[:, :, k - 1, 0], in1=lamP[:, :, k - 1, 0], op=ALU.mult), reads=[b], writes=[b])
                        P.op("dve", lambda e, k=k: e.tensor_tensor(out=tq[:, 1, :], in0=lamP[:, :, k - 1, 1], in1=lamP[:, :, k - 1, 1], op=ALU.mult), reads=[b], writes=[b])
                        P.op("dve", lambda e, k=k: e.tensor_tensor(out=lamP[:, :, k, 0], in0=tq[:, 0, :], in1=tq[:, 1, :], op=ALU.subtract), reads=[b], writes=[b])
                        P.op("dve", lambda e, k=k: e.scalar_tensor_tensor(out=lamP[:, :, k, 1], in0=lamP[:, :, k - 1, 0], scalar=2.0, in1=lamP[:, :, k - 1, 1],
                                                                          op0=ALU.mult, op1=ALU.mult), reads=[b], writes=[b])
                    P.op("dve", lambda e, k=k: e.tensor_scalar(out=lamP[:, :, k, 2], in0=lamP[:, :, k, 1], scalar1=-1.0, scalar2=None, op0=ALU.mult), reads=[b], writes=[b])
                A3 = sbuf(st, "A3D", [128, 4, 64], F32)
                W3 = sbuf(st, "W3D", [128, 4, 64], F32)
                L3 = sbuf(st, "L3D", [128, 4, 64], F32)
                Br3 = sbuf(st, "Br3D", [128, 4, 64], F32)
                Bi3 = sbuf(st, "Bi3D", [128, 4, 64], F32)
                for (dst, idx) in ((Br3, 0), (Bi3, 1), (A3, 2), (W3, 3), (L3, 4)):
                    P.op("dve", lambda e, dst=dst, idx=idx: e.tensor_copy(out=dst[:], in_=bTraw[:, :, idx, :]), reads=[b], writes=[b])
                fl = lambda t_: t_[:].rearrange("p a n -> p (a n)")
                tb = disc(fl(A3), fl(W3), fl(L3), 256, True, "db_")
                Bb = sbuf(st, "BbD", [128, 2, 256], F32)
                tt_ = lambda o, i0, i1, op: P.op("dve", lambda e: e.tensor_tensor(out=o, in0=i0, in1=i1, op=op), reads=[b], writes=[b])
                tt_(Bb[:, 0, :], tb["gr"][:], fl(Br3), ALU.mult)
                tt_(tb["t2"][:], tb["gi"][:], fl(Bi3), ALU.mult)
                tt_(Bb[:, 0, :], Bb[:, 0, :], tb["t2"][:], ALU.subtract)
                tt_(Bb[:, 1, :], tb["gr"][:], fl(Bi3), ALU.mult)
                tt_(tb["t2"][:], tb["gi"][:], fl(Br3), ALU.mult)
                tt_(Bb[:, 1, :], Bb[:, 1, :], tb["t2"][:], ALU.add)
                Bblk = sbuf(st, "BblkD", [128, 4, 2, 512], BF16)
                for dc in range(4):
                    for ri in range(2):
                        P.op("dve", lambda e, dc=dc, ri=ri: e.tensor_tensor(
                            out=Bblk[:, dc, ri, :].rearrange("p (g n) -> p g n", n=64),
                            in0=Bb[:, ri, dc * 64:(dc + 1) * 64].unsqueeze(1).to_broadcast([128, 8, 64]),
                            in1=maskB[:, :].unsqueeze(2).to_broadcast([128, 8, 64]), op=ALU.mult), reads=[b], writes=[b])
                Cblk = sbuf(st, "CblkD", [128, 16, 2, 128], F32)
                for dp in range(16):
                    pl = dp % 4
                    for k in range(2):
                        P.op("dve", lambda e, dp=dp, pl=pl, k=k: e.tensor_tensor(
                            out=Cblk[:, dp, k, :].rearrange("p (g o) -> p g o", o=16),
                            in0=cT[:, k, dp, :].unsqueeze(1).to_broadcast([128, 8, 16]),
                            in1=maskC[:, k, pl, :].unsqueeze(2).to_broadcast([128, 8, 16]), op=ALU.mult), reads=[b], writes=[b])

                G = sbuf(st, "GD", [128, 2, S], BF16)
                bG = Buf()
                UTs = sbuf(st, "UTsD", [128, S], BF16)
                Yacc = sbuf(st, "YaccD", [128, S], F32)
                bUT, bY = Buf(), Buf()
                X = [[(sbuf(st, "XD%d%d" % (i, j), [128, SEG], F32), Buf()) for j in range(2)] for i in range(2)]
                endst = sbuf(st, "endstD", [128, 2], F32)
                ptmp = sbuf(st, "ptmpD", [128, SEG], F32)
                bptmp = Buf()
                bend = Buf()
                psr = Rot([psum(st, "psrD%d" % i) for i in range(2)])
                psi = Rot([psum(st, "psiD%d" % i) for i in range(2)])
                psY = Rot([psum(st, "psYD%d" % i) for i in range(2)])

                def stt(eng, out, in0, scalar, in1, rd, wr):
                    P.op(eng, lambda e: e.scalar_tensor_tensor(out=out, in0=in0, scalar=scalar, in1=in1, op0=ALU.mult, op1=ALU.add), reads=rd + [bprm], writes=wr)

                for ct in range(2):
                    P.dma("sp", lambda e, ct=ct: e.dma_start(out=UTs[:], in_=UT[ct * 128:(ct + 1) * 128, :]), writes=[bUT])
                    for q4 in range(4):
                        P.op("pool", lambda e, q4=q4, ct=ct: e.tensor_scalar(out=Yacc[:, q4 * 2048:(q4 + 1) * 2048], in0=UTs[:, q4 * 2048:(q4 + 1) * 2048],
                                                                             scalar1=dcol[:, ct:ct + 1], scalar2=None, op0=ALU.mult), reads=[bUT, bprm], writes=[bY])
                    for d in range(2):
                        for pl in range(4):
                            P8 = ct * 4 + pl
                            dp = d * 8 + P8
                            dc = d * 2 + ct
                            segs = list(range(S // SEG))
                            if d == 1:
                                segs = segs[::-1]
                            for si, sg_ in enumerate(segs):
                                c0 = sg_ * SEG
                                (Ar, bAr), (Ai, bAi) = X[0]
                                for b4 in range(SEG // 512):
                                    pr, bpr = psr.next()
                                    pi_, bpi = psi.next()
                                    P.op("pe", lambda e, pr=pr, dc=dc, pl=pl, c0=c0, b4=b4: e.matmul(pr[:], lhsT=Bblk[:, dc, 0, pl * 128:(pl + 1) * 128],
                                                                                                    rhs=UTs[:, c0 + b4 * 512:c0 + (b4 + 1) * 512], start=True, stop=True),
                                         reads=[bprm, bUT], writes=[bpr])
                                    P.op("pe", lambda e, pi_=pi_, dc=dc, pl=pl, c0=c0, b4=b4: e.matmul(pi_[:], lhsT=Bblk[:, dc, 1, pl * 128:(pl + 1) * 128],
                                                                                                      rhs=UTs[:, c0 + b4 * 512:c0 + (b4 + 1) * 512], start=True, stop=True),
                                         reads=[bprm, bUT], writes=[bpi])
                                    P.op("act", lambda e, pr=pr, Ar=Ar, b4=b4: e.activation(out=Ar[:, b4 * 512:(b4 + 1) * 512], in_=pr[:], func=AF.Copy), reads=[bpr], writes=[bAr])
                                    P.op("act", lambda e, pi_=pi_, Ai=Ai, b4=b4: e.activation(out=Ai[:, b4 * 512:(b4 + 1) * 512], in_=pi_[:], func=AF.Copy), reads=[bpi], writes=[bAi])
                                if si > 0:
                                    col = 0 if d == 0 else SEG - 1
                                    cs_ = slice(col, col + 1)
                                    stt("dve", Ar[:, cs_], endst[:, 0:1], lamP[:, dp, 0, 0:1], Ar[:, cs_], [bend, bAr], [bAr])
                                    stt("dve", Ar[:, cs_], endst[:, 1:2], lamP[:, dp, 0, 2:3], Ar[:, cs_], [bend, bAr], [bAr])
                                    stt("dve", Ai[:, cs_], endst[:, 1:2], lamP[:, dp, 0, 0:1], Ai[:, cs_], [bend, bAi], [bAi])
                                    stt("dve", Ai[:, cs_], endst[:, 0:1], lamP[:, dp, 0, 1:2], Ai[:, cs_], [bend, bAi], [bAi])
                                cur = 0
                                for k in range(NK):
                                    dd = 1 << k
                                    (Sr, bSr), (Si, bSi) = X[cur]
                                    (Dr, bDr), (Di, bDi) = X[1 - cur]
                                    if d == 0:
                                        o, i_, hd = slice(dd, SEG), slice(0, SEG - dd), slice(0, dd)
                                    else:
                                        o, i_, hd = slice(0, SEG - dd), slice(dd, SEG), slice(SEG - dd, SEG)
                                    stt("dve", Dr[:, o], Sr[:, i_], lamP[:, dp, k, 0:1], Sr[:, o], [bSr], [bDr])
                                    stt("dve", Dr[:, o], Si[:, i_], lamP[:, dp, k, 2:3], Dr[:, o], [bSi, bDr], [bDr])
                                    stt("dve", Di[:, o], Si[:, i_], lamP[:, dp, k, 0:1], Si[:, o], [bSi], [bDi])
                                    P.op("pool", lambda e, Sr=Sr, i_=i_, dp=dp, k=k, dd=dd: e.tensor_scalar(out=ptmp[:, 0:SEG - dd], in0=Sr[:, i_], scalar1=lamP[:, dp, k, 1:2],
                                                                                                         scalar2=None, op0=ALU.mult), reads=[bSr, bprm], writes=[bptmp])
                                    P.op("pool", lambda e, Di=Di, o=o, dd=dd: e.tensor_tensor(out=Di[:, o], in0=Di[:, o], in1=ptmp[:, 0:SEG - dd], op=ALU.add),
                                         reads=[bptmp, bDi], writes=[bDi])
                                    P.op("act", lambda e, Dr=Dr, Sr=Sr, hd=hd: e.activation(out=Dr[:, hd], in_=Sr[:, hd], func=AF.Copy), reads=[bSr], writes=[bDr])
                                    P.op("act", lambda e, Di=Di, Si=Si, hd=hd: e.activation(out=Di[:, hd], in_=Si[:, hd], func=AF.Copy), reads=[bSi], writes=[bDi])
                                    cur = 1 - cur
                                (Fr, bFr), (Fi, bFi) = X[cur]
                                lc = SEG - 1 if d == 0 else 0
                                P.op("act", lambda e, Fr=Fr, lc=lc: e.activation(out=endst[:, 0:1], in_=Fr[:, lc:lc + 1], func=AF.Copy), reads=[bFr], writes=[bend])
                                P.op("act", lambda e, Fi=Fi, lc=lc: e.activation(out=endst[:, 1:2], in_=Fi[:, lc:lc + 1], func=AF.Copy), reads=[bFi], writes=[bend])
                                for b4 in range(SEG // 512):
                                    py, bpy = psY.next()
                                    P.op("pe", lambda e, py=py, Fr=Fr, dp=dp, b4=b4: e.matmul(py[:], lhsT=Cblk[:, dp, 0, :], rhs=Fr[:, b4 * 512:(b4 + 1) * 512], start=True, stop=False),
                                         reads=[bprm, bFr], writes=[bpy])
                                    P.op("pe", lambda e, py=py, Fi=Fi, dp=dp, b4=b4: e.matmul(py[:], lhsT=Cblk[:, dp, 1, :], rhs=Fi[:, b4 * 512:(b4 + 1) * 512], start=False, stop=True),
                                         reads=[bprm, bFi], writes=[bpy])
                                    P.op("dve", lambda e, py=py, c0=c0, b4=b4: e.tensor_tensor(out=Yacc[:, c0 + b4 * 512:c0 + (b4 + 1) * 512], in0=py[:],
                                                                                              in1=Yacc[:, c0 + b4 * 512:c0 + (b4 + 1) * 512], op=ALU.add), reads=[bpy, bY], writes=[bY])
                    for q4 in range(4):
                        P.op("act", lambda e, q4=q4, ct=ct: e.activation(out=G[:, ct, q4 * 2048:(q4 + 1) * 2048], in_=Yacc[:, q4 * 2048:(q4 + 1) * 2048], func=AF.Gelu),
                             reads=[bY], writes=[bG])
                sig = Rot([sbuf(st, "sigD%d" % i, [128, 512], F32) for i in range(2)])
                oc = Rot([sbuf(st, "ocD%d" % i, [128, 2, 512], F32) for i in range(2)])
                sq = Rot([sbuf(st, "sqD%d" % i, [128, 2, 512], BF16) for i in range(2)])
                rs = Rot([sbuf(st, "rsD%d" % i, [128, 512], F32) for i in range(2)])
                ocn = Rot([sbuf(st, "ocnD%d" % i, [128, 2, 512], BF16) for i in range(2)])
                for blk in range(S // 512):
                    t0 = blk * 512
                    o_c, boc = oc.next()
                    s_q, bsq = sq.next()
                    for jt in range(2):
                        pz, bpz = psr.next()
                        for kt in range(2):
                            P.op("pe", lambda e, pz=pz, kt=kt, jt=jt, t0=t0: e.matmul(pz[:], lhsT=Wglu[:, kt, jt * 128:(jt + 1) * 128], rhs=G[:, kt, t0:t0 + 512],
                                                                                      start=(kt == 0), stop=(kt == 1)), reads=[bprm, bG], writes=[bpz])
                        s_g, bsg = sig.next()
                        P.op("act", lambda e, s_g=s_g, pz=pz: e.activation(out=s_g[:], in_=pz[:], func=AF.Sigmoid), reads=[bpz], writes=[bsg])
                        P.op("dve", lambda e, o_c=o_c, s_g=s_g, jt=jt, t0=t0: e.tensor_tensor(out=o_c[:, jt, :], in0=G[:, jt, t0:t0 + 512], in1=s_g[:], op=ALU.mult),
                             reads=[bG, bsg], writes=[boc])
                        P.op("pool", lambda e, o_c=o_c, s_q=s_q, jt=jt: e.tensor_tensor(out=s_q[:, jt, :], in0=o_c[:, jt, :], in1=o_c[:, jt, :], op=ALU.mult),
                             reads=[boc], writes=[bsq])
                    p2, bp2 = psY.next()
                    for jt in range(2):
                        P.op("pe", lambda e, p2=p2, s_q=s_q, jt=jt: e.matmul(p2[:], lhsT=ones_bf, rhs=s_q[:, jt, :], start=(jt == 0), stop=(jt == 1)),
                             reads=[bsq, b_const], writes=[bp2])
                    r_s, brs = rs.next()
                    P.op("act", lambda e, r_s=r_s, p2=p2: e.activation(out=r_s[:], in_=p2[:], func=AF.Sqrt, scale=1.0 / 256, bias=epsT[:, 0:1]),
                         reads=[bp2, b_const], writes=[brs])
                    P.op("dve", lambda e, r_s=r_s: e.reciprocal(out=r_s[:], in_=r_s[:]), reads=[brs], writes=[brs])
                    o_n, bon = ocn.next()
                    P.op("dve", lambda e, o_n=o_n, o_c=o_c, r_s=r_s: e.tensor_tensor(out=o_n[:], in0=o_c[:], in1=r_s[:].unsqueeze(1).to_broadcast([128, 2, 512]), op=ALU.mult),
                         reads=[boc, brs], writes=[bon])
                    P.dma("pool", lambda e, o_n=o_n, t0=t0: e.dma_start(out=MIXC[:, t0:t0 + 512].rearrange("(k p) t -> p k t", p=128), in_=o_n[:]), reads=[bon])
                P.barrier()

        def phase_E(l):
            xsrc = x_in if l == 0 else OUT
            with ExitStack() as st:
                Wout = sbuf(st, "Wout", [128, 8, 1024], BF16)
                Wr = sbuf(st, "Wr", [128, 8, 16], F32)
                gffn = sbuf(st, "gffn", [128, 1024], F32)
                bW, bWr, bg = Buf(), Buf(), Buf()
                wst = Rot([sbuf(st, "wstE%d" % i, [128, 1024], F32) for i in range(2)])
                for kc in range(8):
                    w, bw = wst.next()
                    P.dma("sp", lambda e, w=w, kc=kc: e.dma_start(out=w[:], in_=w_out[l, kc * 128:(kc + 1) * 128, :]), writes=[bw])
                    P.op("dve", lambda e, w=w, kc=kc: e.tensor_scalar(out=Wout[:, kc, :], in0=w[:], scalar1=gcol[:, l, 1, kc:kc + 1],
                                                                       scalar2=None, op0=ALU.mult), reads=[bw, b_const], writes=[bW])
                P.dma("sp", lambda e: e.dma_start(out=Wr[:], in_=w_router[l].rearrange("(k p) e -> p k e", p=128)), writes=[bWr])
                P.dma("sp", lambda e: e.dma_start(out=gffn[:], in_=gffn_d[l]), writes=[bg])
                acc = Rot([sbuf(st, "accE%d" % i, [128, 3, 520], F32) for i in range(2)])
                accb = Rot([sbuf(st, "accbE%d" % i, [128, 260], F32) for i in range(2)])
                xt = Rot([sbuf(st, "xtE%d" % i, [128, 1024], F32) for i in range(2)])
                mc = Rot([sbuf(st, "mcE%d" % i, [128, 2, 128], BF16) for i in range(2)])
                sa = Rot([sbuf(st, "saE%d" % i, [128, 520], F32) for i in range(2)])
                rd = Rot([sbuf(st, "rdE%d" % i, [128, 16], F32) for i in range(2)])
                oab = Rot([sbuf(st, "oabE%d" % i, [128, 768], F32) for i in range(2)])
                junk = sbuf(st, "junkE", [128, 1024], BF16)
                bjunk = Buf()
                ssq = Rot([sbuf(st, "ssqE%d" % i, [128, 4], F32) for i in range(2)])
                mixn = Rot([sbuf(st, "mixnE%d" % i, [128, 768], BF16) for i in range(2)])
                mT = Rot([sbuf(st, "mTE%d" % i, [128, 6, 128], BF16) for i in range(2)])
                x1 = Rot([sbuf(st, "x1E%d" % i, [128, 1024], F32) for i in range(2)])
                hf = Rot([sbuf(st, "hfE%d" % i, [128, 1024], F32) for i in range(2)])
                hfb = Rot([sbuf(st, "hfbE%d" % i, [128, 1024], BF16) for i in range(2)])
                hfT = Rot([sbuf(st, "hfTE%d" % i, [128, 8, 128], F32) for i in range(2)])
                ex = Rot([sbuf(st, "exE%d" % i, [128, 16], F32) for i in range(2)])
                se = Rot([sbuf(st, "seE%d" % i, [128, 2], F32) for i in range(2)])
                aff = Rot([sbuf(st, "affE%d" % i, [128, 16], F32) for i in range(2)])
                aT = Rot([sbuf(st, "aTE%d" % i, [16, 128], F32) for i in range(2)])
                pT = Rot([psum(st, "pTE%d" % i, [128, 8, 128], BF16) for i in range(1)])
                psx = Rot([psum(st, "psxE%d" % i) for i in range(2)])
                pTf = Rot([psum(st, "pTfE%d" % i, [128, 4, 128], F32) for i in range(2)])
                psl = Rot([psum(st, "pslE%d" % i) for i in range(2)])
                for tt in range(NT):
                    t0 = tt * 128
                    a_t, ba = acc.next()
                    P.dma("sp", lambda e, a_t=a_t, t0=t0: e.dma_start(out=a_t[:], in_=ACCA[:, t0:t0 + 128, :].rearrange("c p f -> p c f")),
                          writes=[ba])
                    ab_t, bab = accb.next()
                    P.dma("sp", lambda e, ab_t=ab_t, t0=t0: e.dma_start(out=ab_t[:], in_=ACCB[t0:t0 + 128, :]), writes=[bab])
                    x_t, bx = xt.next()
                    P.dma("sp", lambda e, x_t=x_t, t0=t0: e.dma_start(out=x_t[:], in_=xsrc[t0:t0 + 128, :]), writes=[bx])
                    m_c, bmc = mc.next()
                    P.dma("sp", lambda e, m_c=m_c, t0=t0: e.dma_start(out=m_c[:], in_=MIXC[:, t0:t0 + 128].rearrange("(k p) t -> p k t", p=128)),
                          writes=[bmc])
                    s_a, bsa = sa.next()
                    P.op("pool", lambda e, s_a=s_a, a_t=a_t: e.tensor_tensor(out=s_a[:], in0=a_t[:, 0, :], in1=a_t[:, 1, :], op=ALU.add), reads=[ba], writes=[bsa])
                    P.op("pool", lambda e, s_a=s_a, a_t=a_t: e.tensor_tensor(out=s_a[:], in0=s_a[:], in1=a_t[:, 2, :], op=ALU.add), reads=[ba, bsa], writes=[bsa])
                    r_d, brd = rd.next()
                    sa3 = s_a[:].rearrange("p (h d) -> p h d", d=65)
                    ab3 = ab_t[:].rearrange("p (h d) -> p h d", d=65)
                    P.op("dve", lambda e, r_d=r_d, sa3=sa3: e.reciprocal(out=r_d[:, 0:8], in_=sa3[:, :, 64]), reads=[bsa], writes=[brd])
                    P.op("dve", lambda e, r_d=r_d, ab3=ab3: e.reciprocal(out=r_d[:, 8:12], in_=ab3[:, :, 64]), reads=[bab], writes=[brd])
                    o_t, bo = oab.next()
                    P.op("dve", lambda e, o_t=o_t, sa3=sa3, r_d=r_d: e.tensor_tensor(
                        out=o_t[:, 0:512].rearrange("p (h d) -> p h d", d=64), in0=sa3[:, :, 0:64],
                        in1=r_d[:, 0:8].unsqueeze(2).to_broadcast([128, 8, 64]), op=ALU.mult), reads=[bsa, brd], writes=[bo])
                    P.op("dve", lambda e, o_t=o_t, ab3=ab3, r_d=r_d: e.tensor_tensor(
                        out=o_t[:, 512:768].rearrange("p (h d) -> p h d", d=64), in0=ab3[:, :, 0:64],
                        in1=r_d[:, 8:12].unsqueeze(2).to_broadcast([128, 4, 64]), op=ALU.mult), reads=[bab, brd], writes=[bo])
                    s_s, bss = ssq.next()
                    P.op("act", lambda e, o_t=o_t, s_s=s_s: e.activation(out=junk[:, 0:512], in_=o_t[:, 0:512], func=AF.Square, accum_out=s_s[:, 0:1]),
                         reads=[bo], writes=[bjunk, bss])
                    P.op("act", lambda e, o_t=o_t, s_s=s_s: e.activation(out=junk[:, 0:256], in_=o_t[:, 512:768], func=AF.Square, accum_out=s_s[:, 1:2]),
                         reads=[bo], writes=[bjunk, bss])
                    P.op("act", lambda e, s_s=s_s: e.activation(out=s_s[:, 0:1], in_=s_s[:, 0:1], func=AF.Sqrt, scale=1.0 / 512, bias=epsT[:, 0:1]),
                         reads=[bss, b_const], writes=[bss])
                    P.op("act", lambda e, s_s=s_s: e.activation(out=s_s[:, 1:2], in_=s_s[:, 1:2], func=AF.Sqrt, scale=1.0 / 256, bias=epsT[:, 0:1]),
                         reads=[bss, b_const], writes=[bss])
                    P.op("dve", lambda e, s_s=s_s: e.reciprocal(out=s_s[:, 0:2], in_=s_s[:, 0:2]), reads=[bss], writes=[bss])
                    m_n, bmn = mixn.next()
                    P.op("dve", lambda e, m_n=m_n, o_t=o_t, s_s=s_s: e.tensor_scalar(out=m_n[:, 0:512], in0=o_t[:, 0:512], scalar1=s_s[:, 0:1],
                                                                                     scalar2=None, op0=ALU.mult), reads=[bo, bss], writes=[bmn])
                    P.op("pool", lambda e, m_n=m_n, o_t=o_t, s_s=s_s: e.tensor_scalar(out=m_n[:, 512:768], in0=o_t[:, 512:768], scalar1=s_s[:, 1:2],
                                                                                      scalar2=None, op0=ALU.mult), reads=[bo, bss], writes=[bmn])
                    p_T, bpT = pT.next()
                    for kc in range(6):
                        P.op("pe", lambda e, p_T=p_T, m_n=m_n, kc=kc: e.transpose(out=p_T[:, kc, :], in_=m_n[:, kc * 128:(kc + 1) * 128], identity=ident_bf),
                             reads=[bmn, b_const], writes=[bpT])
                    m_T, bmT = mT.next()
                    P.op("act", lambda e, m_T=m_T, p_T=p_T: e.activation(out=m_T[:], in_=p_T[:, 0:6, :], func=AF.Copy), reads=[bpT], writes=[bmT])
                    x_1, bx1 = x1.next()
                    for half in range(2):
                        px, bpx = psx.next()
                        for kc in range(8):
                            if kc < 6:
                                P.op("pe", lambda e, px=px, m_T=m_T, kc=kc, half=half: e.matmul(px[:], lhsT=m_T[:, kc, :], rhs=Wout[:, kc, half * 512:(half + 1) * 512],
                                                                                                 start=(kc == 0), stop=False), reads=[bmT, bW], writes=[bpx])
                            else:
                                P.op("pe", lambda e, px=px, m_c=m_c, kc=kc, half=half: e.matmul(px[:], lhsT=m_c[:, kc - 6, :], rhs=Wout[:, kc, half * 512:(half + 1) * 512],
                                                                                                 start=False, stop=(kc == 7)), reads=[bmc, bW], writes=[bpx])
                        P.op("dve", lambda e, x_1=x_1, px=px, x_t=x_t, half=half: e.tensor_tensor(out=x_1[:, half * 512:(half + 1) * 512], in0=px[:],
                                                                                                 in1=x_t[:, half * 512:(half + 1) * 512], op=ALU.add),
                             reads=[bpx, bx], writes=[bx1])
                    P.dma("pool", lambda e, x_1=x_1, t0=t0: e.dma_start(out=OUT[t0:t0 + 128, :], in_=x_1[:]), reads=[bx1])
                    P.op("act", lambda e, x_1=x_1, s_s=s_s: e.activation(out=junk[:], in_=x_1[:], func=AF.Square, accum_out=s_s[:, 2:3]),
                         reads=[bx1], writes=[bjunk, bss])
                    P.op("act", lambda e, s_s=s_s: e.activation(out=s_s[:, 2:3], in_=s_s[:, 2:3], func=AF.Sqrt, scale=1.0 / D, bias=epsT[:, 0:1]),
                         reads=[bss, b_const], writes=[bss])
                    P.op("dve", lambda e, s_s=s_s: e.reciprocal(out=s_s[:, 2:3], in_=s_s[:, 2:3]), reads=[bss], writes=[bss])
                    h_f, bhf = hf.next()
                    P.op("dve", lambda e, h_f=h_f, x_1=x_1, s_s=s_s: e.scalar_tensor_tensor(out=h_f[:], in0=x_1[:], scalar=s_s[:, 2:3], in1=gffn[:],
                                                                                           op0=ALU.mult, op1=ALU.mult), reads=[bx1, bss, bg], writes=[bhf])
                    h_b, bhb = hfb.next()
                    P.op("pool", lambda e, h_b=h_b, h_f=h_f: e.tensor_copy(out=h_b[:], in_=h_f[:]), reads=[bhf], writes=[bhb])
                    P.dma("pool", lambda e, h_b=h_b, t0=t0: e.dma_start(out=HF[t0:t0 + 128, :], in_=h_b[:]), reads=[bhb], writes=[])
                    h_T, bhT = hfT.next()
                    for q4 in range(2):
                        pf, bpf = pTf.next()
                        for k4 in range(4):
                            kc = q4 * 4 + k4
                            P.op("pe", lambda e, pf=pf, h_f=h_f, kc=kc, k4=k4: e.transpose(out=pf[:, k4, :], in_=h_f[:, kc * 128:(kc + 1) * 128], identity=identf[:]),
                                 reads=[bhf, b_const], writes=[bpf])
                        P.op("act", lambda e, h_T=h_T, pf=pf, q4=q4: e.activation(out=h_T[:, q4 * 4:(q4 + 1) * 4, :], in_=pf[:], func=AF.Copy),
                             reads=[bpf], writes=[bhT])
                    pl, bpl = psl.next()
                    for kc in range(8):
                        P.op("pe", lambda e, pl=pl, h_T=h_T, kc=kc: e.matmul(pl[:, 0:16], lhsT=h_T[:, kc, :], rhs=Wr[:, kc, :], start=(kc == 0), stop=(kc == 7)),
                             reads=[bhT, bWr], writes=[bpl])
                    e_x, bex = ex.next()
                    s_e, bse = se.next()
                    P.op("act", lambda e, e_x=e_x, pl=pl, s_e=s_e: e.activation(out=e_x[:], in_=pl[:, 0:16], func=AF.Exp, accum_out=s_e[:, 0:1]),
                         reads=[bpl], writes=[bex, bse])
                    P.op("dve", lambda e, s_e=s_e: e.reciprocal(out=s_e[:, 1:2], in_=s_e[:, 0:1]), reads=[bse], writes=[bse])
                    a_f, baf = aff.next()
                    P.op("dve", lambda e, a_f=a_f, e_x=e_x, s_e=s_e: e.tensor_scalar(out=a_f[:], in0=e_x[:], scalar1=s_e[:, 1:2], scalar2=None, op0=ALU.mult),
                         reads=[bex, bse], writes=[baf])
                    P.dma("pool", lambda e, a_f=a_f, t0=t0: e.dma_start(out=AFFD[t0:t0 + 128, :], in_=a_f[:]), reads=[baf], writes=[])
                    pl2, bpl2 = psl.next()
                    P.op("pe", lambda e, pl2=pl2, a_f=a_f: e.transpose(out=pl2[0:16, 0:128], in_=a_f[:, 0:16], identity=identf[:]),
                         reads=[baf, b_const], writes=[bpl2])
                    a_T, baT = aT.next()
                    P.op("act", lambda e, pl2=pl2, a_T=a_T: e.activation(out=a_T[:], in_=pl2[0:16, 0:128], func=AF.Copy),
                         reads=[bpl2], writes=[baT])
                    P.dma("pool", lambda e, a_T=a_T, t0=t0: e.dma_start(out=AFFTD[:, t0:t0 + 128], in_=a_T[:]), reads=[baT])
                P.barrier()

        def phase_F(l):
            with ExitStack() as st:
                with ExitStack() as s1:
                    work = sbuf(s1, "workF", [16, S], F32)
                    mask = sbuf(s1, "maskF", [16, S], F32)
                    posf = sbuf(s1, "posF", [16, S], F32)
                    m8 = sbuf(s1, "m8F", [16, 8], F32)
                    ecst = sbuf(s1, "ecstF", [128, 18], F32)
                    bwk, bmk, bps_, bm8, bec = Buf(), Buf(), Buf(), Buf(), Buf()
                    P.dma("sp", lambda e: e.dma_start(out=ecst[:], in_=ecst_d[:, :]), writes=[bec])
                    affc = sbuf(s1, "affcF", [16, S], F32)
                    b_AFFT = Buf()
                    P.dma("sp", lambda e: e.dma_start(out=affc[:], in_=AFFTD[:, :]), writes=[b_AFFT])
                    P.dma("sp", lambda e: e.dma_start(out=work[:], in_=AFFTD[:, :]), writes=[bwk])
                    nit = CAP // 8
                    for it in range(nit):
                        P.op("dve", lambda e: e.max(out=m8[:], in_=work[:]), reads=[bwk], writes=[bm8])
                        if it < nit - 1:
                            P.op("dve", lambda e: e.match_replace(out=work[:], in_to_replace=m8[:], in_values=work[:], imm_value=-1.0),
                                 reads=[bm8, bwk], writes=[bwk])
                    P.op("dve", lambda e: e.tensor_scalar(out=mask[:], in0=affc[:], scalar1=m8[:, 7:8], scalar2=None, op0=ALU.is_ge),
                         reads=[b_AFFT, bm8], writes=[bmk])
                    P.op("pool", lambda e: e.memset(work[:], 1.0), reads=[bm8], writes=[bwk])
                    P.op("dve", lambda e: e.tensor_tensor_scan(out=posf[:], data0=work[:], data1=mask[:], initial=0.0, op0=ALU.mult, op1=ALU.add),
                         reads=[bwk, bmk], writes=[bps_])
                    afl = Rot([sbuf(s1, "aflF%d" % i, [128, 16], F32) for i in range(3)])
                    dst = Rot([sbuf(s1, "dstF%d" % i, [128, 16], F32) for i in range(3)])
                    dsi = Rot([sbuf(s1, "dsiF%d" % i, [128, 16], I32) for i in range(3)])
                    pair = Rot([sbuf(s1, "pairF%d" % i, [128, 16, 2], F32) for i in range(3)])
                    ptr = Rot([psum(s1, "ptrF%d" % i) for i in range(2)])
                    for tt in range(NT):
                        t0 = tt * 128
                        pt_, bpt_ = ptr.next()
                        P.op("pe", lambda e, pt_=pt_, t0=t0: e.transpose(out=pt_[:, 0:16], in_=mask[:, t0:t0 + 128], identity=identf[0:16, 0:16]),
                             reads=[bmk, b_const], writes=[bpt_])
                        P.op("pe", lambda e, pt_=pt_, t0=t0: e.transpose(out=pt_[:, 16:32], in_=posf[:, t0:t0 + 128], identity=identf[0:16, 0:16]),
                             reads=[bps_, b_const], writes=[bpt_])
                        a_l, bal = afl.next()
                        P.dma("sp", lambda e, a_l=a_l, t0=t0: e.dma_start(out=a_l[:], in_=AFFD[t0:t0 + 128, :]), writes=[bal])
                        d_t, bdt = dst.next()
                        P.op("dve", lambda e, d_t=d_t, pt_=pt_: e.tensor_scalar(out=d_t[:], in0=pt_[:, 16:32], scalar1=ecst[:, 16:17], scalar2=None, op0=ALU.subtract),
                             reads=[bpt_, bec], writes=[bdt])
                        P.op("dve", lambda e, d_t=d_t, pt_=pt_: e.tensor_tensor(out=d_t[:], in0=d_t[:], in1=pt_[:, 0:16], op=ALU.mult),
                             reads=[bpt_, bdt], writes=[bdt])
                        P.op("dve", lambda e, d_t=d_t: e.tensor_tensor(out=d_t[:], in0=d_t[:], in1=ecst[:, 0:16], op=ALU.add), reads=[bdt, bec], writes=[bdt])
                        d_i, bdi = dsi.next()
                        P.op("dve", lambda e, d_i=d_i, d_t=d_t: e.tensor_copy(out=d_i[:], in_=d_t[:]), reads=[bdt], writes=[bdi])
                        p_r, bpr = pair.next()
                        P.op("dve", lambda e, p_r=p_r, t0=t0: e.tensor_scalar(out=p_r[:, :, 0], in0=ecst[:, 17:18].to_broadcast([128, 16]), scalar1=float(t0),
                                                                               scalar2=None, op0=ALU.add), reads=[bec], writes=[bpr])
                        P.op("act", lambda e, p_r=p_r, a_l=a_l: e.activation(out=p_r[:, :, 1], in_=a_l[:], func=AF.Copy), reads=[bal], writes=[bpr])
                        for ex_ in range(NEXP):
                            P.dma("pool", lambda e, p_r=p_r, d_i=d_i, ex_=ex_: e.indirect_dma_start(
                                out=LIST[:, :], out_offset=bass.IndirectOffsetOnAxis(ap=d_i[:, ex_:ex_ + 1], axis=0),
                                in_=p_r[:, ex_, :], in_offset=None), reads=[bpr, bdi], writes=[])
                    P.barrier()
                Wg = sbuf(st, "WgF", [128, 8, 2048], BF16)
                Wu = sbuf(st, "WuF", [128, 8, 2048], BF16)
                Wd = sbuf(st, "WdF", [128, 16, 1024], BF16)
                bWg, bWu, bWd = Buf(), Buf(), Buf()
                wst = Rot([sbuf(st, "wstF%d" % i, [128, 2048], F32) for i in range(2)])
                li = sbuf(st, "liF", [128, 8, 2], F32)
                lii = sbuf(st, "liiF", [128, 8], I32)
                bli, blii = Buf(), Buf()
                xe = Rot([sbuf(st, "xeF%d" % i, [128, 1024], BF16) for i in range(2)])
                xeT = sbuf(st, "xeTF", [128, 8, 1024], BF16)
                bxeT = Buf()
                hid = sbuf(st, "hidF", [128, 16, 1024], BF16)
                bhid = Buf()
                sg = Rot([sbuf(st, "sgF%d" % i, [128, 512], F32) for i in range(2)])
                ys = Rot([sbuf(st, "ysF%d" % i, [128, 1024], F32) for i in range(2)])
                xr = Rot([sbuf(st, "xrF%d" % i, [128, 1024], F32) for i in range(2)])
                pT = Rot([psum(st, "pTF%d" % i, [128, 8, 128], BF16) for i in range(2)])
                psg = Rot([psum(st, "psgF%d" % i) for i in range(2)])
                psu = Rot([psum(st, "psuF%d" % i) for i in range(2)])
                psy = Rot([psum(st, "psyF%d" % i) for i in range(2)])
                cvt = [0]

                def convert(dst_ap, src_ap, rd, wr):
                    k = cvt[0] % 3
                    cvt[0] += 1
                    if k == 0:
                        P.op("act", lambda e: e.activation(out=dst_ap, in_=src_ap, func=AF.Copy), reads=rd, writes=wr)
                    elif k == 1:
                        P.op("pool", lambda e: e.tensor_copy(out=dst_ap, in_=src_ap), reads=rd, writes=wr)
                    else:
                        P.op("dve", lambda e: e.tensor_copy(out=dst_ap, in_=src_ap), reads=rd, writes=wr)

                for ex_ in range(NEXP):
                    for kc in range(8):
                        w, bw = wst.next()
                        P.dma("sp", lambda e, w=w, kc=kc, ex_=ex_: e.dma_start(out=w[:], in_=w_gate[l][ex_, kc * 128:(kc + 1) * 128, :]), writes=[bw])
                        convert(Wg[:, kc, :], w[:], [bw], [bWg])
                        w, bw = wst.next()
                        P.dma("sp", lambda e, w=w, kc=kc, ex_=ex_: e.dma_start(out=w[:], in_=w_up[l][ex_, kc * 128:(kc + 1) * 128, :]), writes=[bw])
                        convert(Wu[:, kc, :], w[:], [bw], [bWu])
                    for fc2 in range(8):
                        w, bw = wst.next()
                        P.dma("sp", lambda e, w=w, fc2=fc2, ex_=ex_: e.dma_start(
                            out=w[:].rearrange("p (a d) -> p a d", a=2), in_=w_down[l][ex_, fc2 * 256:(fc2 + 1) * 256, :].rearrange("(a p) d -> p a d", p=128)),
                            writes=[bw])
                        convert(Wd[:, fc2 * 2:fc2 * 2 + 2, :], w[:].rearrange("p (a d) -> p a d", a=2), [bw], [bWd])
                    P.dma("sp", lambda e, ex_=ex_: e.dma_start(out=li[:], in_=LIST[ex_ * ROWS:ex_ * ROWS + CAP, :].rearrange("(j p) c -> p j c", p=128)),
                          writes=[bli])
                    P.op("dve", lambda e: e.tensor_copy(out=lii[:], in_=li[:, :, 0]), reads=[bli], writes=[blii])
                    for j in range(8):
                        x_e, bxe = xe.next()
                        P.dma("pool", lambda e, x_e=x_e, j=j: e.indirect_dma_start(out=x_e[:], out_offset=None, in_=HF[:, :],
                                                                                    in_offset=bass.IndirectOffsetOnAxis(ap=lii[:, j:j + 1], axis=0)),
                              reads=[blii], writes=[bxe])
                        p_T, bpT = pT.next()
                        for kc in range(8):
                            P.op("pe", lambda e, p_T=p_T, x_e=x_e, kc=kc: e.transpose(out=p_T[:, kc, :], in_=x_e[:, kc * 128:(kc + 1) * 128], identity=ident_bf),
                                 reads=[bxe, b_const], writes=[bpT])
                        P.op("act" if j % 2 == 0 else "dve", (lambda e, p_T=p_T, j=j: e.activation(out=xeT[:, :, j * 128:(j + 1) * 128], in_=p_T[:], func=AF.Copy)) if j % 2 == 0
                             else (lambda e, p_T=p_T, j=j: e.tensor_copy(out=xeT[:, :, j * 128:(j + 1) * 128], in_=p_T[:])), reads=[bpT], writes=[bxeT])
                    for tb in range(2):
                        for fc in range(16):
                            pg, bpg = psg.next()
                            pu, bpu = psu.next()
                            for kc in range(8):
                                P.op("pe", lambda e, pg=pg, kc=kc, fc=fc, tb=tb: e.matmul(pg[:], lhsT=Wg[:, kc, fc * 128:(fc + 1) * 128], rhs=xeT[:, kc, tb * 512:(tb + 1) * 512],
                                                                                          start=(kc == 0), stop=(kc == 7)), reads=[bWg, bxeT], writes=[bpg])
                            for kc in range(8):
                                P.op("pe", lambda e, pu=pu, kc=kc, fc=fc, tb=tb: e.matmul(pu[:], lhsT=Wu[:, kc, fc * 128:(fc + 1) * 128], rhs=xeT[:, kc, tb * 512:(tb + 1) * 512],
                                                                                          start=(kc == 0), stop=(kc == 7)), reads=[bWu, bxeT], writes=[bpu])
                            s_g, bsg = sg.next()
                            P.op("act", lambda e, s_g=s_g, pg=pg: e.activation(out=s_g[:], in_=pg[:], func=AF.Silu), reads=[bpg], writes=[bsg])
                            P.op("dve", lambda e, s_g=s_g, pu=pu, fc=fc, tb=tb: e.tensor_tensor(out=hid[:, fc, tb * 512:(tb + 1) * 512], in0=pu[:], in1=s_g[:], op=ALU.mult),
                                 reads=[bpu, bsg], writes=[bhid])
                    for j in range(8):
                        y_s, bys = ys.next()
                        for half in range(2):
                            py, bpy = psy.next()
                            for fc in range(16):
                                P.op("pe", lambda e, py=py, fc=fc, j=j, half=half: e.matmul(py[:], lhsT=hid[:, fc, j * 128:(j + 1) * 128], rhs=Wd[:, fc, half * 512:(half + 1) * 512],
                                                                                            start=(fc == 0), stop=(fc == 15)), reads=[bhid, bWd], writes=[bpy])
                            P.op("act", lambda e, y_s=y_s, py=py, half=half, j=j: e.activation(out=y_s[:, half * 512:(half + 1) * 512], in_=py[:], func=AF.Copy,
                                                                                                 scale=li[:, j, 1:2]), reads=[bpy, bli], writes=[bys])
                        x_r, bxr = xr.next()
                        P.dma("pool", lambda e, x_r=x_r, j=j: e.indirect_dma_start(out=x_r[:], out_offset=None, in_=OUT[:, :],
                                                                                    in_offset=bass.IndirectOffsetOnAxis(ap=lii[:, j:j + 1], axis=0)),
                              reads=[blii, b_X], writes=[bxr])
                        P.op("pool", lambda e, x_r=x_r, y_s=y_s: e.tensor_tensor(out=x_r[:], in0=x_r[:], in1=y_s[:], op=ALU.add), reads=[bxr, bys], writes=[bxr])
                        P.dma("pool", lambda e, x_r=x_r, j=j: e.indirect_dma_start(out=OUT[:, :], out_offset=bass.IndirectOffsetOnAxis(ap=lii[:, j:j + 1], axis=0),
                                                                                    in_=x_r[:], in_offset=None), reads=[bxr, blii], writes=[b_X])
                P.barrier()

        PHASES = {}
        PHASES["A"] = phase_A
        PHASES["B"] = phase_B
        PHASES["C"] = phase_C
        PHASES["D"] = phase_D
        PHASES["E"] = phase_E
        PHASES["F"] = phase_F
        order = "ABCDEF"
        stop = False
        for l in range(depth):
            if l > 0:
                P.new_epoch()
            for ph in order:
                if ph in PHASES and (only is None or ph in only):
                    PHASES[ph](l)
                if done(l, ph):
                    stop = True
                    break
            if stop:
                break
        P.finish_wait_all("sp")
        P.emit()
    return nc


def _consts():
    c = {}
    bf = ml_dtypes.bfloat16
    cm = np.zeros((6, 128, 128), np.float32)
    i = np.arange(128)
    cm[0] = np.eye(128)
    cm[1] = (i[:, None] // 64 == i[None, :] // 64)
    cm[2] = 1.0
    cm[3] = (i[:, None] < i[None, :])
    cm[4] = np.eye(128)[::-1]
    c["cmat"] = cm.astype(bf)
    c["identf"] = np.eye(128, dtype=np.float32)
    slopes = np.array([2.0 ** (-8.0 * (h + 1) / 8) for h in range(8)], np.float64)
    db = np.zeros((3, 8, 128, 256), np.float64)
    k = np.arange(128)[:, None]
    q = np.arange(256)[None, :]
    rel = k - q + 64
    valid = np.abs(rel) <= 64
    for ci, dil in enumerate((1, 4, 16)):
        for h in range(8):
            db[ci, h] = np.where(valid, -slopes[h] * dil * np.abs(rel) * 8.0, NEGB)
    c["dbias"] = db.astype(np.float32).astype(bf)
    variants = _na_variants()
    nm = np.zeros((21, 128, 128), np.float32)
    kk = np.arange(128)
    krl, kc = kk // 64, kk % 64
    qrl, qc = kk // 64, kk % 64
    cs = np.clip(qc - 8, 0, 48)
    colv = (kc[:, None] >= cs[None, :]) & (kc[:, None] < cs[None, :] + 16)
    for vi, (a, jp) in enumerate(variants):
        krow = 2 * (a + jp) + krl
        qrow = 2 * a + qrl
        rs = np.clip(qrow - 4, 0, 120)
        rowv = (krow[:, None] >= rs[None, :]) & (krow[:, None] < rs[None, :] + 8)
        nm[vi] = np.where(colv & rowv, 0.0, NEGB)
    c["namask"] = nm.astype(bf)
    p = np.arange(128)
    c["maskB"] = (p[:, None] // 16 == np.arange(8)[None, :]).astype(np.float32)
    mc = np.zeros((128, 4, 8), np.float32)
    for pl in range(4):
        for g8 in range(8):
            mc[:, pl, g8] = (g8 == 2 * pl + p // 64)
    c["maskC"] = mc
    ec = np.zeros((128, 18), np.float32)
    ec[:, 0:16] = np.arange(16)[None, :] * ROWS + CAP + p[:, None]
    ec[:, 16] = CAP + p + 1
    ec[:, 17] = p
    c["ecst"] = ec
    return c


def _na_variants():
    v = [(10, jp) for jp in (-2, -1, 0, 1, 2)]
    v += [(0, jp) for jp in (0, 1, 2, 3)]
    v += [(1, jp) for jp in (-1, 0, 1, 2)]
    v += [(62, jp) for jp in (-2, -1, 0, 1)]
    v += [(63, jp) for jp in (-3, -2, -1, 0)]
    return v


def _na_block_tiles(a):
    if a == 0:
        return [(j, 5 + j, j) for j in range(4)]
    if a == 1:
        return [(a + jp, 9 + (jp + 1), jp) for jp in (-1, 0, 1, 2)]
    if a == 62:
        return [(a + jp, 13 + (jp + 2), jp) for jp in (-2, -1, 0, 1)]
    if a == 63:
        return [(a + jp, 17 + (jp + 3), jp) for jp in (-3, -2, -1, 0)]
    return [(a + jp, jp + 2, jp) for jp in (-2, -1, 0, 1, 2)]


def _prep_shared(inp):
    L = DEPTH
    f = lambda a: np.ascontiguousarray(np.asarray(a, dtype=np.float32))
    sh = {}
    for k in ("w_in", "w_out", "w_glu", "w_router", "w_gate", "w_up", "w_down"):
        sh[k] = f(inp[k])
    onorm = np.concatenate([inp["out_norm_a"], inp["out_norm_b"], inp["out_norm_c"]], axis=1)
    g3 = np.stack([inp["attn_norm"], onorm, inp["ffn_norm"]], axis=1)
    sh["gcol"] = f(g3.reshape(L, 3, 8, 128).transpose(3, 0, 1, 2))
    gq = np.stack([inp["q_norm_a"], inp["k_norm_a"], inp["q_norm_b"], inp["k_norm_b"]], axis=1)
    sh["gqk"] = f(np.concatenate([gq, gq], axis=2).transpose(2, 0, 1))
    sh["gffn"] = f(np.broadcast_to(np.asarray(inp["ffn_norm"])[:, None, :], (L, 128, D)))
    rp = np.asarray(inp["rel_pos_bias"], np.float32)
    kk = np.arange(128)
    krl, kc = kk // 64, kk % 64
    nabt = np.zeros((L, 7, 4, 128, 128), np.float32)
    dc = np.clip(kc[:, None] - kc[None, :] + 15, 0, 30)
    for jp in range(-3, 4):
        dr = np.clip(2 * jp + krl[:, None] - krl[None, :] + 7, 0, 14)
        nabt[:, jp + 3] = rp[:, :, dr, dc]
    sh["nab"] = nabt
    are = np.asarray(inp["s5_a_re"], np.float32)
    aim = np.asarray(inp["s5_a_im"], np.float32)
    ldt = np.broadcast_to(np.asarray(inp["s5_log_dt"], np.float32)[..., None], are.shape)
    p3 = np.stack([are, aim, ldt], axis=1)
    t = p3.reshape(L, 3, 2, 8, 2, 64)
    sh["s5lam"] = f(t.transpose(4, 5, 0, 1, 2, 3).reshape(128, L, 3, 16))
    bre = np.asarray(inp["s5_b_re"], np.float32)
    bim = np.asarray(inp["s5_b_im"], np.float32)
    rep = lambda a: np.broadcast_to(a[..., None], a.shape + (16,))
    q5 = np.stack([bre, bim, rep(are), rep(aim), rep(ldt)], axis=0)
    q5 = q5.reshape(5, L, 2, 2, 8, 64, 16)
    sh["s5bT"] = f(q5.transpose(4, 6, 1, 2, 3, 0, 5).reshape(128, L, 4, 5, 64))
    cre = np.asarray(inp["s5_c_re"], np.float32)
    cim = np.asarray(inp["s5_c_im"], np.float32)
    c2 = np.stack([cre, cim], axis=0).reshape(2, L, 2, 8, 2, 16, 64)
    sh["s5cT"] = f(c2.transpose(4, 6, 1, 0, 2, 3, 5).reshape(128, L, 2, 16, 16))
    sh["s5dd"] = f(np.asarray(inp["s5_d"]).reshape(L, 2, 128).transpose(2, 0, 1))
    sh.update(_consts())
    return sh


_NC_CACHE = {}
_AX0 = ("w_in", "w_out", "w_glu", "w_router", "gffn", "nab")
_SPLIT = ("w_gate", "w_up", "w_down")
_AX1 = ("gcol", "gqk", "s5lam", "s5bT", "s5cT", "s5dd")
LAYERS_PER_LAUNCH = 4


def kernel(**inputs):
    x = np.asarray(inputs["x"], dtype=np.float32)
    sh = _prep_shared(inputs)
    npl = LAYERS_PER_LAUNCH
    if "nc" not in _NC_CACHE:
        _NC_CACHE["nc"] = build_program(depth=npl, nl=npl)
    nc = _NC_CACHE["nc"]
    cur = [np.ascontiguousarray(x[c]) for c in range(8)]
    for l0 in range(0, DEPTH, npl):
        shl = {}
        for k, v in sh.items():
            if k in _SPLIT:
                for i in range(npl):
                    shl["%s%d" % (k, i)] = v[l0 + i]
            elif k in _AX0:
                shl[k] = np.ascontiguousarray(v[l0:l0 + npl])
            elif k in _AX1:
                shl[k] = np.ascontiguousarray(v[:, l0:l0 + npl])
            else:
                shl[k] = v
        in_maps = []
        for c in range(8):
            m = dict(shl)
            m["x"] = cur[c]
            in_maps.append(m)
        res = run_bass_kernel_spmd(nc, in_maps, core_ids=list(range(8)))
        cur = [np.ascontiguousarray(np.asarray(r["out"], dtype=np.float32)) for r in res.results]
    return np.stack(cur, axis=0)
```

```python
import math
import numpy as np
import ml_dtypes
from contextlib import ExitStack
import concourse.bass as bass
import concourse.mybir as mybir
from concourse.bass_utils import run_bass_kernel_spmd

F32 = mybir.dt.float32
BF16 = mybir.dt.bfloat16
I32 = mybir.dt.int32
AF = mybir.ActivationFunctionType
ALU = mybir.AluOpType
AX = mybir.AxisListType

S = 8192
D = 1024
DEPTH = 4
NT = S // 128
EPS = 1e-6
PADR = 1024
NEGB = -30000.0
NEXP = 16
CAP = 1024
ROWS = CAP + 128
SEG = 2048


class Buf:
    __slots__ = ("name", "lw", "rd")

    def __init__(self, name=""):
        self.name = name
        self.lw = None
        self.rd = {}


class Prog:
    ENGS = ("pe", "act", "dve", "pool", "sp")

    def __init__(self, nc, es, ndma_sems=16):
        self.nc = nc
        self.es = es
        self.q = {e: [] for e in self.ENGS}
        self.seq = {}
        self.sem = {}
        self.cur = {}
        self.epoch = 0
        for e in ("pe", "act", "dve", "pool"):
            self.cur[e] = e + "#0"
            self.sem[self.cur[e]] = es.enter_context(nc.semaphore("prog_" + e + "_0"))
            self.seq[self.cur[e]] = 0
        self.waited = {e: {} for e in self.ENGS}
        self.dpool = {}
        self.dval = {}
        self.drr = {}
        for qn in ("sp", "pool", "act"):
            self.dpool[qn] = [es.enter_context(nc.semaphore("dq_%s_%d" % (qn, i))) for i in range(ndma_sems)]
            for i in range(ndma_sems):
                self.dval[(qn, i)] = 0
            self.drr[qn] = 0
        self.ninstr = 0

    def new_epoch(self):
        self.epoch += 1
        for e in ("pe", "act", "dve", "pool"):
            k = "%s#%d" % (e, self.epoch)
            self.cur[e] = k
            self.sem[k] = self.es.enter_context(self.nc.semaphore("prog_%s_%d" % (e, self.epoch)))
            self.seq[k] = 0

    def _semobj(self, key):
        if isinstance(key, str):
            return self.sem[key]
        return self.dpool[key[0]][key[1]]

    def _wait(self, eng, key, val):
        if val <= 0:
            return
        w = self.waited[eng]
        if w.get(key, 0) >= val:
            return
        w[key] = val
        self.q[eng].append(("w", key, val))

    def _deps(self, eng, reads, writes):
        deps = {}
        for b in reads:
            if b.lw is not None and deps.get(b.lw[0], 0) < b.lw[1]:
                deps[b.lw[0]] = b.lw[1]
        for b in writes:
            if b.lw is not None and deps.get(b.lw[0], 0) < b.lw[1]:
                deps[b.lw[0]] = b.lw[1]
            for k, v in b.rd.items():
                if deps.get(k, 0) < v:
                    deps[k] = v
        for k, v in deps.items():
            if eng == "pe" and isinstance(k, str) and k.startswith("pe#"):
                continue
            self._wait(eng, k, v)

    def op(self, eng, fn, reads=(), writes=()):
        self._deps(eng, reads, writes)
        key = self.cur[eng]
        self.seq[key] += 1
        v = self.seq[key]
        self.q[eng].append(("i", fn, key, 1))
        for b in writes:
            b.lw = (key, v)
            b.rd = {}
        for b in reads:
            if b.rd.get(key, 0) < v:
                b.rd[key] = v
        self.ninstr += 1

    def dma(self, qn, fn, reads=(), writes=()):
        self._deps(qn, reads, writes)
        i = self.drr[qn]
        self.drr[qn] = (i + 1) % len(self.dpool[qn])
        key = (qn, i)
        self._wait(qn, key, self.dval[key])
        self.dval[key] += 16
        v = self.dval[key]
        self.q[qn].append(("i", fn, key, 16))
        for b in writes:
            b.lw = (key, v)
            b.rd = {}
        for b in reads:
            b.rd[key] = v
        self.ninstr += 1

    def barrier(self):
        for eng in self.ENGS:
            for e in ("pe", "act", "dve", "pool"):
                self._wait(eng, self.cur[e], self.seq[self.cur[e]])
            for key, v in self.dval.items():
                self._wait(eng, key, v)

    def finish_wait_all(self, eng="sp"):
        for e in ("pe", "act", "dve", "pool"):
            self._wait(eng, self.cur[e], self.seq[self.cur[e]])
        for key, v in self.dval.items():
            self._wait(eng, key, v)

    def emit(self):
        nc = self.nc
        engmap = {"pe": "tensor", "act": "scalar", "dve": "vector", "pool": "gpsimd", "sp": "sync"}
        with nc.Block() as block:
            for e in self.ENGS:
                items = self.q[e]

                def body(engobj, items=items):
                    for it in items:
                        if it[0] == "w":
                            engobj.wait_ge(self._semobj(it[1]), it[2])
                        else:
                            it[1](engobj).then_inc(self._semobj(it[2]), it[3])
                getattr(block, engmap[e])(body)
        self.q = {e: [] for e in self.ENGS}


class Rot:
    def __init__(self, tiles):
        self.t = [(t, Buf()) for t in tiles]
        self.i = 0

    def next(self):
        r = self.t[self.i]
        self.i = (self.i + 1) % len(self.t)
        return r


def build_program(depth=DEPTH, stop_after=None, dbg=(), only=None, feed=(), nl=DEPTH):
    nc = bass.Bass("TRN2", target_bir_lowering=False)

    def din(name, shape, dt=F32):
        return nc.dram_tensor(name, list(shape), dt, kind="ExternalInput").ap()

    def dscr(name, shape, dt):
        kind = "ExternalOutput" if name in dbg else ("ExternalInput" if name in feed else "Internal")
        return nc.dram_tensor(name, list(shape), dt, kind=kind).ap()

    L = nl
    x_in = din("x", [S, D])
    OUT = nc.dram_tensor("out", [S, D], F32, kind="ExternalOutput").ap()
    w_in = din("w_in", [L, D, 2560])
    w_out = din("w_out", [L, D, D])
    w_glu = din("w_glu", [L, 256, 256])
    w_router = din("w_router", [L, D, NEXP])
    if only is None or "F" in only:
        w_gate = [din("w_gate%d" % i, [NEXP, D, 2048]) for i in range(L)]
        w_up = [din("w_up%d" % i, [NEXP, D, 2048]) for i in range(L)]
        w_down = [din("w_down%d" % i, [NEXP, 2048, D]) for i in range(L)]
    gcol_d = din("gcol", [128, L, 3, 8])
    gqk_d = din("gqk", [128, L, 4])
    gffn_d = din("gffn", [L, 128, D])
    nab_d = din("nab", [L, 7, 4, 128, 128])
    dbias_d = din("dbias", [3, 8, 128, 256], BF16)
    namask_d = din("namask", [21, 128, 128], BF16)
    cmat_d = din("cmat", [6, 128, 128], BF16)
    identf_d = din("identf", [128, 128])
    s5lam_d = din("s5lam", [128, L, 3, 16])
    s5bT_d = din("s5bT", [128, L, 4, 5, 64])
    s5cT_d = din("s5cT", [128, L, 2, 16, 16])
    s5d_d = din("s5dd", [128, L, 2])
    maskB_d = din("maskB", [128, 8])
    maskC_d = din("maskC", [128, 4, 8])
    ecst_d = din("ecst", [128, 18])

    QTa = dscr("QTa", [512, S], BF16)
    KTa = dscr("KTa", [512, S], BF16)
    Va = dscr("Va", [S + 2 * PADR, 8 * 128], BF16)
    QTb = dscr("QTb", [256, S], BF16)
    KTb = dscr("KTb", [256, S], BF16)
    Vb = dscr("Vb", [S, 4 * 128], BF16)
    UT = dscr("UT", [256, S], BF16)
    ACCA = dscr("ACCA", [3, S, 8 * 65], F32)
    ACCB = dscr("ACCB", [S, 4 * 65], F32)
    MIXC = dscr("MIXC", [256, S], BF16)
    HF = dscr("HF", [S, D], BF16)
    LIST = dscr("LIST", [NEXP * ROWS, 2], F32)
    AFFD = dscr("AFFD", [S, NEXP], F32)
    AFFTD = dscr("AFFTD", [NEXP, S], F32)

    es = ExitStack()
    with es:
        P = Prog(nc, es)

        uid = [0]

        def sbuf(stack, name, shape, dt):
            uid[0] += 1
            return stack.enter_context(nc.sbuf_tensor("s%d_%s" % (uid[0], name), list(shape), dt))

        def psum(stack, name, shape=(128, 512), dt=F32):
            uid[0] += 1
            return stack.enter_context(nc.psum_tensor("p%d_%s" % (uid[0], name), list(shape), dt))

        cmat = sbuf(es, "cmat", [128, 6, 128], BF16)
        identf = sbuf(es, "identf", [128, 128], F32)
        gcol = sbuf(es, "gcol", [128, L, 3, 8], F32)
        gqk = sbuf(es, "gqk", [128, L, 4], F32)
        epsT = sbuf(es, "epsT", [128, 1], F32)
        b_const = Buf("const")
        P.dma("sp", lambda e: e.dma_start(out=cmat[:], in_=cmat_d.rearrange("k p f -> p k f")), writes=[b_const])
        P.dma("sp", lambda e: e.dma_start(out=identf[:], in_=identf_d[:, :]), writes=[b_const])
        P.dma("sp", lambda e: e.dma_start(out=gcol[:], in_=gcol_d[:, :, :, :]), writes=[b_const])
        P.dma("sp", lambda e: e.dma_start(out=gqk[:], in_=gqk_d[:, :, :]), writes=[b_const])
        P.op("pool", lambda e: e.memset(epsT[:], EPS), writes=[b_const])
        ident_bf = cmat[:, 0, :]
        blockones = cmat[:, 1, :]
        ones_bf = cmat[:, 2, :]
        b_X = Buf("X")
        b_QK = Buf("QK")
        b_ACCA = Buf("ACCA")
        b_ACCB = Buf("ACCB")
        b_MIXC = Buf("MIXC")
        b_HF = Buf("HF")
        b_LIST = Buf("LIST")
        b_AFF = Buf("AFF")

        with ExitStack() as s0:
            z = sbuf(s0, "zpad", [128, 8, 1024], BF16)
            bz = Buf()
            P.op("pool", lambda e: e.memset(z[:], 0.0), writes=[bz])
            for r0 in (0, PADR + S):
                P.dma("sp", lambda e, r0=r0: e.dma_start(out=Va[r0:r0 + PADR, :].rearrange("(p a) c -> p a c", a=8), in_=z[:]),
                      reads=[bz], writes=[])
            P.barrier()

        def done(l, ph):
            return stop_after is not None and (l, ph) == tuple(stop_after)

        def phase_A(l):
            xsrc = x_in if l == 0 else OUT
            with ExitStack() as st:
                Win = sbuf(st, "Win", [128, 8, 2560], BF16)
                wst = Rot([sbuf(st, "wst%d" % i, [128, 2560], F32) for i in range(2)])
                xt = Rot([sbuf(st, "xt%d" % i, [128, 4, 1024], F32) for i in range(2)])
                hn = Rot([sbuf(st, "hn%d" % i, [128, 1024], BF16) for i in range(2)])
                hT = Rot([sbuf(st, "hT%d" % i, [128, 8, 512], BF16) for i in range(2)])
                junk = sbuf(st, "junkA", [128, 1024], BF16)
                ss = Rot([sbuf(st, "ssA%d" % i, [128, 4], F32) for i in range(2)])
                sq = Rot([sbuf(st, "sqA%d" % i, [128, 512], BF16) for i in range(2)])
                rr = Rot([sbuf(st, "rrA%d" % i, [128, 512], F32) for i in range(2)])
                qn = Rot([sbuf(st, "qnA%d" % i, [128, 512], BF16) for i in range(3)])
                vst = Rot([sbuf(st, "vstA%d" % i, [128, 8, 128], BF16) for i in range(2)])
                vbst = Rot([sbuf(st, "vbstA%d" % i, [128, 4, 128], BF16) for i in range(2)])
                pT = Rot([psum(st, "pTA%d" % i, [128, 8, 128], BF16) for i in range(2)])
                psA = Rot([psum(st, "psA%d" % i) for i in range(2)])
                ps2 = Rot([psum(st, "ps2A%d" % i) for i in range(1)])
                psv = Rot([psum(st, "psvA%d" % i) for i in range(2)])
                bWin = Buf()
                bjunk = Buf()
                for (t, b) in vst.t + vbst.t:
                    P.op("pool", lambda e, t=t: e.memset(t[:, :, 64:128], 1.0), writes=[b])
                for kc in range(8):
                    w, bw = wst.next()
                    P.dma("sp", lambda e, w=w, kc=kc: e.dma_start(out=w[:], in_=w_in[l, kc * 128:(kc + 1) * 128, :]), writes=[bw])
                    if kc % 2 == 0:
                        P.op("dve", lambda e, w=w, kc=kc: e.tensor_scalar(out=Win[:, kc, :], in0=w[:], scalar1=gcol[:, l, 0, kc:kc + 1],
                                                                           scalar2=None, op0=ALU.mult), reads=[bw, b_const], writes=[bWin])
                    else:
                        P.op("act", lambda e, w=w, kc=kc: e.activation(out=Win[:, kc, :], in_=w[:], func=AF.Copy,
                                                                        scale=gcol[:, l, 0, kc:kc + 1]), reads=[bw, b_const], writes=[bWin])
                fm_tiles = [(f0, QTa, f0, 0) for f0 in range(0, 512, 128)] + \
                           [(512 + f0, KTa, f0, 1) for f0 in range(0, 512, 128)] + \
                           [(1536 + f0, QTb, f0, 2) for f0 in range(0, 256, 128)] + \
                           [(1792 + f0, KTb, f0, 3) for f0 in range(0, 256, 128)] + \
                           [(2304 + f0, UT, f0, None) for f0 in range(0, 256, 128)]
                for blk in range(S // 512):
                    t0 = blk * 512
                    x_t, bx = xt.next()
                    P.dma("sp", lambda e, x_t=x_t, t0=t0: e.dma_start(out=x_t[:], in_=xsrc[t0:t0 + 512, :].rearrange("(j p) d -> p j d", p=128)),
                          reads=[b_X], writes=[bx])
                    s_t, bs = ss.next()
                    for j in range(4):
                        P.op("act", lambda e, x_t=x_t, s_t=s_t, j=j: e.activation(out=junk[:], in_=x_t[:, j, :], func=AF.Square,
                                                                                   accum_out=s_t[:, j:j + 1]), reads=[bx], writes=[bjunk, bs])
                    P.op("act", lambda e, s_t=s_t: e.activation(out=s_t[:], in_=s_t[:], func=AF.Sqrt, scale=1.0 / D, bias=epsT[:, 0:1]),
                         reads=[bs, b_const], writes=[bs])
                    P.op("dve", lambda e, s_t=s_t: e.reciprocal(out=s_t[:], in_=s_t[:]), reads=[bs], writes=[bs])
                    h_T, bhT = hT.next()
                    for j in range(4):
                        h_n, bhn = hn.next()
                        P.op("dve", lambda e, h_n=h_n, x_t=x_t, s_t=s_t, j=j: e.tensor_scalar(out=h_n[:], in0=x_t[:, j, :], scalar1=s_t[:, j:j + 1],
                                                                                               scalar2=None, op0=ALU.mult), reads=[bx, bs], writes=[bhn])
                        p_T, bpT = pT.next()
                        for kc in range(8):
                            P.op("pe", lambda e, p_T=p_T, h_n=h_n, kc=kc: e.transpose(out=p_T[:, kc, :], in_=h_n[:, kc * 128:(kc + 1) * 128],
                                                                                      identity=ident_bf), reads=[bhn, b_const], writes=[bpT])
                        P.op("act", lambda e, p_T=p_T, h_T=h_T, j=j: e.activation(out=h_T[:, :, j * 128:(j + 1) * 128], in_=p_T[:], func=AF.Copy),
                             reads=[bpT], writes=[bhT])
                    for (f0, dst, r0, gi) in fm_tiles:
                        ps, bps = psA.next()
                        for kc in range(8):
                            P.op("pe", lambda e, ps=ps, h_T=h_T, kc=kc, f0=f0: e.matmul(ps[:], lhsT=Win[:, kc, f0:f0 + 128], rhs=h_T[:, kc, :],
                                                                                        start=(kc == 0), stop=(kc == 7)), reads=[bWin, bhT], writes=[bps])
                        q_n, bqn = qn.next()
                        if gi is None:
                            P.op("act", lambda e, q_n=q_n, ps=ps: e.activation(out=q_n[:], in_=ps[:], func=AF.Copy), reads=[bps], writes=[bqn])
                        else:
                            s_q, bsq = sq.next()
                            P.op("act", lambda e, s_q=s_q, ps=ps: e.activation(out=s_q[:], in_=ps[:], func=AF.Square), reads=[bps], writes=[bsq])
                            p2, bp2 = ps2.next()
                            P.op("pe", lambda e, p2=p2, s_q=s_q: e.matmul(p2[:], lhsT=blockones, rhs=s_q[:], start=True, stop=True),
                                 reads=[bsq, b_const], writes=[bp2])
                            r_t, brt = rr.next()
                            P.op("act", lambda e, r_t=r_t, p2=p2: e.activation(out=r_t[:], in_=p2[:], func=AF.Sqrt, scale=1.0 / 64, bias=epsT[:, 0:1]),
                                 reads=[bp2, b_const], writes=[brt])
                            P.op("dve", lambda e, r_t=r_t: e.reciprocal(out=r_t[:], in_=r_t[:]), reads=[brt], writes=[brt])
                            P.op("dve", lambda e, q_n=q_n, ps=ps, r_t=r_t, gi=gi: e.scalar_tensor_tensor(out=q_n[:], in0=ps[:], scalar=gqk[:, l, gi:gi + 1],
                                                                                                         in1=r_t[:], op0=ALU.mult, op1=ALU.mult),
                                 reads=[bps, brt, b_const], writes=[bqn])
                        P.dma("pool", lambda e, q_n=q_n, dst=dst, r0=r0, t0=t0: e.dma_start(out=dst[r0:r0 + 128, t0:t0 + 512], in_=q_n[:]),
                              reads=[bqn], writes=[])
                    for j in range(4):
                        tk = t0 + j * 128
                        pv, bpv = psv.next()
                        for kc in range(8):
                            P.op("pe", lambda e, pv=pv, h_T=h_T, kc=kc, j=j: e.matmul(pv[:], lhsT=h_T[:, kc, j * 128:(j + 1) * 128], rhs=Win[:, kc, 1024:1536],
                                                                                      start=(kc == 0), stop=(kc == 7)), reads=[bWin, bhT], writes=[bpv])
                        v_s, bvs = vst.next()
                        P.op("act", lambda e, v_s=v_s, pv=pv: e.activation(out=v_s[:, :, 0:64], in_=pv[:].rearrange("p (h d) -> p h d", d=64), func=AF.Copy),
                             reads=[bpv], writes=[bvs])
                        P.dma("pool", lambda e, v_s=v_s, tk=tk: e.dma_start(out=Va[PADR + tk:PADR + tk + 128, :], in_=v_s[:].rearrange("p h d -> p (h d)")),
                              reads=[bvs], writes=[])
                        pw, bpw = psv.next()
                        for kc in range(8):
                            P.op("pe", lambda e, pw=pw, h_T=h_T, kc=kc, j=j: e.matmul(pw[:, 0:256], lhsT=h_T[:, kc, j * 128:(j + 1) * 128], rhs=Win[:, kc, 2048:2304],
                                                                                      start=(kc == 0), stop=(kc == 7)), reads=[bWin, bhT], writes=[bpw])
                        vb_s, bvbs = vbst.next()
                        P.op("dve", lambda e, vb_s=vb_s, pw=pw: e.tensor_copy(out=vb_s[:, :, 0:64], in_=pw[:, 0:256].rearrange("p (h d) -> p h d", d=64)),
                             reads=[bpw], writes=[bvbs])
                        P.dma("pool", lambda e, vb_s=vb_s, tk=tk: e.dma_start(out=Vb[tk:tk + 128, :], in_=vb_s[:].rearrange("p h d -> p (h d)")),
                              reads=[bvbs], writes=[])
                P.barrier()

        def phase_B(l):
            with ExitStack() as st:
                dbias = sbuf(st, "dbias", [128, 24, 256], BF16)
                bdb = Buf()
                P.dma("sp", lambda e: e.dma_start(out=dbias[:], in_=dbias_d.rearrange("c h p f -> p (c h) f")), writes=[bdb])
                QT = sbuf(st, "QTB", [128, S], BF16)
                KT = sbuf(st, "KTB", [128, S + 2 * PADR], BF16)
                bQT, bKT = Buf(), Buf()
                P.op("pool", lambda e: e.memset(KT[:, 0:PADR], 0.0), writes=[bKT])
                P.op("pool", lambda e: e.memset(KT[:, PADR + S:], 0.0), writes=[bKT])
                vt = Rot([sbuf(st, "vtB%d" % i, [128, 256], BF16) for i in range(4)])
                pt = Rot([sbuf(st, "ptB%d" % i, [128, 256], BF16) for i in range(4)])
                ost = Rot([sbuf(st, "ostB%d" % i, [128, 65], F32) for i in range(4)])
                psS = Rot([psum(st, "psSB%d" % i) for i in range(3)])
                pso = [[(psum(st, "psoB%d_%d" % (hh, par)), Buf()) for par in range(2)] for hh in range(2)]
                for hp in range(4):
                    P.dma("sp", lambda e, hp=hp: e.dma_start(out=QT[:], in_=QTa[hp * 128:(hp + 1) * 128, :]), writes=[bQT])
                    P.dma("sp", lambda e, hp=hp: e.dma_start(out=KT[:, PADR:PADR + S], in_=KTa[hp * 128:(hp + 1) * 128, :]), writes=[bKT])
                    for c, dil in enumerate((1, 4, 16)):
                        nq = S // dil // 128
                        for r in range(dil):
                            for m in range(nq + 1):
                                base = r + dil * (128 * m - 64)
                                v_t, bvt = vt.next()
                                P.dma("sp", lambda e, v_t=v_t, base=base, dil=dil, hp=hp: e.dma_start(
                                    out=v_t[:], in_=Va[PADR + base:PADR + base + 127 * dil + 1:dil, hp * 256:(hp + 1) * 256]),
                                    writes=[bvt])
                                clo = 128 if m == 0 else 0
                                chi = 128 if m == nq else 256
                                q0 = r + dil * (128 * (m - 1) + clo)
                                ncol = chi - clo
                                for hh in range(2):
                                    h = 2 * hp + hh
                                    pb = 64 * hh
                                    ps, bps = psS.next()
                                    P.op("pe", lambda e, ps=ps, pb=pb, base=base, dil=dil, q0=q0, ncol=ncol, clo=clo, chi=chi: e.matmul(
                                        ps[:, clo:chi], lhsT=KT[pb:pb + 64, PADR + base:PADR + base + 127 * dil + 1:dil],
                                        rhs=QT[pb:pb + 64, q0:q0 + (ncol - 1) * dil + 1:dil], start=True, stop=False),
                                        reads=[bKT, bQT], writes=[bps])
                                    P.op("pe", lambda e, ps=ps, c=c, h=h, clo=clo, chi=chi: e.matmul(
                                        ps[:, clo:chi], lhsT=ident_bf, rhs=dbias[:, c * 8 + h, clo:chi], start=False, stop=True),
                                        reads=[bdb, b_const], writes=[bps])
                                    p_t, bpt = pt.next()
                                    P.op("act", lambda e, p_t=p_t, ps=ps, clo=clo, chi=chi: e.activation(out=p_t[:, clo:chi], in_=ps[:, clo:chi],
                                                                                                        func=AF.Exp, scale=0.125), reads=[bps], writes=[bpt])
                                    for j in (m - 1, m):
                                        if j < 0 or j >= nq:
                                            continue
                                        co = (j - (m - 1)) * 128
                                        po, bpo = pso[hh][j % 2]
                                        first = (j == m)
                                        P.op("pe", lambda e, po=po, p_t=p_t, v_t=v_t, co=co, hh=hh, first=first: e.matmul(
                                            po[:, 0:128], lhsT=p_t[:, co:co + 128], rhs=v_t[:, hh * 128:(hh + 1) * 128], start=first, stop=(not first)),
                                            reads=[bpt, bvt], writes=[bpo])
                                        if not first:
                                            o_s, bos = ost.next()
                                            P.op("dve", lambda e, o_s=o_s, po=po: e.tensor_copy(out=o_s[:], in_=po[:, 0:65]), reads=[bpo], writes=[bos])
                                            tok0 = r + dil * 128 * j
                                            P.dma("pool", lambda e, o_s=o_s, c=c, tok0=tok0, dil=dil, h=h: e.dma_start(
                                                out=ACCA[c, tok0:tok0 + 127 * dil + 1:dil, h * 65:(h + 1) * 65], in_=o_s[:]),
                                                reads=[bos], writes=[])
                P.barrier()

        def phase_C(l):
            with ExitStack() as st:
                namask = sbuf(st, "namask", [128, 21, 128], BF16)
                nab = sbuf(st, "nab", [128, 28, 128], BF16)
                nst = Rot([sbuf(st, "nabst%d" % i, [128, 4, 128], F32) for i in range(2)])
                bnm, bnab = Buf(), Buf()
                P.dma("sp", lambda e: e.dma_start(out=namask[:], in_=namask_d.rearrange("v p f -> p v f")), writes=[bnm])
                for jj in range(7):
                    n_s, bns = nst.next()
                    P.dma("sp", lambda e, n_s=n_s, jj=jj: e.dma_start(out=n_s[:], in_=nab_d[l, jj].rearrange("h p f -> p h f")), writes=[bns])
                    P.op("act", lambda e, n_s=n_s, jj=jj: e.activation(out=nab[:, jj * 4:(jj + 1) * 4, :], in_=n_s[:], func=AF.Copy, scale=8.0),
                         reads=[bns], writes=[bnab])
                QT = sbuf(st, "QTC", [128, S], BF16)
                KT = sbuf(st, "KTC", [128, S], BF16)
                bQT, bKT = Buf(), Buf()
                vt = Rot([sbuf(st, "vtC%d" % i, [128, 256], BF16) for i in range(4)])
                pt = Rot([sbuf(st, "ptC%d" % i, [128, 128], BF16) for i in range(4)])
                ost = Rot([sbuf(st, "ostC%d" % i, [128, 65], F32) for i in range(4)])
                psS = Rot([psum(st, "psSC%d" % i) for i in range(3)])
                pso = [Rot([psum(st, "psoC%d_%d" % (hh, i)) for i in range(2)]) for hh in range(2)]
                for hp in range(2):
                    P.dma("sp", lambda e, hp=hp: e.dma_start(out=QT[:], in_=QTb[hp * 128:(hp + 1) * 128, :]), writes=[bQT])
                    P.dma("sp", lambda e, hp=hp: e.dma_start(out=KT[:], in_=KTb[hp * 128:(hp + 1) * 128, :]), writes=[bKT])
                    for a in range(64):
                        tiles = _na_block_tiles(a)
                        pos = [pso[hh].next() for hh in range(2)]
                        for ti, (kt, vi, jp) in enumerate(tiles):
                            v_t, bvt = vt.next()
                            P.dma("sp", lambda e, v_t=v_t, kt=kt, hp=hp: e.dma_start(out=v_t[:], in_=Vb[kt * 128:(kt + 1) * 128, hp * 256:(hp + 1) * 256]),
                                  writes=[bvt])
                            for hh in range(2):
                                h = 2 * hp + hh
                                pb = 64 * hh
                                ps, bps = psS.next()
                                P.op("pe", lambda e, ps=ps, pb=pb, kt=kt, a=a: e.matmul(ps[:, 0:128], lhsT=KT[pb:pb + 64, kt * 128:(kt + 1) * 128],
                                                                                         rhs=QT[pb:pb + 64, a * 128:(a + 1) * 128], start=True, stop=False),
                                     reads=[bKT, bQT], writes=[bps])
                                P.op("pe", lambda e, ps=ps, jp=jp, h=h: e.matmul(ps[:, 0:128], lhsT=ident_bf, rhs=nab[:, (jp + 3) * 4 + h, :], start=False, stop=False),
                                     reads=[bnab, b_const], writes=[bps])
                                P.op("pe", lambda e, ps=ps, vi=vi: e.matmul(ps[:, 0:128], lhsT=ident_bf, rhs=namask[:, vi, :], start=False, stop=True),
                                     reads=[bnm, b_const], writes=[bps])
                                p_t, bpt = pt.next()
                                P.op("act", lambda e, p_t=p_t, ps=ps: e.activation(out=p_t[:], in_=ps[:, 0:128], func=AF.Exp, scale=0.125),
                                     reads=[bps], writes=[bpt])
                                po, bpo = pos[hh]
                                P.op("pe", lambda e, po=po, p_t=p_t, v_t=v_t, hh=hh, ti=ti, nt=len(tiles): e.matmul(
                                    po[:, 0:128], lhsT=p_t[:], rhs=v_t[:, hh * 128:(hh + 1) * 128], start=(ti == 0), stop=(ti == nt - 1)),
                                    reads=[bpt, bvt], writes=[bpo])
                        for hh in range(2):
                            h = 2 * hp + hh
                            po, bpo = pos[hh]
                            o_s, bos = ost.next()
                            P.op("dve", lambda e, o_s=o_s, po=po: e.tensor_copy(out=o_s[:], in_=po[:, 0:65]), reads=[bpo], writes=[bos])
                            P.dma("pool", lambda e, o_s=o_s, a=a, h=h: e.dma_start(out=ACCB[a * 128:(a + 1) * 128, h * 65:(h + 1) * 65], in_=o_s[:]),
                                  reads=[bos], writes=[])
                P.barrier()

        def phase_D(l):
            PI = math.pi
            with ExitStack() as st:
                lamraw = sbuf(st, "lamraw", [128, 3, 16], F32)
                bTraw = sbuf(st, "bTraw", [128, 4, 5, 64], F32)
                cT = sbuf(st, "cTD", [128, 2, 16, 16], F32)
                maskB = sbuf(st, "maskB", [128, 8], F32)
                maskC = sbuf(st, "maskC", [128, 2, 4, 8], F32)
                dcol = sbuf(st, "dcolD", [128, 2], F32)
                wgst = sbuf(st, "wgst", [128, 2, 256], F32)
                Wglu = sbuf(st, "WgluD", [128, 2, 256], BF16)
                bprm = Buf()
                P.dma("sp", lambda e: e.dma_start(out=lamraw[:], in_=s5lam_d[:, l]), writes=[bprm])
                P.dma("sp", lambda e: e.dma_start(out=bTraw[:], in_=s5bT_d[:, l]), writes=[bprm])
                P.dma("sp", lambda e: e.dma_start(out=cT[:], in_=s5cT_d[:, l]), writes=[bprm])
                P.dma("sp", lambda e: e.dma_start(out=maskB[:], in_=maskB_d[:, :]), writes=[bprm])
                P.dma("sp", lambda e: e.dma_start(out=maskC[:, 0], in_=maskC_d[:, :, :]), writes=[bprm])
                P.dma("sp", lambda e: e.dma_start(out=dcol[:], in_=s5d_d[:, l, :]), writes=[bprm])
                P.dma("sp", lambda e: e.dma_start(out=wgst[:], in_=w_glu[l].rearrange("(k p) j -> p k j", p=128)), writes=[bprm])
                P.op("dve", lambda e: e.tensor_copy(out=Wglu[:], in_=wgst[:]), reads=[bprm], writes=[bprm])
                P.op("dve", lambda e: e.tensor_scalar(out=maskC[:, 1], in0=maskC[:, 0], scalar1=-1.0, scalar2=None, op0=ALU.mult), reads=[bprm], writes=[bprm])

                def disc(A, W, LD, F, full, tag):
                    t = {}
                    for nm in ("dt", "a", "mag", "ang", "y", "sn", "cs", "abr", "abi", "t1", "t2", "zr", "gr", "gi"):
                        t[nm] = sbuf(st, tag + nm, [128, F], F32)
                    b = bprm
                    P.op("act", lambda e: e.activation(out=t["dt"][:], in_=LD, func=AF.Exp), reads=[b], writes=[b])
                    P.op("dve", lambda e: e.tensor_scalar(out=t["a"][:], in0=A, scalar1=-1e-4, scalar2=None, op0=ALU.min), reads=[b], writes=[b])
                    P.op("dve", lambda e: e.tensor_tensor(out=t["mag"][:], in0=t["a"][:], in1=t["dt"][:], op=ALU.mult), reads=[b], writes=[b])
                    P.op("act", lambda e: e.activation(out=t["mag"][:], in_=t["mag"][:], func=AF.Exp), reads=[b], writes=[b])
                    P.op("dve", lambda e: e.tensor_tensor(out=t["ang"][:], in0=W, in1=t["dt"][:], op=ALU.mult), reads=[b], writes=[b])
                    ki = sbuf(st, tag + "ki", [128, F], I32)
                    for (dst, sh) in (("sn", 0.0), ("cs", 0.5 * PI)):
                        P.op("dve", lambda e, sh=sh: e.tensor_scalar(out=t["y"][:], in0=t["ang"][:], scalar1=sh, scalar2=None, op0=ALU.add), reads=[b], writes=[b])
                        P.op("dve", lambda e: e.tensor_scalar(out=t["t1"][:], in0=t["y"][:], scalar1=1.0 / (2.0 * PI), scalar2=None, op0=ALU.mult), reads=[b], writes=[b])
                        P.op("dve", lambda e: e.tensor_copy(out=ki[:], in_=t["t1"][:]), reads=[b], writes=[b])
                        P.op("dve", lambda e: e.tensor_copy(out=t["t1"][:], in_=ki[:]), reads=[b], writes=[b])
                        P.op("dve", lambda e: e.tensor_scalar(out=t["t1"][:], in0=t["t1"][:], scalar1=-2.0 * PI, scalar2=None, op0=ALU.mult), reads=[b], writes=[b])
                        P.op("dve", lambda e: e.tensor_tensor(out=t["y"][:], in0=t["y"][:], in1=t["t1"][:], op=ALU.add), reads=[b], writes=[b])
                        P.op("dve", lambda e: e.tensor_scalar(out=t["y"][:], in0=t["y"][:], scalar1=-PI, scalar2=PI, op0=ALU.max, op1=ALU.min), reads=[b], writes=[b])
                        P.op("act", lambda e, dst=dst: e.activation(out=t[dst][:], in_=t["y"][:], func=AF.Sin), reads=[b], writes=[b])
                    P.op("dve", lambda e: e.tensor_tensor(out=t["abr"][:], in0=t["mag"][:], in1=t["cs"][:], op=ALU.mult), reads=[b], writes=[b])
                    P.op("dve", lambda e: e.tensor_tensor(out=t["abi"][:], in0=t["mag"][:], in1=t["sn"][:], op=ALU.mult), reads=[b], writes=[b])
                    if full:
                        tt_ = lambda o, i0, i1, op: P.op("dve", lambda e: e.tensor_tensor(out=o, in0=i0, in1=i1, op=op), reads=[b], writes=[b])
                        tt_(t["t1"][:], t["a"][:], t["a"][:], ALU.mult)
                        tt_(t["t2"][:], W, W, ALU.mult)
                        tt_(t["t1"][:], t["t1"][:], t["t2"][:], ALU.add)
                        P.op("dve", lambda e: e.reciprocal(out=t["t1"][:], in_=t["t1"][:]), reads=[b], writes=[b])
                        P.op("dve", lambda e: e.tensor_scalar(out=t["zr"][:], in0=t["abr"][:], scalar1=-1.0, scalar2=None, op0=ALU.add), reads=[b], writes=[b])
                        tt_(t["gr"][:], t["zr"][:], t["a"][:], ALU.mult)
                        tt_(t["t2"][:], t["abi"][:], W, ALU.mult)
                        tt_(t["gr"][:], t["gr"][:], t["t2"][:], ALU.add)
                        tt_(t["gr"][:], t["gr"][:], t["t1"][:], ALU.mult)
                        tt_(t["gi"][:], t["abi"][:], t["a"][:], ALU.mult)
                        tt_(t["t2"][:], t["zr"][:], W, ALU.mult)
                        tt_(t["gi"][:], t["gi"][:], t["t2"][:], ALU.subtract)
                        tt_(t["gi"][:], t["gi"][:], t["t1"][:], ALU.mult)
                    return t

                NK = 11
                tl = disc(lamraw[:, 0, :], lamraw[:, 1, :], lamraw[:, 2, :], 16, False, "dl_")
                lamP = sbuf(st, "lamP", [128, 16, NK, 3], F32)
                tq = sbuf(st, "tqD", [128, 2, 16], F32)
                b = bprm
                P.op("dve", lambda e: e.tensor_copy(out=lamP[:, :, 0, 0], in_=tl["abr"][:]), reads=[b], writes=[b])
                P.op("dve", lambda e: e.tensor_copy(out=lamP[:, :, 0, 1], in_=tl["abi"][:]), reads=[b], writes=[b])
                for k in range(NK):
                    if k > 0:
                        P.op("dve", lambda e, k=k: e.tensor_tensor(out=tq[:, 0, :], in0=lamP[:, :, k - 1, 0], in1=lamP[:, :, k - 1, 0], op=ALU.mult), reads=[b], writes=[b])
                        P.op("dve", lambda e, k=k: e.tensor_tensor(out=tq[:, 1, :], in0=lamP[:, :, k - 1, 1], in1=lamP[:, :, k - 1, 1], op=ALU.mult), reads=[b], writes=[b])
                        P.op("dve", lambda e, k=k: e.tensor_tensor(out=lamP[:, :, k, 0], in0=tq[:, 0, :], in1=tq[:, 1, :], op=ALU.subtract), reads=[b], writes=[b])
                        P.op("dve", lambda e, k=k: e.scalar_tensor_tensor(out=lamP[:, :, k, 1], in0=lamP[:, :, k - 1, 0], scalar=2.0, in1=lamP[:, :, k - 1, 1],
                                                                          op0=ALU.mult, op1=ALU.mult), reads=[b], writes=[b])
                    P.op("dve", lambda e, k=k: e.tensor_scalar(out=lamP[:, :, k, 2], in0=lamP[:, :, k, 1], scalar1=-1.0, scalar2=None, op0=ALU.mult), reads=[b], writes=[b])
                A3 = sbuf(st, "A3D", [128, 4, 64], F32)
                W3 = sbuf(st, "W3D", [128, 4, 64], F32)
                L3 = sbuf(st, "L3D", [128, 4, 64], F32)
                Br3 = sbuf(st, "Br3D", [128, 4, 64], F32)
                Bi3 = sbuf(st, "Bi3D", [128, 4, 64], F32)
                for (dst, idx) in ((Br3, 0), (Bi3, 1), (A3, 2), (W3, 3), (L3, 4)):
                    P.op("dve", lambda e, dst=dst, idx=idx: e.tensor_copy(out=dst[:], in_=bTraw[:, :, idx, :]), reads=[b], writes=[b])
                fl = lambda t_: t_[:].rearrange("p a n -> p (a n)")
                tb = disc(fl(A3), fl(W3), fl(L3), 256, True, "db_")
                Bb = sbuf(st, "BbD", [128, 2, 256], F32)
                tt_ = lambda o, i0, i1, op: P.op("dve", lambda e: e.tensor_tensor(out=o, in0=i0, in1=i1, op=op), reads=[b], writes=[b])
                tt_(Bb[:, 0, :], tb["gr"][:], fl(Br3), ALU.mult)
                tt_(tb["t2"][:], tb["gi"][:], fl(Bi3), ALU.mult)
                tt_(Bb[:, 0, :], Bb[:, 0, :], tb["t2"][:], ALU.subtract)
                tt_(Bb[:, 1, :], tb["gr"][:], fl(Bi3), ALU.mult)
                tt_(tb["t2"][:], tb["gi"][:], fl(Br3), ALU.mult)
                tt_(Bb[:, 1, :], Bb[:, 1, :], tb["t2"][:], ALU.add)
                Bblk = sbuf(st, "BblkD", [128, 4, 2, 512], BF16)
                for dc in range(4):
                    for ri in range(2):
                        P.op("dve", lambda e, dc=dc, ri=ri: e.tensor_tensor(
                            out=Bblk[:, dc, ri, :].rearrange("p (g n) -> p g n", n=64),
                            in0=Bb[:, ri, dc * 64:(dc + 1) * 64].unsqueeze(1).to_broadcast([128, 8, 64]),
                            in1=maskB[:, :].unsqueeze(2).to_broadcast([128, 8, 64]), op=ALU.mult), reads=[b], writes=[b])
                Cblk = sbuf(st, "CblkD", [128, 16, 2, 128], F32)
                for dp in range(16):
                    pl = dp % 4
                    for k in range(2):
                        P.op("dve", lambda e, dp=dp, pl=pl, k=k: e.tensor_tensor(
                            out=Cblk[:, dp, k, :].rearrange("p (g o) -> p g o", o=16),
                            in0=cT[:, k, dp, :].unsqueeze(1).to_broadcast([128, 8, 16]),
                            in1=maskC[:, k, pl, :].unsqueeze(2).to_broadcast([128, 8, 16]), op=ALU.mult), reads=[b], writes=[b])

                G = sbuf(st, "GD", [128, 2, S], BF16)
                bG = Buf()
                UTs = sbuf(st, "UTsD", [128, S], BF16)
                Yacc = sbuf(st, "YaccD", [128, S], F32)
                bUT, bY = Buf(), Buf()
                X = [[(sbuf(st, "XD%d%d" % (i, j), [128, SEG], F32), Buf()) for j in range(2)] for i in range(2)]
                endst = sbuf(st, "endstD", [128, 2], F32)
                ptmp = sbuf(st, "ptmpD", [128, SEG], F32)
                bptmp = Buf()
                bend = Buf()
                psr = Rot([psum(st, "psrD%d" % i) for i in range(2)])
                psi = Rot([psum(st, "psiD%d" % i) for i in range(2)])
                psY = Rot([psum(st, "psYD%d" % i) for i in range(2)])

                def stt(eng, out, in0, scalar, in1, rd, wr):
                    P.op(eng, lambda e: e.scalar_tensor_tensor(out=out, in0=in0, scalar=scalar, in1=in1, op0=ALU.mult, op1=ALU.add), reads=rd + [bprm], writes=wr)

                for ct in range(2):
                    P.dma("sp", lambda e, ct=ct: e.dma_start(out=UTs[:], in_=UT[ct * 128:(ct + 1) * 128, :]), writes=[bUT])
                    for q4 in range(4):
                        P.op("act", lambda e, q4=q4, ct=ct: e.activation(out=Yacc[:, q4 * 2048:(q4 + 1) * 2048], in_=UTs[:, q4 * 2048:(q4 + 1) * 2048], func=AF.Copy,
                                                                         scale=dcol[:, ct:ct + 1]), reads=[bUT, bprm], writes=[bY])
                    for d in range(2):
                        for pl in range(4):
                            P8 = ct * 4 + pl
                            dp = d * 8 + P8
                            dc = d * 2 + ct
                            segs = list(range(S // SEG))
                            if d == 1:
                                segs = segs[::-1]
                            for si, sg_ in enumerate(segs):
                                c0 = sg_ * SEG
                                (Ar, bAr), (Ai, bAi) = X[0]
                                for b4 in range(SEG // 512):
                                    pr, bpr = psr.next()
                                    pi_, bpi = psi.next()
                                    P.op("pe", lambda e, pr=pr, dc=dc, pl=pl, c0=c0, b4=b4: e.matmul(pr[:], lhsT=Bblk[:, dc, 0, pl * 128:(pl + 1) * 128],
                                                                                                    rhs=UTs[:, c0 + b4 * 512:c0 + (b4 + 1) * 512], start=True, stop=True),
                                         reads=[bprm, bUT], writes=[bpr])
                                    P.op("pe", lambda e, pi_=pi_, dc=dc, pl=pl, c0=c0, b4=b4: e.matmul(pi_[:], lhsT=Bblk[:, dc, 1, pl * 128:(pl + 1) * 128],
                                                                                                      rhs=UTs[:, c0 + b4 * 512:c0 + (b4 + 1) * 512], start=True, stop=True),
                                         reads=[bprm, bUT], writes=[bpi])
                                    P.op("act", lambda e, pr=pr, Ar=Ar, b4=b4: e.activation(out=Ar[:, b4 * 512:(b4 + 1) * 512], in_=pr[:], func=AF.Copy), reads=[bpr], writes=[bAr])
                                    P.op("act", lambda e, pi_=pi_, Ai=Ai, b4=b4: e.activation(out=Ai[:, b4 * 512:(b4 + 1) * 512], in_=pi_[:], func=AF.Copy), reads=[bpi], writes=[bAi])
                                if si > 0:
                                    col = 0 if d == 0 else SEG - 1
                                    cs_ = slice(col, col + 1)
                                    stt("dve", Ar[:, cs_], endst[:, 0:1], lamP[:, dp, 0, 0:1], Ar[:, cs_], [bend, bAr], [bAr])
                                    stt("dve", Ar[:, cs_], endst[:, 1:2], lamP[:, dp, 0, 2:3], Ar[:, cs_], [bend, bAr], [bAr])
                                    stt("dve", Ai[:, cs_], endst[:, 1:2], lamP[:, dp, 0, 0:1], Ai[:, cs_], [bend, bAi], [bAi])
                                    stt("dve", Ai[:, cs_], endst[:, 0:1], lamP[:, dp, 0, 1:2], Ai[:, cs_], [bend, bAi], [bAi])
                                cur = 0
                                for k in range(NK):
                                    dd = 1 << k
                                    (Sr, bSr), (Si, bSi) = X[cur]
                                    (Dr, bDr), (Di, bDi) = X[1 - cur]
                                    if d == 0:
                                        o, i_, hd = slice(dd, SEG), slice(0, SEG - dd), slice(0, dd)
                                    else:
                                        o, i_, hd = slice(0, SEG - dd), slice(dd, SEG), slice(SEG - dd, SEG)
                                    stt("dve", Dr[:, o], Sr[:, i_], lamP[:, dp, k, 0:1], Sr[:, o], [bSr], [bDr])
                                    stt("dve", Dr[:, o], Si[:, i_], lamP[:, dp, k, 2:3], Dr[:, o], [bSi, bDr], [bDr])
                                    stt("dve", Di[:, o], Si[:, i_], lamP[:, dp, k, 0:1], Si[:, o], [bSi], [bDi])
                                    stt("dve", Di[:, o], Sr[:, i_], lamP[:, dp, k, 1:2], Di[:, o], [bSr, bDi], [bDi])
                                    P.op("act", lambda e, Dr=Dr, Sr=Sr, hd=hd: e.activation(out=Dr[:, hd], in_=Sr[:, hd], func=AF.Copy), reads=[bSr], writes=[bDr])
                                    P.op("act", lambda e, Di=Di, Si=Si, hd=hd: e.activation(out=Di[:, hd], in_=Si[:, hd], func=AF.Copy), reads=[bSi], writes=[bDi])
                                    cur = 1 - cur
                                (Fr, bFr), (Fi, bFi) = X[cur]
                                lc = SEG - 1 if d == 0 else 0
                                P.op("act", lambda e, Fr=Fr, lc=lc: e.activation(out=endst[:, 0:1], in_=Fr[:, lc:lc + 1], func=AF.Copy), reads=[bFr], writes=[bend])
                                P.op("act", lambda e, Fi=Fi, lc=lc: e.activation(out=endst[:, 1:2], in_=Fi[:, lc:lc + 1], func=AF.Copy), reads=[bFi], writes=[bend])
                                for b4 in range(SEG // 512):
                                    py, bpy = psY.next()
                                    P.op("pe", lambda e, py=py, Fr=Fr, dp=dp, b4=b4: e.matmul(py[:], lhsT=Cblk[:, dp, 0, :], rhs=Fr[:, b4 * 512:(b4 + 1) * 512], start=True, stop=False),
                                         reads=[bprm, bFr], writes=[bpy])
                                    P.op("pe", lambda e, py=py, Fi=Fi, dp=dp, b4=b4: e.matmul(py[:], lhsT=Cblk[:, dp, 1, :], rhs=Fi[:, b4 * 512:(b4 + 1) * 512], start=False, stop=True),
                                         reads=[bprm, bFi], writes=[bpy])
                                    P.op("dve", lambda e, py=py, c0=c0, b4=b4: e.tensor_tensor(out=Yacc[:, c0 + b4 * 512:c0 + (b4 + 1) * 512], in0=py[:],
                                                                                              in1=Yacc[:, c0 + b4 * 512:c0 + (b4 + 1) * 512], op=ALU.add), reads=[bpy, bY], writes=[bY])
                    for q4 in range(4):
                        P.op("act", lambda e, q4=q4, ct=ct: e.activation(out=G[:, ct, q4 * 2048:(q4 + 1) * 2048], in_=Yacc[:, q4 * 2048:(q4 + 1) * 2048], func=AF.Gelu),
                             reads=[bY], writes=[bG])
                sig = Rot([sbuf(st, "sigD%d" % i, [128, 512], F32) for i in range(2)])
                oc = Rot([sbuf(st, "ocD%d" % i, [128, 2, 512], F32) for i in range(2)])
                sq = Rot([sbuf(st, "sqD%d" % i, [128, 2, 512], BF16) for i in range(2)])
                rs = Rot([sbuf(st, "rsD%d" % i, [128, 512], F32) for i in range(2)])
                ocn = Rot([sbuf(st, "ocnD%d" % i, [128, 2, 512], BF16) for i in range(2)])
                for blk in range(S // 512):
                    t0 = blk * 512
                    o_c, boc = oc.next()
                    s_q, bsq = sq.next()
                    for jt in range(2):
                        pz, bpz = psr.next()
                        for kt in range(2):
                            P.op("pe", lambda e, pz=pz, kt=kt, jt=jt, t0=t0: e.matmul(pz[:], lhsT=Wglu[:, kt, jt * 128:(jt + 1) * 128], rhs=G[:, kt, t0:t0 + 512],
                                                                                      start=(kt == 0), stop=(kt == 1)), reads=[bprm, bG], writes=[bpz])
                        s_g, bsg = sig.next()
                        P.op("act", lambda e, s_g=s_g, pz=pz: e.activation(out=s_g[:], in_=pz[:], func=AF.Sigmoid), reads=[bpz], writes=[bsg])
                        P.op("dve", lambda e, o_c=o_c, s_g=s_g, jt=jt, t0=t0: e.tensor_tensor(out=o_c[:, jt, :], in0=G[:, jt, t0:t0 + 512], in1=s_g[:], op=ALU.mult),
                             reads=[bG, bsg], writes=[boc])
                        P.op("act", lambda e, o_c=o_c, s_q=s_q, jt=jt: e.activation(out=s_q[:, jt, :], in_=o_c[:, jt, :], func=AF.Square),
                             reads=[boc], writes=[bsq])
                    p2, bp2 = psY.next()
                    for jt in range(2):
                        P.op("pe", lambda e, p2=p2, s_q=s_q, jt=jt: e.matmul(p2[:], lhsT=ones_bf, rhs=s_q[:, jt, :], start=(jt == 0), stop=(jt == 1)),
                             reads=[bsq, b_const], writes=[bp2])
                    r_s, brs = rs.next()
                    P.op("act", lambda e, r_s=r_s, p2=p2: e.activation(out=r_s[:], in_=p2[:], func=AF.Sqrt, scale=1.0 / 256, bias=epsT[:, 0:1]),
                         reads=[bp2, b_const], writes=[brs])
                    P.op("dve", lambda e, r_s=r_s: e.reciprocal(out=r_s[:], in_=r_s[:]), reads=[brs], writes=[brs])
                    o_n, bon = ocn.next()
                    P.op("dve", lambda e, o_n=o_n, o_c=o_c, r_s=r_s: e.tensor_tensor(out=o_n[:], in0=o_c[:], in1=r_s[:].unsqueeze(1).to_broadcast([128, 2, 512]), op=ALU.mult),
                         reads=[boc, brs], writes=[bon])
                    P.dma("pool", lambda e, o_n=o_n, t0=t0: e.dma_start(out=MIXC[:, t0:t0 + 512].rearrange("(k p) t -> p k t", p=128), in_=o_n[:]), reads=[bon])
                P.barrier()

        def phase_E(l):
            xsrc = x_in if l == 0 else OUT
            with ExitStack() as st:
                Wout = sbuf(st, "Wout", [128, 8, 1024], BF16)
                Wr = sbuf(st, "Wr", [128, 8, 16], F32)
                gffn = sbuf(st, "gffn", [128, 1024], F32)
                bW, bWr, bg = Buf(), Buf(), Buf()
                wst = Rot([sbuf(st, "wstE%d" % i, [128, 1024], F32) for i in range(2)])
                for kc in range(8):
                    w, bw = wst.next()
                    P.dma("sp", lambda e, w=w, kc=kc: e.dma_start(out=w[:], in_=w_out[l, kc * 128:(kc + 1) * 128, :]), writes=[bw])
                    P.op("dve", lambda e, w=w, kc=kc: e.tensor_scalar(out=Wout[:, kc, :], in0=w[:], scalar1=gcol[:, l, 1, kc:kc + 1],
                                                                       scalar2=None, op0=ALU.mult), reads=[bw, b_const], writes=[bW])
                P.dma("sp", lambda e: e.dma_start(out=Wr[:], in_=w_router[l].rearrange("(k p) e -> p k e", p=128)), writes=[bWr])
                P.dma("sp", lambda e: e.dma_start(out=gffn[:], in_=gffn_d[l]), writes=[bg])
                acc = Rot([sbuf(st, "accE%d" % i, [128, 3, 520], F32) for i in range(2)])
                accb = Rot([sbuf(st, "accbE%d" % i, [128, 260], F32) for i in range(2)])
                xt = Rot([sbuf(st, "xtE%d" % i, [128, 1024], F32) for i in range(2)])
                mc = Rot([sbuf(st, "mcE%d" % i, [128, 2, 128], BF16) for i in range(2)])
                sa = Rot([sbuf(st, "saE%d" % i, [128, 520], F32) for i in range(2)])
                rd = Rot([sbuf(st, "rdE%d" % i, [128, 16], F32) for i in range(2)])
                oab = Rot([sbuf(st, "oabE%d" % i, [128, 768], F32) for i in range(2)])
                junk = sbuf(st, "junkE", [128, 1024], BF16)
                bjunk = Buf()
                ssq = Rot([sbuf(st, "ssqE%d" % i, [128, 4], F32) for i in range(2)])
                mixn = Rot([sbuf(st, "mixnE%d" % i, [128, 768], BF16) for i in range(2)])
                mT = Rot([sbuf(st, "mTE%d" % i, [128, 6, 128], BF16) for i in range(2)])
                x1 = Rot([sbuf(st, "x1E%d" % i, [128, 1024], F32) for i in range(2)])
                hf = Rot([sbuf(st, "hfE%d" % i, [128, 1024], F32) for i in range(2)])
                hfb = Rot([sbuf(st, "hfbE%d" % i, [128, 1024], BF16) for i in range(2)])
                hfT = Rot([sbuf(st, "hfTE%d" % i, [128, 8, 128], F32) for i in range(2)])
                ex = Rot([sbuf(st, "exE%d" % i, [128, 16], F32) for i in range(2)])
                se = Rot([sbuf(st, "seE%d" % i, [128, 2], F32) for i in range(2)])
                aff = Rot([sbuf(st, "affE%d" % i, [128, 16], F32) for i in range(2)])
                aT = Rot([sbuf(st, "aTE%d" % i, [16, 128], F32) for i in range(2)])
                pT = Rot([psum(st, "pTE%d" % i, [128, 8, 128], BF16) for i in range(1)])
                psx = Rot([psum(st, "psxE%d" % i) for i in range(2)])
                pTf = Rot([psum(st, "pTfE%d" % i, [128, 4, 128], F32) for i in range(2)])
                psl = Rot([psum(st, "pslE%d" % i) for i in range(2)])
                for tt in range(NT):
                    t0 = tt * 128
                    a_t, ba = acc.next()
                    P.dma("sp", lambda e, a_t=a_t, t0=t0: e.dma_start(out=a_t[:], in_=ACCA[:, t0:t0 + 128, :].rearrange("c p f -> p c f")),
                          writes=[ba])
                    ab_t, bab = accb.next()
                    P.dma("sp", lambda e, ab_t=ab_t, t0=t0: e.dma_start(out=ab_t[:], in_=ACCB[t0:t0 + 128, :]), writes=[bab])
                    x_t, bx = xt.next()
                    P.dma("sp", lambda e, x_t=x_t, t0=t0: e.dma_start(out=x_t[:], in_=xsrc[t0:t0 + 128, :]), writes=[bx])
                    m_c, bmc = mc.next()
                    P.dma("sp", lambda e, m_c=m_c, t0=t0: e.dma_start(out=m_c[:], in_=MIXC[:, t0:t0 + 128].rearrange("(k p) t -> p k t", p=128)),
                          writes=[bmc])
                    s_a, bsa = sa.next()
                    P.op("dve", lambda e, s_a=s_a, a_t=a_t: e.tensor_tensor(out=s_a[:], in0=a_t[:, 0, :], in1=a_t[:, 1, :], op=ALU.add), reads=[ba], writes=[bsa])
                    P.op("dve", lambda e, s_a=s_a, a_t=a_t: e.tensor_tensor(out=s_a[:], in0=s_a[:], in1=a_t[:, 2, :], op=ALU.add), reads=[ba, bsa], writes=[bsa])
                    r_d, brd = rd.next()
                    sa3 = s_a[:].rearrange("p (h d) -> p h d", d=65)
                    ab3 = ab_t[:].rearrange("p (h d) -> p h d", d=65)
                    P.op("dve", lambda e, r_d=r_d, sa3=sa3: e.reciprocal(out=r_d[:, 0:8], in_=sa3[:, :, 64]), reads=[bsa], writes=[brd])
                    P.op("dve", lambda e, r_d=r_d, ab3=ab3: e.reciprocal(out=r_d[:, 8:12], in_=ab3[:, :, 64]), reads=[bab], writes=[brd])
                    o_t, bo = oab.next()
                    P.op("dve", lambda e, o_t=o_t, sa3=sa3, r_d=r_d: e.tensor_tensor(
                        out=o_t[:, 0:512].rearrange("p (h d) -> p h d", d=64), in0=sa3[:, :, 0:64],
                        in1=r_d[:, 0:8].unsqueeze(2).to_broadcast([128, 8, 64]), op=ALU.mult), reads=[bsa, brd], writes=[bo])
                    P.op("dve", lambda e, o_t=o_t, ab3=ab3, r_d=r_d: e.tensor_tensor(
                        out=o_t[:, 512:768].rearrange("p (h d) -> p h d", d=64), in0=ab3[:, :, 0:64],
                        in1=r_d[:, 8:12].unsqueeze(2).to_broadcast([128, 4, 64]), op=ALU.mult), reads=[bab, brd], writes=[bo])
                    s_s, bss = ssq.next()
                    P.op("act", lambda e, o_t=o_t, s_s=s_s: e.activation(out=junk[:, 0:512], in_=o_t[:, 0:512], func=AF.Square, accum_out=s_s[:, 0:1]),
                         reads=[bo], writes=[bjunk, bss])
                    P.op("act", lambda e, o_t=o_t, s_s=s_s: e.activation(out=junk[:, 0:256], in_=o_t[:, 512:768], func=AF.Square, accum_out=s_s[:, 1:2]),
                         reads=[bo], writes=[bjunk, bss])
                    P.op("act", lambda e, s_s=s_s: e.activation(out=s_s[:, 0:1], in_=s_s[:, 0:1], func=AF.Sqrt, scale=1.0 / 512, bias=epsT[:, 0:1]),
                         reads=[bss, b_const], writes=[bss])
                    P.op("act", lambda e, s_s=s_s: e.activation(out=s_s[:, 1:2], in_=s_s[:, 1:2], func=AF.Sqrt, scale=1.0 / 256, bias=epsT[:, 0:1]),
                         reads=[bss, b_const], writes=[bss])
                    P.op("dve", lambda e, s_s=s_s: e.reciprocal(out=s_s[:, 0:2], in_=s_s[:, 0:2]), reads=[bss], writes=[bss])
                    m_n, bmn = mixn.next()
                    P.op("dve", lambda e, m_n=m_n, o_t=o_t, s_s=s_s: e.tensor_scalar(out=m_n[:, 0:512], in0=o_t[:, 0:512], scalar1=s_s[:, 0:1],
                                                                                     scalar2=None, op0=ALU.mult), reads=[bo, bss], writes=[bmn])
                    P.op("act", lambda e, m_n=m_n, o_t=o_t, s_s=s_s: e.activation(out=m_n[:, 512:768], in_=o_t[:, 512:768], func=AF.Copy, scale=s_s[:, 1:2]),
                         reads=[bo, bss], writes=[bmn])
                    p_T, bpT = pT.next()
                    for kc in range(6):
                        P.op("pe", lambda e, p_T=p_T, m_n=m_n, kc=kc: e.transpose(out=p_T[:, kc, :], in_=m_n[:, kc * 128:(kc + 1) * 128], identity=ident_bf),
                             reads=[bmn, b_const], writes=[bpT])
                    m_T, bmT = mT.next()
                    P.op("act", lambda e, m_T=m_T, p_T=p_T: e.activation(out=m_T[:], in_=p_T[:, 0:6, :], func=AF.Copy), reads=[bpT], writes=[bmT])
                    x_1, bx1 = x1.next()
                    for half in range(2):
                        px, bpx = psx.next()
                        for kc in range(8):
                            if kc < 6:
                                P.op("pe", lambda e, px=px, m_T=m_T, kc=kc, half=half: e.matmul(px[:], lhsT=m_T[:, kc, :], rhs=Wout[:, kc, half * 512:(half + 1) * 512],
                                                                                                 start=(kc == 0), stop=False), reads=[bmT, bW], writes=[bpx])
                            else:
                                P.op("pe", lambda e, px=px, m_c=m_c, kc=kc, half=half: e.matmul(px[:], lhsT=m_c[:, kc - 6, :], rhs=Wout[:, kc, half * 512:(half + 1) * 512],
                                                                                                 start=False, stop=(kc == 7)), reads=[bmc, bW], writes=[bpx])
                        P.op("dve", lambda e, x_1=x_1, px=px, x_t=x_t, half=half: e.tensor_tensor(out=x_1[:, half * 512:(half + 1) * 512], in0=px[:],
                                                                                                 in1=x_t[:, half * 512:(half + 1) * 512], op=ALU.add),
                             reads=[bpx, bx], writes=[bx1])
                    P.dma("pool", lambda e, x_1=x_1, t0=t0: e.dma_start(out=OUT[t0:t0 + 128, :], in_=x_1[:]), reads=[bx1])
                    P.op("act", lambda e, x_1=x_1, s_s=s_s: e.activation(out=junk[:], in_=x_1[:], func=AF.Square, accum_out=s_s[:, 2:3]),
                         reads=[bx1], writes=[bjunk, bss])
                    P.op("act", lambda e, s_s=s_s: e.activation(out=s_s[:, 2:3], in_=s_s[:, 2:3], func=AF.Sqrt, scale=1.0 / D, bias=epsT[:, 0:1]),
                         reads=[bss, b_const], writes=[bss])
                    P.op("dve", lambda e, s_s=s_s: e.reciprocal(out=s_s[:, 2:3], in_=s_s[:, 2:3]), reads=[bss], writes=[bss])
                    h_f, bhf = hf.next()
                    P.op("dve", lambda e, h_f=h_f, x_1=x_1, s_s=s_s: e.scalar_tensor_tensor(out=h_f[:], in0=x_1[:], scalar=s_s[:, 2:3], in1=gffn[:],
                                                                                           op0=ALU.mult, op1=ALU.mult), reads=[bx1, bss, bg], writes=[bhf])
                    h_b, bhb = hfb.next()
                    P.op("act", lambda e, h_b=h_b, h_f=h_f: e.activation(out=h_b[:], in_=h_f[:], func=AF.Copy), reads=[bhf], writes=[bhb])
                    P.dma("pool", lambda e, h_b=h_b, t0=t0: e.dma_start(out=HF[t0:t0 + 128, :], in_=h_b[:]), reads=[bhb], writes=[])
                    h_T, bhT = hfT.next()
                    for q4 in range(2):
                        pf, bpf = pTf.next()
                        for k4 in range(4):
                            kc = q4 * 4 + k4
                            P.op("pe", lambda e, pf=pf, h_f=h_f, kc=kc, k4=k4: e.transpose(out=pf[:, k4, :], in_=h_f[:, kc * 128:(kc + 1) * 128], identity=identf[:]),
                                 reads=[bhf, b_const], writes=[bpf])
                        P.op("act", lambda e, h_T=h_T, pf=pf, q4=q4: e.activation(out=h_T[:, q4 * 4:(q4 + 1) * 4, :], in_=pf[:], func=AF.Copy),
                             reads=[bpf], writes=[bhT])
                    pl, bpl = psl.next()
                    for kc in range(8):
                        P.op("pe", lambda e, pl=pl, h_T=h_T, kc=kc: e.matmul(pl[:, 0:16], lhsT=h_T[:, kc, :], rhs=Wr[:, kc, :], start=(kc == 0), stop=(kc == 7)),
                             reads=[bhT, bWr], writes=[bpl])
                    e_x, bex = ex.next()
                    s_e, bse = se.next()
                    P.op("act", lambda e, e_x=e_x, pl=pl, s_e=s_e: e.activation(out=e_x[:], in_=pl[:, 0:16], func=AF.Exp, accum_out=s_e[:, 0:1]),
                         reads=[bpl], writes=[bex, bse])
                    P.op("dve", lambda e, s_e=s_e: e.reciprocal(out=s_e[:, 1:2], in_=s_e[:, 0:1]), reads=[bse], writes=[bse])
                    a_f, baf = aff.next()
                    P.op("dve", lambda e, a_f=a_f, e_x=e_x, s_e=s_e: e.tensor_scalar(out=a_f[:], in0=e_x[:], scalar1=s_e[:, 1:2], scalar2=None, op0=ALU.mult),
                         reads=[bex, bse], writes=[baf])
                    P.dma("pool", lambda e, a_f=a_f, t0=t0: e.dma_start(out=AFFD[t0:t0 + 128, :], in_=a_f[:]), reads=[baf], writes=[])
                    pl2, bpl2 = psl.next()
                    P.op("pe", lambda e, pl2=pl2, a_f=a_f: e.transpose(out=pl2[0:16, 0:128], in_=a_f[:, 0:16], identity=identf[:]),
                         reads=[baf, b_const], writes=[bpl2])
                    a_T, baT = aT.next()
                    P.op("act", lambda e, pl2=pl2, a_T=a_T: e.activation(out=a_T[:], in_=pl2[0:16, 0:128], func=AF.Copy),
                         reads=[bpl2], writes=[baT])
                    P.dma("pool", lambda e, a_T=a_T, t0=t0: e.dma_start(out=AFFTD[:, t0:t0 + 128], in_=a_T[:]), reads=[baT])
                P.barrier()

        def phase_F(l):
            with ExitStack() as st:
                with ExitStack() as s1:
                    work = sbuf(s1, "workF", [16, S], F32)
                    mask = sbuf(s1, "maskF", [16, S], F32)
                    posf = sbuf(s1, "posF", [16, S], F32)
                    m8 = sbuf(s1, "m8F", [16, 8], F32)
                    ecst = sbuf(s1, "ecstF", [128, 18], F32)
                    bwk, bmk, bps_, bm8, bec = Buf(), Buf(), Buf(), Buf(), Buf()
                    P.dma("sp", lambda e: e.dma_start(out=ecst[:], in_=ecst_d[:, :]), writes=[bec])
                    affc = sbuf(s1, "affcF", [16, S], F32)
                    b_AFFT = Buf()
                    P.dma("sp", lambda e: e.dma_start(out=affc[:], in_=AFFTD[:, :]), writes=[b_AFFT])
                    P.dma("sp", lambda e: e.dma_start(out=work[:], in_=AFFTD[:, :]), writes=[bwk])
                    nit = CAP // 8
                    for it in range(nit):
                        P.op("dve", lambda e: e.max(out=m8[:], in_=work[:]), reads=[bwk], writes=[bm8])
                        if it < nit - 1:
                            P.op("dve", lambda e: e.match_replace(out=work[:], in_to_replace=m8[:], in_values=work[:], imm_value=-1.0),
                                 reads=[bm8, bwk], writes=[bwk])
                    P.op("dve", lambda e: e.tensor_scalar(out=mask[:], in0=affc[:], scalar1=m8[:, 7:8], scalar2=None, op0=ALU.is_ge),
                         reads=[b_AFFT, bm8], writes=[bmk])
                    P.op("pool", lambda e: e.memset(work[:], 1.0), reads=[bm8], writes=[bwk])
                    P.op("dve", lambda e: e.tensor_tensor_scan(out=posf[:], data0=work[:], data1=mask[:], initial=0.0, op0=ALU.mult, op1=ALU.add),
                         reads=[bwk, bmk], writes=[bps_])
                    afl = Rot([sbuf(s1, "aflF%d" % i, [128, 16], F32) for i in range(3)])
                    dst = Rot([sbuf(s1, "dstF%d" % i, [128, 16], F32) for i in range(3)])
                    dsi = Rot([sbuf(s1, "dsiF%d" % i, [128, 16], I32) for i in range(3)])
                    pair = Rot([sbuf(s1, "pairF%d" % i, [128, 16, 2], F32) for i in range(3)])
                    ptr = Rot([psum(s1, "ptrF%d" % i) for i in range(2)])
                    for tt in range(NT):
                        t0 = tt * 128
                        pt_, bpt_ = ptr.next()
                        P.op("pe", lambda e, pt_=pt_, t0=t0: e.transpose(out=pt_[:, 0:16], in_=mask[:, t0:t0 + 128], identity=identf[0:16, 0:16]),
                             reads=[bmk, b_const], writes=[bpt_])
                        P.op("pe", lambda e, pt_=pt_, t0=t0: e.transpose(out=pt_[:, 16:32], in_=posf[:, t0:t0 + 128], identity=identf[0:16, 0:16]),
                             reads=[bps_, b_const], writes=[bpt_])
                        a_l, bal = afl.next()
                        P.dma("sp", lambda e, a_l=a_l, t0=t0: e.dma_start(out=a_l[:], in_=AFFD[t0:t0 + 128, :]), writes=[bal])
                        d_t, bdt = dst.next()
                        P.op("dve", lambda e, d_t=d_t, pt_=pt_: e.tensor_scalar(out=d_t[:], in0=pt_[:, 16:32], scalar1=ecst[:, 16:17], scalar2=None, op0=ALU.subtract),
                             reads=[bpt_, bec], writes=[bdt])
                        P.op("dve", lambda e, d_t=d_t, pt_=pt_: e.tensor_tensor(out=d_t[:], in0=d_t[:], in1=pt_[:, 0:16], op=ALU.mult),
                             reads=[bpt_, bdt], writes=[bdt])
                        P.op("dve", lambda e, d_t=d_t: e.tensor_tensor(out=d_t[:], in0=d_t[:], in1=ecst[:, 0:16], op=ALU.add), reads=[bdt, bec], writes=[bdt])
                        d_i, bdi = dsi.next()
                        P.op("dve", lambda e, d_i=d_i, d_t=d_t: e.tensor_copy(out=d_i[:], in_=d_t[:]), reads=[bdt], writes=[bdi])
                        p_r, bpr = pair.next()
                        P.op("dve", lambda e, p_r=p_r, t0=t0: e.tensor_scalar(out=p_r[:, :, 0], in0=ecst[:, 17:18].to_broadcast([128, 16]), scalar1=float(t0),
                                                                               scalar2=None, op0=ALU.add), reads=[bec], writes=[bpr])
                        P.op("act", lambda e, p_r=p_r, a_l=a_l: e.activation(out=p_r[:, :, 1], in_=a_l[:], func=AF.Copy), reads=[bal], writes=[bpr])
                        for ex_ in range(NEXP):
                            P.dma("pool", lambda e, p_r=p_r, d_i=d_i, ex_=ex_: e.indirect_dma_start(
                                out=LIST[:, :], out_offset=bass.IndirectOffsetOnAxis(ap=d_i[:, ex_:ex_ + 1], axis=0),
                                in_=p_r[:, ex_, :], in_offset=None), reads=[bpr, bdi], writes=[])
                    P.barrier()
                Wg = sbuf(st, "WgF", [128, 8, 2048], BF16)
                Wu = sbuf(st, "WuF", [128, 8, 2048], BF16)
                Wd = sbuf(st, "WdF", [128, 16, 1024], BF16)
                bWg, bWu, bWd = Buf(), Buf(), Buf()
                wst = Rot([sbuf(st, "wstF%d" % i, [128, 2048], F32) for i in range(2)])
                li = sbuf(st, "liF", [128, 8, 2], F32)
                lii = sbuf(st, "liiF", [128, 8], I32)
                bli, blii = Buf(), Buf()
                xe = Rot([sbuf(st, "xeF%d" % i, [128, 1024], BF16) for i in range(2)])
                xeT = sbuf(st, "xeTF", [128, 8, 1024], BF16)
                bxeT = Buf()
                hid = sbuf(st, "hidF", [128, 16, 1024], BF16)
                bhid = Buf()
                sg = Rot([sbuf(st, "sgF%d" % i, [128, 512], F32) for i in range(2)])
                ys = Rot([sbuf(st, "ysF%d" % i, [128, 1024], F32) for i in range(2)])
                xr = Rot([sbuf(st, "xrF%d" % i, [128, 1024], F32) for i in range(2)])
                pT = Rot([psum(st, "pTF%d" % i, [128, 8, 128], BF16) for i in range(2)])
                psg = Rot([psum(st, "psgF%d" % i) for i in range(2)])
                psu = Rot([psum(st, "psuF%d" % i) for i in range(2)])
                psy = Rot([psum(st, "psyF%d" % i) for i in range(2)])
                cvt = [0]

                def convert(dst_ap, src_ap, rd, wr):
                    k = cvt[0] % 2
                    cvt[0] += 1
                    if k == 0:
                        P.op("act", lambda e: e.activation(out=dst_ap, in_=src_ap, func=AF.Copy), reads=rd, writes=wr)
                    else:
                        P.op("dve", lambda e: e.tensor_copy(out=dst_ap, in_=src_ap), reads=rd, writes=wr)

                for ex_ in range(NEXP):
                    for kc in range(8):
                        w, bw = wst.next()
                        P.dma("sp", lambda e, w=w, kc=kc, ex_=ex_: e.dma_start(out=w[:], in_=w_gate[l][ex_, kc * 128:(kc + 1) * 128, :]), writes=[bw])
                        convert(Wg[:, kc, :], w[:], [bw], [bWg])
                        w, bw = wst.next()
                        P.dma("sp", lambda e, w=w, kc=kc, ex_=ex_: e.dma_start(out=w[:], in_=w_up[l][ex_, kc * 128:(kc + 1) * 128, :]), writes=[bw])
                        convert(Wu[:, kc, :], w[:], [bw], [bWu])
                    for fc2 in range(8):
                        w, bw = wst.next()
                        P.dma("sp", lambda e, w=w, fc2=fc2, ex_=ex_: e.dma_start(
                            out=w[:].rearrange("p (a d) -> p a d", a=2), in_=w_down[l][ex_, fc2 * 256:(fc2 + 1) * 256, :].rearrange("(a p) d -> p a d", p=128)),
                            writes=[bw])
                        convert(Wd[:, fc2 * 2:fc2 * 2 + 2, :], w[:].rearrange("p (a d) -> p a d", a=2), [bw], [bWd])
                    P.dma("sp", lambda e, ex_=ex_: e.dma_start(out=li[:], in_=LIST[ex_ * ROWS:ex_ * ROWS + CAP, :].rearrange("(j p) c -> p j c", p=128)),
                          writes=[bli])
                    P.op("dve", lambda e: e.tensor_copy(out=lii[:], in_=li[:, :, 0]), reads=[bli], writes=[blii])
                    for j in range(8):
                        x_e, bxe = xe.next()
                        P.dma("pool", lambda e, x_e=x_e, j=j: e.indirect_dma_start(out=x_e[:], out_offset=None, in_=HF[:, :],
                                                                                    in_offset=bass.IndirectOffsetOnAxis(ap=lii[:, j:j + 1], axis=0)),
                              reads=[blii], writes=[bxe])
                        p_T, bpT = pT.next()
                        for kc in range(8):
                            P.op("pe", lambda e, p_T=p_T, x_e=x_e, kc=kc: e.transpose(out=p_T[:, kc, :], in_=x_e[:, kc * 128:(kc + 1) * 128], identity=ident_bf),
                                 reads=[bxe, b_const], writes=[bpT])
                        P.op("act" if j % 2 == 0 else "dve", (lambda e, p_T=p_T, j=j: e.activation(out=xeT[:, :, j * 128:(j + 1) * 128], in_=p_T[:], func=AF.Copy)) if j % 2 == 0
                             else (lambda e, p_T=p_T, j=j: e.tensor_copy(out=xeT[:, :, j * 128:(j + 1) * 128], in_=p_T[:])), reads=[bpT], writes=[bxeT])
                    for tb in range(2):
                        for fc in range(16):
                            pg, bpg = psg.next()
                            pu, bpu = psu.next()
                            for kc in range(8):
                                P.op("pe", lambda e, pg=pg, kc=kc, fc=fc, tb=tb: e.matmul(pg[:], lhsT=Wg[:, kc, fc * 128:(fc + 1) * 128], rhs=xeT[:, kc, tb * 512:(tb + 1) * 512],
                                                                                          start=(kc == 0), stop=(kc == 7)), reads=[bWg, bxeT], writes=[bpg])
                            for kc in range(8):
                                P.op("pe", lambda e, pu=pu, kc=kc, fc=fc, tb=tb: e.matmul(pu[:], lhsT=Wu[:, kc, fc * 128:(fc + 1) * 128], rhs=xeT[:, kc, tb * 512:(tb + 1) * 512],
                                                                                          start=(kc == 0), stop=(kc == 7)), reads=[bWu, bxeT], writes=[bpu])
                            s_g, bsg = sg.next()
                            P.op("act", lambda e, s_g=s_g, pg=pg: e.activation(out=s_g[:], in_=pg[:], func=AF.Silu), reads=[bpg], writes=[bsg])
                            P.op("dve", lambda e, s_g=s_g, pu=pu, fc=fc, tb=tb: e.tensor_tensor(out=hid[:, fc, tb * 512:(tb + 1) * 512], in0=pu[:], in1=s_g[:], op=ALU.mult),
                                 reads=[bpu, bsg], writes=[bhid])
                    for j in range(8):
                        y_s, bys = ys.next()
                        for half in range(2):
                            py, bpy = psy.next()
                            for fc in range(16):
                                P.op("pe", lambda e, py=py, fc=fc, j=j, half=half: e.matmul(py[:], lhsT=hid[:, fc, j * 128:(j + 1) * 128], rhs=Wd[:, fc, half * 512:(half + 1) * 512],
                                                                                            start=(fc == 0), stop=(fc == 15)), reads=[bhid, bWd], writes=[bpy])
                            P.op("act", lambda e, y_s=y_s, py=py, half=half, j=j: e.activation(out=y_s[:, half * 512:(half + 1) * 512], in_=py[:], func=AF.Copy,
                                                                                                 scale=li[:, j, 1:2]), reads=[bpy, bli], writes=[bys])
                        x_r, bxr = xr.next()
                        P.dma("pool", lambda e, x_r=x_r, j=j: e.indirect_dma_start(out=x_r[:], out_offset=None, in_=OUT[:, :],
                                                                                    in_offset=bass.IndirectOffsetOnAxis(ap=lii[:, j:j + 1], axis=0)),
                              reads=[blii, b_X], writes=[bxr])
                        P.op("dve", lambda e, x_r=x_r, y_s=y_s: e.tensor_tensor(out=x_r[:], in0=x_r[:], in1=y_s[:], op=ALU.add), reads=[bxr, bys], writes=[bxr])
                        P.dma("pool", lambda e, x_r=x_r, j=j: e.indirect_dma_start(out=OUT[:, :], out_offset=bass.IndirectOffsetOnAxis(ap=lii[:, j:j + 1], axis=0),
                                                                                    in_=x_r[:], in_offset=None), reads=[bxr, blii], writes=[b_X])
                P.barrier()

        PHASES = {}
        PHASES["A"] = phase_A
        PHASES["B"] = phase_B
        PHASES["C"] = phase_C
        PHASES["D"] = phase_D
        PHASES["E"] = phase_E
        PHASES["F"] = phase_F
        order = "ABCDEF"
        stop = False
        for l in range(depth):
            if l > 0:
                P.new_epoch()
            for ph in order:
                if ph in PHASES and (only is None or ph in only):
                    PHASES[ph](l)
                if done(l, ph):
                    stop = True
                    break
            if stop:
                break
        P.finish_wait_all("sp")
        P.emit()
    return nc


def _consts():
    c = {}
    bf = ml_dtypes.bfloat16
    cm = np.zeros((6, 128, 128), np.float32)
    i = np.arange(128)
    cm[0] = np.eye(128)
    cm[1] = (i[:, None] // 64 == i[None, :] // 64)
    cm[2] = 1.0
    cm[3] = (i[:, None] < i[None, :])
    cm[4] = np.eye(128)[::-1]
    c["cmat"] = cm.astype(bf)
    c["identf"] = np.eye(128, dtype=np.float32)
    slopes = np.array([2.0 ** (-8.0 * (h + 1) / 8) for h in range(8)], np.float64)
    db = np.zeros((3, 8, 128, 256), np.float64)
    k = np.arange(128)[:, None]
    q = np.arange(256)[None, :]
    rel = k - q + 64
    valid = np.abs(rel) <= 64
    for ci, dil in enumerate((1, 4, 16)):
        for h in range(8):
            db[ci, h] = np.where(valid, -slopes[h] * dil * np.abs(rel) * 8.0, NEGB)
    c["dbias"] = db.astype(np.float32).astype(bf)
    variants = _na_variants()
    nm = np.zeros((21, 128, 128), np.float32)
    kk = np.arange(128)
    krl, kc = kk // 64, kk % 64
    qrl, qc = kk // 64, kk % 64
    cs = np.clip(qc - 8, 0, 48)
    colv = (kc[:, None] >= cs[None, :]) & (kc[:, None] < cs[None, :] + 16)
    for vi, (a, jp) in enumerate(variants):
        krow = 2 * (a + jp) + krl
        qrow = 2 * a + qrl
        rs = np.clip(qrow - 4, 0, 120)
        rowv = (krow[:, None] >= rs[None, :]) & (krow[:, None] < rs[None, :] + 8)
        nm[vi] = np.where(colv & rowv, 0.0, NEGB)
    c["namask"] = nm.astype(bf)
    p = np.arange(128)
    c["maskB"] = (p[:, None] // 16 == np.arange(8)[None, :]).astype(np.float32)
    mc = np.zeros((128, 4, 8), np.float32)
    for pl in range(4):
        for g8 in range(8):
            mc[:, pl, g8] = (g8 == 2 * pl + p // 64)
    c["maskC"] = mc
    ec = np.zeros((128, 18), np.float32)
    ec[:, 0:16] = np.arange(16)[None, :] * ROWS + CAP + p[:, None]
    ec[:, 16] = CAP + p + 1
    ec[:, 17] = p
    c["ecst"] = ec
    return c


def _na_variants():
    v = [(10, jp) for jp in (-2, -1, 0, 1, 2)]
    v += [(0, jp) for jp in (0, 1, 2, 3)]
    v += [(1, jp) for jp in (-1, 0, 1, 2)]
    v += [(62, jp) for jp in (-2, -1, 0, 1)]
    v += [(63, jp) for jp in (-3, -2, -1, 0)]
    return v


def _na_block_tiles(a):
    if a == 0:
        return [(j, 5 + j, j) for j in range(4)]
    if a == 1:
        return [(a + jp, 9 + (jp + 1), jp) for jp in (-1, 0, 1, 2)]
    if a == 62:
        return [(a + jp, 13 + (jp + 2), jp) for jp in (-2, -1, 0, 1)]
    if a == 63:
        return [(a + jp, 17 + (jp + 3), jp) for jp in (-3, -2, -1, 0)]
    return [(a + jp, jp + 2, jp) for jp in (-2, -1, 0, 1, 2)]


def _prep_shared(inp):
    L = DEPTH
    f = lambda a: np.ascontiguousarray(np.asarray(a, dtype=np.float32))
    sh = {}
    for k in ("w_in", "w_out", "w_glu", "w_router", "w_gate", "w_up", "w_down"):
        sh[k] = f(inp[k])
    onorm = np.concatenate([inp["out_norm_a"], inp["out_norm_b"], inp["out_norm_c"]], axis=1)
    g3 = np.stack([inp["attn_norm"], onorm, inp["ffn_norm"]], axis=1)
    sh["gcol"] = f(g3.reshape(L, 3, 8, 128).transpose(3, 0, 1, 2))
    gq = np.stack([inp["q_norm_a"], inp["k_norm_a"], inp["q_norm_b"], inp["k_norm_b"]], axis=1)
    sh["gqk"] = f(np.concatenate([gq, gq], axis=2).transpose(2, 0, 1))
    sh["gffn"] = f(np.broadcast_to(np.asarray(inp["ffn_norm"])[:, None, :], (L, 128, D)))
    rp = np.asarray(inp["rel_pos_bias"], np.float32)
    kk = np.arange(128)
    krl, kc = kk // 64, kk % 64
    nabt = np.zeros((L, 7, 4, 128, 128), np.float32)
    dc = np.clip(kc[:, None] - kc[None, :] + 15, 0, 30)
    for jp in range(-3, 4):
        dr = np.clip(2 * jp + krl[:, None] - krl[None, :] + 7, 0, 14)
        nabt[:, jp + 3] = rp[:, :, dr, dc]
    sh["nab"] = nabt
    are = np.asarray(inp["s5_a_re"], np.float32)
    aim = np.asarray(inp["s5_a_im"], np.float32)
    ldt = np.broadcast_to(np.asarray(inp["s5_log_dt"], np.float32)[..., None], are.shape)
    p3 = np.stack([are, aim, ldt], axis=1)
    t = p3.reshape(L, 3, 2, 8, 2, 64)
    sh["s5lam"] = f(t.transpose(4, 5, 0, 1, 2, 3).reshape(128, L, 3, 16))
    bre = np.asarray(inp["s5_b_re"], np.float32)
    bim = np.asarray(inp["s5_b_im"], np.float32)
    rep = lambda a: np.broadcast_to(a[..., None], a.shape + (16,))
    q5 = np.stack([bre, bim, rep(are), rep(aim), rep(ldt)], axis=0)
    q5 = q5.reshape(5, L, 2, 2, 8, 64, 16)
    sh["s5bT"] = f(q5.transpose(4, 6, 1, 2, 3, 0, 5).reshape(128, L, 4, 5, 64))
    cre = np.asarray(inp["s5_c_re"], np.float32)
    cim = np.asarray(inp["s5_c_im"], np.float32)
    c2 = np.stack([cre, cim], axis=0).reshape(2, L, 2, 8, 2, 16, 64)
    sh["s5cT"] = f(c2.transpose(4, 6, 1, 0, 2, 3, 5).reshape(128, L, 2, 16, 16))
    sh["s5dd"] = f(np.asarray(inp["s5_d"]).reshape(L, 2, 128).transpose(2, 0, 1))
    sh.update(_consts())
    return sh


_NC_CACHE = {}
_AX0 = ("w_in", "w_out", "w_glu", "w_router", "gffn", "nab")
_SPLIT = ("w_gate", "w_up", "w_down")
_AX1 = ("gcol", "gqk", "s5lam", "s5bT", "s5cT", "s5dd")
LAYERS_PER_LAUNCH = 4


def kernel(**inputs):
    x = np.asarray(inputs["x"], dtype=np.float32)
    sh = _prep_shared(inputs)
    npl = LAYERS_PER_LAUNCH
    if "nc" not in _NC_CACHE:
        _NC_CACHE["nc"] = build_program(depth=npl, nl=npl)
    nc = _NC_CACHE["nc"]
    cur = [np.ascontiguousarray(x[c]) for c in range(8)]
    for l0 in range(0, DEPTH, npl):
        shl = {}
        for k, v in sh.items():
            if k in _SPLIT:
                for i in range(npl):
                    shl["%s%d" % (k, i)] = v[l0 + i]
            elif k in _AX0:
                shl[k] = np.ascontiguousarray(v[l0:l0 + npl])
            elif k in _AX1:
                shl[k] = np.ascontiguousarray(v[:, l0:l0 + npl])
            else:
                shl[k] = v
        in_maps = []
        for c in range(8):
            m = dict(shl)
            m["x"] = cur[c]
            in_maps.append(m)
        res = run_bass_kernel_spmd(nc, in_maps, core_ids=list(range(8)))
        cur = [np.ascontiguousarray(np.asarray(r["out"], dtype=np.float32)) for r in res.results]
    return np.stack(cur, axis=0)
```

```python
import math
import numpy as np
import ml_dtypes
from contextlib import ExitStack
import concourse.bass as bass
import concourse.mybir as mybir
from concourse.bass_utils import run_bass_kernel_spmd

F32 = mybir.dt.float32
BF16 = mybir.dt.bfloat16
I32 = mybir.dt.int32
AF = mybir.ActivationFunctionType
ALU = mybir.AluOpType
AX = mybir.AxisListType

S = 8192
D = 1024
DEPTH = 4
NT = S // 128
EPS = 1e-6
PADR = 1024
NEGB = -30000.0
NEXP = 16
CAP = 1024
ROWS = CAP + 128
SEG = 2048


class Buf:
    __slots__ = ("name", "lw", "rd")

    def __init__(self, name=""):
        self.name = name
        self.lw = None
        self.rd = {}


class Prog:
    ENGS = ("pe", "act", "dve", "pool", "sp")

    def __init__(self, nc, es, ndma_sems=16):
        self.nc = nc
        self.es = es
        self.q = {e: [] for e in self.ENGS}
        self.seq = {}
        self.sem = {}
        self.cur = {}
        self.epoch = 0
        for e in ("pe", "act", "dve", "pool"):
            self.cur[e] = e + "#0"
            self.sem[self.cur[e]] = es.enter_context(nc.semaphore("prog_" + e + "_0"))
            self.seq[self.cur[e]] = 0
        self.waited = {e: {} for e in self.ENGS}
        self.dpool = {}
        self.dval = {}
        self.drr = {}
        for qn in ("sp", "pool", "act"):
            self.dpool[qn] = [es.enter_context(nc.semaphore("dq_%s_%d" % (qn, i))) for i in range(ndma_sems)]
            for i in range(ndma_sems):
                self.dval[(qn, i)] = 0
            self.drr[qn] = 0
        self.ninstr = 0

    def new_epoch(self):
        self.epoch += 1
        for e in ("pe", "act", "dve", "pool"):
            k = "%s#%d" % (e, self.epoch)
            self.cur[e] = k
            self.sem[k] = self.es.enter_context(self.nc.semaphore("prog_%s_%d" % (e, self.epoch)))
            self.seq[k] = 0

    def _semobj(self, key):
        if isinstance(key, str):
            return self.sem[key]
        return self.dpool[key[0]][key[1]]

    def _wait(self, eng, key, val):
        if val <= 0:
            return
        w = self.waited[eng]
        if w.get(key, 0) >= val:
            return
        w[key] = val
        self.q[eng].append(("w", key, val))

    def _deps(self, eng, reads, writes):
        deps = {}
        for b in reads:
            if b.lw is not None and deps.get(b.lw[0], 0) < b.lw[1]:
                deps[b.lw[0]] = b.lw[1]
        for b in writes:
            if b.lw is not None and deps.get(b.lw[0], 0) < b.lw[1]:
                deps[b.lw[0]] = b.lw[1]
            for k, v in b.rd.items():
                if deps.get(k, 0) < v:
                    deps[k] = v
        for k, v in deps.items():
            if eng == "pe" and isinstance(k, str) and k.startswith("pe#"):
                continue
            self._wait(eng, k, v)

    def op(self, eng, fn, reads=(), writes=()):
        self._deps(eng, reads, writes)
        key = self.cur[eng]
        self.seq[key] += 1
        v = self.seq[key]
        self.q[eng].append(("i", fn, key, 1))
        for b in writes:
            b.lw = (key, v)
            b.rd = {}
        for b in reads:
            if b.rd.get(key, 0) < v:
                b.rd[key] = v
        self.ninstr += 1

    def dma(self, qn, fn, reads=(), writes=()):
        self._deps(qn, reads, writes)
        i = self.drr[qn]
        self.drr[qn] = (i + 1) % len(self.dpool[qn])
        key = (qn, i)
        self._wait(qn, key, self.dval[key])
        self.dval[key] += 16
        v = self.dval[key]
        self.q[qn].append(("i", fn, key, 16))
        for b in writes:
            b.lw = (key, v)
            b.rd = {}
        for b in reads:
            b.rd[key] = v
        self.ninstr += 1

    def barrier(self):
        for eng in self.ENGS:
            for e in ("pe", "act", "dve", "pool"):
                self._wait(eng, self.cur[e], self.seq[self.cur[e]])
            for key, v in self.dval.items():
                self._wait(eng, key, v)

    def finish_wait_all(self, eng="sp"):
        for e in ("pe", "act", "dve", "pool"):
            self._wait(eng, self.cur[e], self.seq[self.cur[e]])
        for key, v in self.dval.items():
            self._wait(eng, key, v)

    def emit(self):
        nc = self.nc
        engmap = {"pe": "tensor", "act": "scalar", "dve": "vector", "pool": "gpsimd", "sp": "sync"}
        with nc.Block() as block:
            for e in self.ENGS:
                items = self.q[e]

                def body(engobj, items=items):
                    for it in items:
                        if it[0] == "w":
                            engobj.wait_ge(self._semobj(it[1]), it[2])
                        else:
                            it[1](engobj).then_inc(self._semobj(it[2]), it[3])
                getattr(block, engmap[e])(body)
        self.q = {e: [] for e in self.ENGS}


class Rot:
    def __init__(self, tiles):
        self.t = [(t, Buf()) for t in tiles]
        self.i = 0

    def next(self):
        r = self.t[self.i]
        self.i = (self.i + 1) % len(self.t)
        return r


def build_program(depth=DEPTH, stop_after=None, dbg=(), only=None, feed=(), nl=DEPTH):
    nc = bass.Bass("TRN2", target_bir_lowering=False)

    def din(name, shape, dt=F32):
        return nc.dram_tensor(name, list(shape), dt, kind="ExternalInput").ap()

    def dscr(name, shape, dt):
        kind = "ExternalOutput" if name in dbg else ("ExternalInput" if name in feed else "Internal")
        return nc.dram_tensor(name, list(shape), dt, kind=kind).ap()

    L = nl
    x_in = din("x", [S, D])
    OUT = nc.dram_tensor("out", [S, D], F32, kind="ExternalOutput").ap()
    w_in = din("w_in", [L, D, 2560])
    w_out = din("w_out", [L, D, D])
    w_glu = din("w_glu", [L, 256, 256])
    w_router = din("w_router", [L, D, NEXP])
    if only is None or "F" in only:
        w_gate = [din("w_gate%d" % i, [NEXP, D, 2048]) for i in range(L)]
        w_up = [din("w_up%d" % i, [NEXP, D, 2048]) for i in range(L)]
        w_down = [din("w_down%d" % i, [NEXP, 2048, D]) for i in range(L)]
    gcol_d = din("gcol", [128, L, 3, 8])
    gqk_d = din("gqk", [128, L, 4])
    gffn_d = din("gffn", [L, 128, D])
    nab_d = din("nab", [L, 7, 4, 128, 128])
    dbias_d = din("dbias", [3, 8, 128, 256], BF16)
    namask_d = din("namask", [21, 128, 128], BF16)
    cmat_d = din("cmat", [6, 128, 128], BF16)
    identf_d = din("identf", [128, 128])
    s5lam_d = din("s5lam", [128, L, 3, 16])
    s5bT_d = din("s5bT", [128, L, 4, 5, 64])
    s5cT_d = din("s5cT", [128, L, 2, 16, 16])
    s5d_d = din("s5dd", [128, L, 2])
    maskB_d = din("maskB", [128, 8])
    maskC_d = din("maskC", [128, 4, 8])
    ecst_d = din("ecst", [128, 18])

    QTa = dscr("QTa", [512, S], BF16)
    KTa = dscr("KTa", [512, S], BF16)
    Va = dscr("Va", [S + 2 * PADR, 8 * 128], BF16)
    QTb = dscr("QTb", [256, S], BF16)
    KTb = dscr("KTb", [256, S], BF16)
    Vb = dscr("Vb", [S, 4 * 128], BF16)
    UT = dscr("UT", [256, S], BF16)
    ACCA = dscr("ACCA", [3, S, 8 * 65], F32)
    ACCB = dscr("ACCB", [S, 4 * 65], F32)
    MIXC = dscr("MIXC", [256, S], BF16)
    HF = dscr("HF", [S, D], BF16)
    LIST = dscr("LIST", [NEXP * ROWS, 2], F32)
    AFFD = dscr("AFFD", [S, NEXP], F32)
    AFFTD = dscr("AFFTD", [NEXP, S], F32)

    es = ExitStack()
    with es:
        P = Prog(nc, es)

        uid = [0]

        def sbuf(stack, name, shape, dt):
            uid[0] += 1
            return stack.enter_context(nc.sbuf_tensor("s%d_%s" % (uid[0], name), list(shape), dt))

        def psum(stack, name, shape=(128, 512), dt=F32):
            uid[0] += 1
            return stack.enter_context(nc.psum_tensor("p%d_%s" % (uid[0], name), list(shape), dt))

        cmat = sbuf(es, "cmat", [128, 6, 128], BF16)
        identf = sbuf(es, "identf", [128, 128], F32)
        gcol = sbuf(es, "gcol", [128, L, 3, 8], F32)
        gqk = sbuf(es, "gqk", [128, L, 4], F32)
        epsT = sbuf(es, "epsT", [128, 1], F32)
        b_const = Buf("const")
        P.dma("sp", lambda e: e.dma_start(out=cmat[:], in_=cmat_d.rearrange("k p f -> p k f")), writes=[b_const])
        P.dma("sp", lambda e: e.dma_start(out=identf[:], in_=identf_d[:, :]), writes=[b_const])
        P.dma("sp", lambda e: e.dma_start(out=gcol[:], in_=gcol_d[:, :, :, :]), writes=[b_const])
        P.dma("sp", lambda e: e.dma_start(out=gqk[:], in_=gqk_d[:, :, :]), writes=[b_const])
        P.op("pool", lambda e: e.memset(epsT[:], EPS), writes=[b_const])
        ident_bf = cmat[:, 0, :]
        blockones = cmat[:, 1, :]
        ones_bf = cmat[:, 2, :]
        b_X = Buf("X")
        b_QK = Buf("QK")
        b_ACCA = Buf("ACCA")
        b_ACCB = Buf("ACCB")
        b_MIXC = Buf("MIXC")
        b_HF = Buf("HF")
        b_LIST = Buf("LIST")
        b_AFF = Buf("AFF")

        with ExitStack() as s0:
            z = sbuf(s0, "zpad", [128, 8, 1024], BF16)
            bz = Buf()
            P.op("pool", lambda e: e.memset(z[:], 0.0), writes=[bz])
            for r0 in (0, PADR + S):
                P.dma("sp", lambda e, r0=r0: e.dma_start(out=Va[r0:r0 + PADR, :].rearrange("(p a) c -> p a c", a=8), in_=z[:]),
                      reads=[bz], writes=[])
            P.barrier()

        def done(l, ph):
            return stop_after is not None and (l, ph) == tuple(stop_after)

        def phase_A(l):
            xsrc = x_in if l == 0 else OUT
            with ExitStack() as st:
                Win = sbuf(st, "Win", [128, 8, 2560], BF16)
                wst = Rot([sbuf(st, "wst%d" % i, [128, 2560], F32) for i in range(2)])
                xt = Rot([sbuf(st, "xt%d" % i, [128, 4, 1024], F32) for i in range(2)])
                hn = Rot([sbuf(st, "hn%d" % i, [128, 1024], BF16) for i in range(2)])
                hT = Rot([sbuf(st, "hT%d" % i, [128, 8, 512], BF16) for i in range(2)])
                junk = sbuf(st, "junkA", [128, 1024], BF16)
                ss = Rot([sbuf(st, "ssA%d" % i, [128, 4], F32) for i in range(2)])
                sq = Rot([sbuf(st, "sqA%d" % i, [128, 512], BF16) for i in range(2)])
                rr = Rot([sbuf(st, "rrA%d" % i, [128, 512], F32) for i in range(2)])
                qn = Rot([sbuf(st, "qnA%d" % i, [128, 512], BF16) for i in range(3)])
                vst = Rot([sbuf(st, "vstA%d" % i, [128, 8, 128], BF16) for i in range(2)])
                vbst = Rot([sbuf(st, "vbstA%d" % i, [128, 4, 128], BF16) for i in range(2)])
                pT = Rot([psum(st, "pTA%d" % i, [128, 8, 128], BF16) for i in range(2)])
                psA = Rot([psum(st, "psA%d" % i) for i in range(2)])
                ps2 = Rot([psum(st, "ps2A%d" % i) for i in range(1)])
                psv = Rot([psum(st, "psvA%d" % i) for i in range(2)])
                bWin = Buf()
                bjunk = Buf()
                for (t, b) in vst.t + vbst.t:
                    P.op("pool", lambda e, t=t: e.memset(t[:, :, 64:128], 1.0), writes=[b])
                for kc in range(8):
                    w, bw = wst.next()
                    P.dma("sp", lambda e, w=w, kc=kc: e.dma_start(out=w[:], in_=w_in[l, kc * 128:(kc + 1) * 128, :]), writes=[bw])
                    if kc % 2 == 0:
                        P.op("dve", lambda e, w=w, kc=kc: e.tensor_scalar(out=Win[:, kc, :], in0=w[:], scalar1=gcol[:, l, 0, kc:kc + 1],
                                                                           scalar2=None, op0=ALU.mult), reads=[bw, b_const], writes=[bWin])
                    else:
                        P.op("act", lambda e, w=w, kc=kc: e.activation(out=Win[:, kc, :], in_=w[:], func=AF.Copy,
                                                                        scale=gcol[:, l, 0, kc:kc + 1]), reads=[bw, b_const], writes=[bWin])
                fm_tiles = [(f0, QTa, f0, 0) for f0 in range(0, 512, 128)] + \
                           [(512 + f0, KTa, f0, 1) for f0 in range(0, 512, 128)] + \
                           [(1536 + f0, QTb, f0, 2) for f0 in range(0, 256, 128)] + \
                           [(1792 + f0, KTb, f0, 3) for f0 in range(0, 256, 128)] + \
                           [(2304 + f0, UT, f0, None) for f0 in range(0, 256, 128)]
                for blk in range(S // 512):
                    t0 = blk * 512
                    x_t, bx = xt.next()
                    P.dma("sp", lambda e, x_t=x_t, t0=t0: e.dma_start(out=x_t[:], in_=xsrc[t0:t0 + 512, :].rearrange("(j p) d -> p j d", p=128)),
                          reads=[b_X], writes=[bx])
                    s_t, bs = ss.next()
                    for j in range(4):
                        P.op("act", lambda e, x_t=x_t, s_t=s_t, j=j: e.activation(out=junk[:], in_=x_t[:, j, :], func=AF.Square,
                                                                                   accum_out=s_t[:, j:j + 1]), reads=[bx], writes=[bjunk, bs])
                    P.op("act", lambda e, s_t=s_t: e.activation(out=s_t[:], in_=s_t[:], func=AF.Sqrt, scale=1.0 / D, bias=epsT[:, 0:1]),
                         reads=[bs, b_const], writes=[bs])
                    P.op("dve", lambda e, s_t=s_t: e.reciprocal(out=s_t[:], in_=s_t[:]), reads=[bs], writes=[bs])
                    h_T, bhT = hT.next()
                    for j in range(4):
                        h_n, bhn = hn.next()
                        P.op("dve", lambda e, h_n=h_n, x_t=x_t, s_t=s_t, j=j: e.tensor_scalar(out=h_n[:], in0=x_t[:, j, :], scalar1=s_t[:, j:j + 1],
                                                                                               scalar2=None, op0=ALU.mult), reads=[bx, bs], writes=[bhn])
                        p_T, bpT = pT.next()
                        for kc in range(8):
                            P.op("pe", lambda e, p_T=p_T, h_n=h_n, kc=kc: e.transpose(out=p_T[:, kc, :], in_=h_n[:, kc * 128:(kc + 1) * 128],
                                                                                      identity=ident_bf), reads=[bhn, b_const], writes=[bpT])
                        P.op("act", lambda e, p_T=p_T, h_T=h_T, j=j: e.activation(out=h_T[:, :, j * 128:(j + 1) * 128], in_=p_T[:], func=AF.Copy),
                             reads=[bpT], writes=[bhT])
                    for (f0, dst, r0, gi) in fm_tiles:
                        ps, bps = psA.next()
                        for kc in range(8):
                            P.op("pe", lambda e, ps=ps, h_T=h_T, kc=kc, f0=f0: e.matmul(ps[:], lhsT=Win[:, kc, f0:f0 + 128], rhs=h_T[:, kc, :],
                                                                                        start=(kc == 0), stop=(kc == 7)), reads=[bWin, bhT], writes=[bps])
                        q_n, bqn = qn.next()
                        if gi is None:
                            P.op("act", lambda e, q_n=q_n, ps=ps: e.activation(out=q_n[:], in_=ps[:], func=AF.Copy), reads=[bps], writes=[bqn])
                        else:
                            s_q, bsq = sq.next()
                            P.op("act", lambda e, s_q=s_q, ps=ps: e.activation(out=s_q[:], in_=ps[:], func=AF.Square), reads=[bps], writes=[bsq])
                            p2, bp2 = ps2.next()
                            P.op("pe", lambda e, p2=p2, s_q=s_q: e.matmul(p2[:], lhsT=blockones, rhs=s_q[:], start=True, stop=True),
                                 reads=[bsq, b_const], writes=[bp2])
                            r_t, brt = rr.next()
                            P.op("act", lambda e, r_t=r_t, p2=p2: e.activation(out=r_t[:], in_=p2[:], func=AF.Sqrt, scale=1.0 / 64, bias=epsT[:, 0:1]),
                                 reads=[bp2, b_const], writes=[brt])
                            P.op("dve", lambda e, r_t=r_t: e.reciprocal(out=r_t[:], in_=r_t[:]), reads=[brt], writes=[brt])
                            P.op("dve", lambda e, q_n=q_n, ps=ps, r_t=r_t, gi=gi: e.scalar_tensor_tensor(out=q_n[:], in0=ps[:], scalar=gqk[:, l, gi:gi + 1],
                                                                                                         in1=r_t[:], op0=ALU.mult, op1=ALU.mult),
                                 reads=[bps, brt, b_const], writes=[bqn])
                        P.dma("pool", lambda e, q_n=q_n, dst=dst, r0=r0, t0=t0: e.dma_start(out=dst[r0:r0 + 128, t0:t0 + 512], in_=q_n[:]),
                              reads=[bqn], writes=[])
                    for j in range(4):
                        tk = t0 + j * 128
                        pv, bpv = psv.next()
                        for kc in range(8):
                            P.op("pe", lambda e, pv=pv, h_T=h_T, kc=kc, j=j: e.matmul(pv[:], lhsT=h_T[:, kc, j * 128:(j + 1) * 128], rhs=Win[:, kc, 1024:1536],
                                                                                      start=(kc == 0), stop=(kc == 7)), reads=[bWin, bhT], writes=[bpv])
                        v_s, bvs = vst.next()
                        P.op("act", lambda e, v_s=v_s, pv=pv: e.activation(out=v_s[:, :, 0:64], in_=pv[:].rearrange("p (h d) -> p h d", d=64), func=AF.Copy),
                             reads=[bpv], writes=[bvs])
                        P.dma("pool", lambda e, v_s=v_s, tk=tk: e.dma_start(out=Va[PADR + tk:PADR + tk + 128, :], in_=v_s[:].rearrange("p h d -> p (h d)")),
                              reads=[bvs], writes=[])
                        pw, bpw = psv.next()
                        for kc in range(8):
                            P.op("pe", lambda e, pw=pw, h_T=h_T, kc=kc, j=j: e.matmul(pw[:, 0:256], lhsT=h_T[:, kc, j * 128:(j + 1) * 128], rhs=Win[:, kc, 2048:2304],
                                                                                      start=(kc == 0), stop=(kc == 7)), reads=[bWin, bhT], writes=[bpw])
                        vb_s, bvbs = vbst.next()
                        P.op("dve", lambda e, vb_s=vb_s, pw=pw: e.tensor_copy(out=vb_s[:, :, 0:64], in_=pw[:, 0:256].rearrange("p (h d) -> p h d", d=64)),
                             reads=[bpw], writes=[bvbs])
                        P.dma("pool", lambda e, vb_s=vb_s, tk=tk: e.dma_start(out=Vb[tk:tk + 128, :], in_=vb_s[:].rearrange("p h d -> p (h d)")),
                              reads=[bvbs], writes=[])
                P.barrier()

        def phase_B(l):
            with ExitStack() as st:
                dbias = sbuf(st, "dbias", [128, 24, 256], BF16)
                bdb = Buf()
                P.dma("sp", lambda e: e.dma_start(out=dbias[:], in_=dbias_d.rearrange("c h p f -> p (c h) f")), writes=[bdb])
                QT = sbuf(st, "QTB", [128, S], BF16)
                KT = sbuf(st, "KTB", [128, S + 2 * PADR], BF16)
                bQT, bKT = Buf(), Buf()
                P.op("pool", lambda e: e.memset(KT[:, 0:PADR], 0.0), writes=[bKT])
                P.op("pool", lambda e: e.memset(KT[:, PADR + S:], 0.0), writes=[bKT])
                vt = Rot([sbuf(st, "vtB%d" % i, [128, 256], BF16) for i in range(4)])
                pt = Rot([sbuf(st, "ptB%d" % i, [128, 256], BF16) for i in range(4)])
                ost = Rot([sbuf(st, "ostB%d" % i, [128, 65], F32) for i in range(4)])
                psS = Rot([psum(st, "psSB%d" % i) for i in range(3)])
                pso = [[(psum(st, "psoB%d_%d" % (hh, par)), Buf()) for par in range(2)] for hh in range(2)]
                for hp in range(4):
                    P.dma("sp", lambda e, hp=hp: e.dma_start(out=QT[:], in_=QTa[hp * 128:(hp + 1) * 128, :]), writes=[bQT])
                    P.dma("sp", lambda e, hp=hp: e.dma_start(out=KT[:, PADR:PADR + S], in_=KTa[hp * 128:(hp + 1) * 128, :]), writes=[bKT])
                    for c, dil in enumerate((1, 4, 16)):
                        nq = S // dil // 128
                        for r in range(dil):
                            for m in range(nq + 1):
                                base = r + dil * (128 * m - 64)
                                v_t, bvt = vt.next()
                                P.dma("sp", lambda e, v_t=v_t, base=base, dil=dil, hp=hp: e.dma_start(
                                    out=v_t[:], in_=Va[PADR + base:PADR + base + 127 * dil + 1:dil, hp * 256:(hp + 1) * 256]),
                                    writes=[bvt])
                                clo = 128 if m == 0 else 0
                                chi = 128 if m == nq else 256
                                q0 = r + dil * (128 * (m - 1) + clo)
                                ncol = chi - clo
                                for hh in range(2):
                                    h = 2 * hp + hh
                                    pb = 64 * hh
                                    ps, bps = psS.next()
                                    P.op("pe", lambda e, ps=ps, pb=pb, base=base, dil=dil, q0=q0, ncol=ncol, clo=clo, chi=chi: e.matmul(
                                        ps[:, clo:chi], lhsT=KT[pb:pb + 64, PADR + base:PADR + base + 127 * dil + 1:dil],
                                        rhs=QT[pb:pb + 64, q0:q0 + (ncol - 1) * dil + 1:dil], start=True, stop=False),
                                        reads=[bKT, bQT], writes=[bps])
                                    P.op("pe", lambda e, ps=ps, c=c, h=h, clo=clo, chi=chi: e.matmul(
                                        ps[:, clo:chi], lhsT=ident_bf, rhs=dbias[:, c * 8 + h, clo:chi], start=False, stop=True),
                                        reads=[bdb, b_const], writes=[bps])
                                    p_t, bpt = pt.next()
                                    P.op("act", lambda e, p_t=p_t, ps=ps, clo=clo, chi=chi: e.activation(out=p_t[:, clo:chi], in_=ps[:, clo:chi],
                                                                                                        func=AF.Exp, scale=0.125), reads=[bps], writes=[bpt])
                                    for j in (m - 1, m):
                                        if j < 0 or j >= nq:
                                            continue
                                        co = (j - (m - 1)) * 128
                                        po, bpo = pso[hh][j % 2]
                                        first = (j == m)
                                        P.op("pe", lambda e, po=po, p_t=p_t, v_t=v_t, co=co, hh=hh, first=first: e.matmul(
                                            po[:, 0:128], lhsT=p_t[:, co:co + 128], rhs=v_t[:, hh * 128:(hh + 1) * 128], start=first, stop=(not first)),
                                            reads=[bpt, bvt], writes=[bpo])
                                        if not first:
                                            o_s, bos = ost.next()
                                            P.op("dve", lambda e, o_s=o_s, po=po: e.tensor_copy(out=o_s[:], in_=po[:, 0:65]), reads=[bpo], writes=[bos])
                                            tok0 = r + dil * 128 * j
                                            P.dma("pool", lambda e, o_s=o_s, c=c, tok0=tok0, dil=dil, h=h: e.dma_start(
                                                out=ACCA[c, tok0:tok0 + 127 * dil + 1:dil, h * 65:(h + 1) * 65], in_=o_s[:]),
                                                reads=[bos], writes=[])
                P.barrier()

        def phase_C(l):
            with ExitStack() as st:
                namask = sbuf(st, "namask", [128, 21, 128], BF16)
                nab = sbuf(st, "nab", [128, 28, 128], BF16)
                nst = Rot([sbuf(st, "nabst%d" % i, [128, 4, 128], F32) for i in range(2)])
                bnm, bnab = Buf(), Buf()
                P.dma("sp", lambda e: e.dma_start(out=namask[:], in_=namask_d.rearrange("v p f -> p v f")), writes=[bnm])
                for jj in range(7):
                    n_s, bns = nst.next()
                    P.dma("sp", lambda e, n_s=n_s, jj=jj: e.dma_start(out=n_s[:], in_=nab_d[l, jj].rearrange("h p f -> p h f")), writes=[bns])
                    P.op("act", lambda e, n_s=n_s, jj=jj: e.activation(out=nab[:, jj * 4:(jj + 1) * 4, :], in_=n_s[:], func=AF.Copy, scale=8.0),
                         reads=[bns], writes=[bnab])
                QT = sbuf(st, "QTC", [128, S], BF16)
                KT = sbuf(st, "KTC", [128, S], BF16)
                bQT, bKT = Buf(), Buf()
                vt = Rot([sbuf(st, "vtC%d" % i, [128, 256], BF16) for i in range(4)])
                pt = Rot([sbuf(st, "ptC%d" % i, [128, 128], BF16) for i in range(4)])
                ost = Rot([sbuf(st, "ostC%d" % i, [128, 65], F32) for i in range(4)])
                psS = Rot([psum(st, "psSC%d" % i) for i in range(3)])
                pso = [Rot([psum(st, "psoC%d_%d" % (hh, i)) for i in range(2)]) for hh in range(2)]
                for hp in range(2):
                    P.dma("sp", lambda e, hp=hp: e.dma_start(out=QT[:], in_=QTb[hp * 128:(hp + 1) * 128, :]), writes=[bQT])
                    P.dma("sp", lambda e, hp=hp: e.dma_start(out=KT[:], in_=KTb[hp * 128:(hp + 1) * 128, :]), writes=[bKT])
                    for a in range(64):
                        tiles = _na_block_tiles(a)
                        pos = [pso[hh].next() for hh in range(2)]
                        for ti, (kt, vi, jp) in enumerate(tiles):
                            v_t, bvt = vt.next()
                            P.dma("sp", lambda e, v_t=v_t, kt=kt, hp=hp: e.dma_start(out=v_t[:], in_=Vb[kt * 128:(kt + 1) * 128, hp * 256:(hp + 1) * 256]),
                                  writes=[bvt])
                            for hh in range(2):
                                h = 2 * hp + hh
                                pb = 64 * hh
                                ps, bps = psS.next()
                                P.op("pe", lambda e, ps=ps, pb=pb, kt=kt, a=a: e.matmul(ps[:, 0:128], lhsT=KT[pb:pb + 64, kt * 128:(kt + 1) * 128],
                                                                                         rhs=QT[pb:pb + 64, a * 128:(a + 1) * 128], start=True, stop=False),
                                     reads=[bKT, bQT], writes=[bps])
                                P.op("pe", lambda e, ps=ps, jp=jp, h=h: e.matmul(ps[:, 0:128], lhsT=ident_bf, rhs=nab[:, (jp + 3) * 4 + h, :], start=False, stop=False),
                                     reads=[bnab, b_const], writes=[bps])
                                P.op("pe", lambda e, ps=ps, vi=vi: e.matmul(ps[:, 0:128], lhsT=ident_bf, rhs=namask[:, vi, :], start=False, stop=True),
                                     reads=[bnm, b_const], writes=[bps])
                                p_t, bpt = pt.next()
                                P.op("act", lambda e, p_t=p_t, ps=ps: e.activation(out=p_t[:], in_=ps[:, 0:128], func=AF.Exp, scale=0.125),
                                     reads=[bps], writes=[bpt])
                                po, bpo = pos[hh]
                                P.op("pe", lambda e, po=po, p_t=p_t, v_t=v_t, hh=hh, ti=ti, nt=len(tiles): e.matmul(
                                    po[:, 0:128], lhsT=p_t[:], rhs=v_t[:, hh * 128:(hh + 1) * 128], start=(ti == 0), stop=(ti == nt - 1)),
                                    reads=[bpt, bvt], writes=[bpo])
                        for hh in range(2):
                            h = 2 * hp + hh
                            po, bpo = pos[hh]
                            o_s, bos = ost.next()
                            P.op("dve", lambda e, o_s=o_s, po=po: e.tensor_copy(out=o_s[:], in_=po[:, 0:65]), reads=[bpo], writes=[bos])
                            P.dma("pool", lambda e, o_s=o_s, a=a, h=h: e.dma_start(out=ACCB[a * 128:(a + 1) * 128, h * 65:(h + 1) * 65], in_=o_s[:]),
                                  reads=[bos], writes=[])
                P.barrier()

        def phase_D(l):
            PI = math.pi
            with ExitStack() as st:
                lamraw = sbuf(st, "lamraw", [128, 3, 16], F32)
                bTraw = sbuf(st, "bTraw", [128, 4, 5, 64], F32)
                cT = sbuf(st, "cTD", [128, 2, 16, 16], F32)
                maskB = sbuf(st, "maskB", [128, 8], F32)
                maskC = sbuf(st, "maskC", [128, 2, 4, 8], F32)
                dcol = sbuf(st, "dcolD", [128, 2], F32)
                wgst = sbuf(st, "wgst", [128, 2, 256], F32)
                Wglu = sbuf(st, "WgluD", [128, 2, 256], BF16)
                bprm = Buf()
                P.dma("sp", lambda e: e.dma_start(out=lamraw[:], in_=s5lam_d[:, l]), writes=[bprm])
                P.dma("sp", lambda e: e.dma_start(out=bTraw[:], in_=s5bT_d[:, l]), writes=[bprm])
                P.dma("sp", lambda e: e.dma_start(out=cT[:], in_=s5cT_d[:, l]), writes=[bprm])
                P.dma("sp", lambda e: e.dma_start(out=maskB[:], in_=maskB_d[:, :]), writes=[bprm])
                P.dma("sp", lambda e: e.dma_start(out=maskC[:, 0], in_=maskC_d[:, :, :]), writes=[bprm])
                P.dma("sp", lambda e: e.dma_start(out=dcol[:], in_=s5d_d[:, l, :]), writes=[bprm])
                P.dma("sp", lambda e: e.dma_start(out=wgst[:], in_=w_glu[l].rearrange("(k p) j -> p k j", p=128)), writes=[bprm])
                P.op("dve", lambda e: e.tensor_copy(out=Wglu[:], in_=wgst[:]), reads=[bprm], writes=[bprm])
                P.op("dve", lambda e: e.tensor_scalar(out=maskC[:, 1], in0=maskC[:, 0], scalar1=-1.0, scalar2=None, op0=ALU.mult), reads=[bprm], writes=[bprm])

                def disc(A, W, LD, F, full, tag):
                    t = {}
                    for nm in ("dt", "a", "mag", "ang", "y", "sn", "cs", "abr", "abi", "t1", "t2", "zr", "gr", "gi"):
                        t[nm] = sbuf(st, tag + nm, [128, F], F32)
                    b = bprm
                    P.op("act", lambda e: e.activation(out=t["dt"][:], in_=LD, func=AF.Exp), reads=[b], writes=[b])
                    P.op("dve", lambda e: e.tensor_scalar(out=t["a"][:], in0=A, scalar1=-1e-4, scalar2=None, op0=ALU.min), reads=[b], writes=[b])
                    P.op("dve", lambda e: e.tensor_tensor(out=t["mag"][:], in0=t["a"][:], in1=t["dt"][:], op=ALU.mult), reads=[b], writes=[b])
                    P.op("act", lambda e: e.activation(out=t["mag"][:], in_=t["mag"][:], func=AF.Exp), reads=[b], writes=[b])
                    P.op("dve", lambda e: e.tensor_tensor(out=t["ang"][:], in0=W, in1=t["dt"][:], op=ALU.mult), reads=[b], writes=[b])
                    ki = sbuf(st, tag + "ki", [128, F], I32)
                    for (dst, sh) in (("sn", 0.0), ("cs", 0.5 * PI)):
                        P.op("dve", lambda e, sh=sh: e.tensor_scalar(out=t["y"][:], in0=t["ang"][:], scalar1=sh, scalar2=None, op0=ALU.add), reads=[b], writes=[b])
                        P.op("dve", lambda e: e.tensor_scalar(out=t["t1"][:], in0=t["y"][:], scalar1=1.0 / (2.0 * PI), scalar2=None, op0=ALU.mult), reads=[b], writes=[b])
                        P.op("dve", lambda e: e.tensor_copy(out=ki[:], in_=t["t1"][:]), reads=[b], writes=[b])
                        P.op("dve", lambda e: e.tensor_copy(out=t["t1"][:], in_=ki[:]), reads=[b], writes=[b])
                        P.op("dve", lambda e: e.tensor_scalar(out=t["t1"][:], in0=t["t1"][:], scalar1=-2.0 * PI, scalar2=None, op0=ALU.mult), reads=[b], writes=[b])
                        P.op("dve", lambda e: e.tensor_tensor(out=t["y"][:], in0=t["y"][:], in1=t["t1"][:], op=ALU.add), reads=[b], writes=[b])
                        P.op("dve", lambda e: e.tensor_scalar(out=t["y"][:], in0=t["y"][:], scalar1=-PI, scalar2=PI, op0=ALU.max, op1=ALU.min), reads=[b], writes=[b])
                        P.op("act", lambda e, dst=dst: e.activation(out=t[dst][:], in_=t["y"][:], func=AF.Sin), reads=[b], writes=[b])
                    P.op("dve", lambda e: e.tensor_tensor(out=t["abr"][:], in0=t["mag"][:], in1=t["cs"][:], op=ALU.mult), reads=[b], writes=[b])
                    P.op("dve", lambda e: e.tensor_tensor(out=t["abi"][:], in0=t["mag"][:], in1=t["sn"][:], op=ALU.mult), reads=[b], writes=[b])
                    if full:
                        tt_ = lambda o, i0, i1, op: P.op("dve", lambda e: e.tensor_tensor(out=o, in0=i0, in1=i1, op=op), reads=[b], writes=[b])
                        tt_(t["t1"][:], t["a"][:], t["a"][:], ALU.mult)
                        tt_(t["t2"][:], W, W, ALU.mult)
                        tt_(t["t1"][:], t["t1"][:], t["t2"][:], ALU.add)
                        P.op("dve", lambda e: e.reciprocal(out=t["t1"][:], in_=t["t1"][:]), reads=[b], writes=[b])
                        P.op("dve", lambda e: e.tensor_scalar(out=t["zr"][:], in0=t["abr"][:], scalar1=-1.0, scalar2=None, op0=ALU.add), reads=[b], writes=[b])
                        tt_(t["gr"][:], t["zr"][:], t["a"][:], ALU.mult)
                        tt_(t["t2"][:], t["abi"][:], W, ALU.mult)
                        tt_(t["gr"][:], t["gr"][:], t["t2"][:], ALU.add)
                        tt_(t["gr"][:], t["gr"][:], t["t1"][:], ALU.mult)
                        tt_(t["gi"][:], t["abi"][:], t["a"][:], ALU.mult)
                        tt_(t["t2"][:], t["zr"][:], W, ALU.mult)
                        tt_(t["gi"][:], t["gi"][:], t["t2"][:], ALU.subtract)
                        tt_(t["gi"][:], t["gi"][:], t["t1"][:], ALU.mult)
                    return t

                NK = 11
                tl = disc(lamraw[:, 0, :], lamraw[:, 1, :], lamraw[:, 2, :], 16, False, "dl_")
                lamP = sbuf(st, "lamP", [128, 16, NK, 3], F32)
                tq = sbuf(st, "tqD", [128, 2, 16], F32)
                b = bprm
                P.op("dve", lambda e: e.tensor_copy(out=lamP[:, :, 0, 0], in_=tl["abr"][:]), reads=[b], writes=[b])
                P.op("dve", lambda e: e.tensor_copy(out=lamP[:, :, 0, 1], in_=tl["abi"][:]), reads=[b], writes=[b])
                for k in range(NK):
                    if k > 0:
                        P.op("dve", lambda e, k=k: e.tensor_tensor(out=tq[:, 0, :], in0=lamP[:, :, k - 1, 0], in1=lamP[:, :, k - 1, 0], op=ALU.mult), reads=[b], writes=[b])
                        P.op("dve", lambda e, k=k: e.tensor_tensor(out=tq[:, 1, :], in0=lamP[:, :, k - 1, 1], in1=lamP[:, :, k - 1, 1], op=ALU.mult), reads=[b], writes=[b])
                        P.op("dve", lambda e, k=k: e.tensor_tensor(out=lamP[:, :, k, 0], in0=tq[:, 0, :], in1=tq[:, 1, :], op=ALU.subtract), reads=[b], writes=[b])
                        P.op("dve", lambda e, k=k: e.scalar_tensor_tensor(out=lamP[:, :, k, 1], in0=lamP[:, :, k - 1, 0], scalar=2.0, in1=lamP[:, :, k - 1, 1],
                                                                          op0=ALU.mult, op1=ALU.mult), reads=[b], writes=[b])
                    P.op("dve", lambda e, k=k: e.tensor_scalar(out=lamP[:, :, k, 2], in0=lamP[:, :, k, 1], scalar1=-1.0, scalar2=None, op0=ALU.mult), reads=[b], writes=[b])
                A3 = sbuf(st, "A3D", [128, 4, 64], F32)
                W3 = sbuf(st, "W3D", [128, 4, 64], F32)
                L3 = sbuf(st, "L3D", [128, 4, 64], F32)
                Br3 = sbuf(st, "Br3D", [128, 4, 64], F32)
                Bi3 = sbuf(st, "Bi3D", [128, 4, 64], F32)
                for (dst, idx) in ((Br3, 0), (Bi3, 1), (A3, 2), (W3, 3), (L3, 4)):
                    P.op("dve", lambda e, dst=dst, idx=idx: e.tensor_copy(out=dst[:], in_=bTraw[:, :, idx, :]), reads=[b], writes=[b])
                fl = lambda t_: t_[:].rearrange("p a n -> p (a n)")
                tb = disc(fl(A3), fl(W3), fl(L3), 256, True, "db_")
                Bb = sbuf(st, "BbD", [128, 2, 256], F32)
                tt_ = lambda o, i0, i1, op: P.op("dve", lambda e: e.tensor_tensor(out=o, in0=i0, in1=i1, op=op), reads=[b], writes=[b])
                tt_(Bb[:, 0, :], tb["gr"][:], fl(Br3), ALU.mult)
                tt_(tb["t2"][:], tb["gi"][:], fl(Bi3), ALU.mult)
                tt_(Bb[:, 0, :], Bb[:, 0, :], tb["t2"][:], ALU.subtract)
                tt_(Bb[:, 1, :], tb["gr"][:], fl(Bi3), ALU.mult)
                tt_(tb["t2"][:], tb["gi"][:], fl(Br3), ALU.mult)
                tt_(Bb[:, 1, :], Bb[:, 1, :], tb["t2"][:], ALU.add)
                Bblk = sbuf(st, "BblkD", [128, 4, 2, 512], BF16)
                for dc in range(4):
                    for ri in range(2):
                        P.op("dve", lambda e, dc=dc, ri=ri: e.tensor_tensor(
                            out=Bblk[:, dc, ri, :].rearrange("p (g n) -> p g n", n=64),
                            in0=Bb[:, ri, dc * 64:(dc + 1) * 64].unsqueeze(1).to_broadcast([128, 8, 64]),
                            in1=maskB[:, :].unsqueeze(2).to_broadcast([128, 8, 64]), op=ALU.mult), reads=[b], writes=[b])
                Cblk = sbuf(st, "CblkD", [128, 16, 2, 128], F32)
                for dp in range(16):
                    pl = dp % 4
                    for k in range(2):
                        P.op("dve", lambda e, dp=dp, pl=pl, k=k: e.tensor_tensor(
                            out=Cblk[:, dp, k, :].rearrange("p (g o) -> p g o", o=16),
                            in0=cT[:, k, dp, :].unsqueeze(1).to_broadcast([128, 8, 16]),
                            in1=maskC[:, k, pl, :].unsqueeze(2).to_broadcast([128, 8, 16]), op=ALU.mult), reads=[b], writes=[b])

                G = sbuf(st, "GD", [128, 2, S], BF16)
                bG = Buf()
                UTs = sbuf(st, "UTsD", [128, S], BF16)
                Yacc = sbuf(st, "YaccD", [128, S], F32)
                bUT, bY = Buf(), Buf()
                X = [[(sbuf(st, "XD%d%d" % (i, j), [128, SEG], F32), Buf()) for j in range(2)] for i in range(2)]
                endst = sbuf(st, "endstD", [128, 2], F32)
                ptmp = sbuf(st, "ptmpD", [128, SEG], F32)
                bptmp = Buf()
                bend = Buf()
                psr = Rot([psum(st, "psrD%d" % i) for i in range(2)])
                psi = Rot([psum(st, "psiD%d" % i) for i in range(2)])
                psY = Rot([psum(st, "psYD%d" % i) for i in range(2)])

                def stt(eng, out, in0, scalar, in1, rd, wr):
                    P.op(eng, lambda e: e.scalar_tensor_tensor(out=out, in0=in0, scalar=scalar, in1=in1, op0=ALU.mult, op1=ALU.add), reads=rd + [bprm], writes=wr)

                for ct in range(2):
                    P.dma("sp", lambda e, ct=ct: e.dma_start(out=UTs[:], in_=UT[ct * 128:(ct + 1) * 128, :]), writes=[bUT])
                    for q4 in range(4):
                        P.op("act", lambda e, q4=q4, ct=ct: e.activation(out=Yacc[:, q4 * 2048:(q4 + 1) * 2048], in_=UTs[:, q4 * 2048:(q4 + 1) * 2048], func=AF.Copy,
                                                                         scale=dcol[:, ct:ct + 1]), reads=[bUT, bprm], writes=[bY])
                    for d in range(2):
                        for pl in range(4):
                            P8 = ct * 4 + pl
                            dp = d * 8 + P8
                            dc = d * 2 + ct
                            segs = list(range(S // SEG))
                            if d == 1:
                                segs = segs[::-1]
                            for si, sg_ in enumerate(segs):
                                c0 = sg_ * SEG
                                (Ar, bAr), (Ai, bAi) = X[0]
                                for b4 in range(SEG // 512):
                                    pr, bpr = psr.next()
                                    pi_, bpi = psi.next()
                                    P.op("pe", lambda e, pr=pr, dc=dc, pl=pl, c0=c0, b4=b4: e.matmul(pr[:], lhsT=Bblk[:, dc, 0, pl * 128:(pl + 1) * 128],
                                                                                                    rhs=UTs[:, c0 + b4 * 512:c0 + (b4 + 1) * 512], start=True, stop=True),
                                         reads=[bprm, bUT], writes=[bpr])
                                    P.op("pe", lambda e, pi_=pi_, dc=dc, pl=pl, c0=c0, b4=b4: e.matmul(pi_[:], lhsT=Bblk[:, dc, 1, pl * 128:(pl + 1) * 128],
                                                                                                      rhs=UTs[:, c0 + b4 * 512:c0 + (b4 + 1) * 512], start=True, stop=True),
                                         reads=[bprm, bUT], writes=[bpi])
                                    P.op("act", lambda e, pr=pr, Ar=Ar, b4=b4: e.activation(out=Ar[:, b4 * 512:(b4 + 1) * 512], in_=pr[:], func=AF.Copy), reads=[bpr], writes=[bAr])
                                    P.op("act", lambda e, pi_=pi_, Ai=Ai, b4=b4: e.activation(out=Ai[:, b4 * 512:(b4 + 1) * 512], in_=pi_[:], func=AF.Copy), reads=[bpi], writes=[bAi])
                                if si > 0:
                                    col = 0 if d == 0 else SEG - 1
                                    cs_ = slice(col, col + 1)
                                    stt("dve", Ar[:, cs_], endst[:, 0:1], lamP[:, dp, 0, 0:1], Ar[:, cs_], [bend, bAr], [bAr])
                                    stt("dve", Ar[:, cs_], endst[:, 1:2], lamP[:, dp, 0, 2:3], Ar[:, cs_], [bend, bAr], [bAr])
                                    stt("dve", Ai[:, cs_], endst[:, 1:2], lamP[:, dp, 0, 0:1], Ai[:, cs_], [bend, bAi], [bAi])
                                    stt("dve", Ai[:, cs_], endst[:, 0:1], lamP[:, dp, 0, 1:2], Ai[:, cs_], [bend, bAi], [bAi])
                                cur = 0
                                for k in range(NK):
                                    dd = 1 << k
                                    (Sr, bSr), (Si, bSi) = X[cur]
                                    (Dr, bDr), (Di, bDi) = X[1 - cur]
                                    if d == 0:
                                        o, i_, hd = slice(dd, SEG), slice(0, SEG - dd), slice(0, dd)
                                    else:
                                        o, i_, hd = slice(0, SEG - dd), slice(dd, SEG), slice(SEG - dd, SEG)
                                    stt("dve", Dr[:, o], Sr[:, i_], lamP[:, dp, k, 0:1], Sr[:, o], [bSr], [bDr])
                                    stt("dve", Dr[:, o], Si[:, i_], lamP[:, dp, k, 2:3], Dr[:, o], [bSi, bDr], [bDr])
                                    stt("dve", Di[:, o], Si[:, i_], lamP[:, dp, k, 0:1], Si[:, o], [bSi], [bDi])
                                    stt("dve", Di[:, o], Sr[:, i_], lamP[:, dp, k, 1:2], Di[:, o], [bSr, bDi], [bDi])
                                    P.op("act", lambda e, Dr=Dr, Sr=Sr, hd=hd: e.activation(out=Dr[:, hd], in_=Sr[:, hd], func=AF.Copy), reads=[bSr], writes=[bDr])
                                    P.op("act", lambda e, Di=Di, Si=Si, hd=hd: e.activation(out=Di[:, hd], in_=Si[:, hd], func=AF.Copy), reads=[bSi], writes=[bDi])
                                    cur = 1 - cur
                                (Fr, bFr), (Fi, bFi) = X[cur]
                                lc = SEG - 1 if d == 0 else 0
                                P.op("act", lambda e, Fr=Fr, lc=lc: e.activation(out=endst[:, 0:1], in_=Fr[:, lc:lc + 1], func=AF.Copy), reads=[bFr], writes=[bend])
                                P.op("act", lambda e, Fi=Fi, lc=lc: e.activation(out=endst[:, 1:2], in_=Fi[:, lc:lc + 1], func=AF.Copy), reads=[bFi], writes=[bend])
                                for b4 in range(SEG // 512):
                                    py, bpy = psY.next()
                                    P.op("pe", lambda e, py=py, Fr=Fr, dp=dp, b4=b4: e.matmul(py[:], lhsT=Cblk[:, dp, 0, :], rhs=Fr[:, b4 * 512:(b4 + 1) * 512], start=True, stop=False),
                                         reads=[bprm, bFr], writes=[bpy])
                                    P.op("pe", lambda e, py=py, Fi=Fi, dp=dp, b4=b4: e.matmul(py[:], lhsT=Cblk[:, dp, 1, :], rhs=Fi[:, b4 * 512:(b4 + 1) * 512], start=False, stop=True),
                                         reads=[bprm, bFi], writes=[bpy])
                                    P.op("dve", lambda e, py=py, c0=c0, b4=b4: e.tensor_tensor(out=Yacc[:, c0 + b4 * 512:c0 + (b4 + 1) * 512], in0=py[:],
                                                                                              in1=Yacc[:, c0 + b4 * 512:c0 + (b4 + 1) * 512], op=ALU.add), reads=[bpy, bY], writes=[bY])
                    for q4 in range(4):
                        P.op("act", lambda e, q4=q4, ct=ct: e.activation(out=G[:, ct, q4 * 2048:(q4 + 1) * 2048], in_=Yacc[:, q4 * 2048:(q4 + 1) * 2048], func=AF.Gelu),
                             reads=[bY], writes=[bG])
                sig = Rot([sbuf(st, "sigD%d" % i, [128, 512], F32) for i in range(2)])
                oc = Rot([sbuf(st, "ocD%d" % i, [128, 2, 512], F32) for i in range(2)])
                sq = Rot([sbuf(st, "sqD%d" % i, [128, 2, 512], BF16) for i in range(2)])
                rs = Rot([sbuf(st, "rsD%d" % i, [128, 512], F32) for i in range(2)])
                ocn = Rot([sbuf(st, "ocnD%d" % i, [128, 2, 512], BF16) for i in range(2)])
                for blk in range(S // 512):
                    t0 = blk * 512
                    o_c, boc = oc.next()
                    s_q, bsq = sq.next()
                    for jt in range(2):
                        pz, bpz = psr.next()
                        for kt in range(2):
                            P.op("pe", lambda e, pz=pz, kt=kt, jt=jt, t0=t0: e.matmul(pz[:], lhsT=Wglu[:, kt, jt * 128:(jt + 1) * 128], rhs=G[:, kt, t0:t0 + 512],
                                                                                      start=(kt == 0), stop=(kt == 1)), reads=[bprm, bG], writes=[bpz])
                        s_g, bsg = sig.next()
                        P.op("act", lambda e, s_g=s_g, pz=pz: e.activation(out=s_g[:], in_=pz[:], func=AF.Sigmoid), reads=[bpz], writes=[bsg])
                        P.op("dve", lambda e, o_c=o_c, s_g=s_g, jt=jt, t0=t0: e.tensor_tensor(out=o_c[:, jt, :], in0=G[:, jt, t0:t0 + 512], in1=s_g[:], op=ALU.mult),
                             reads=[bG, bsg], writes=[boc])
                        P.op("act", lambda e, o_c=o_c, s_q=s_q, jt=jt: e.activation(out=s_q[:, jt, :], in_=o_c[:, jt, :], func=AF.Square),
                             reads=[boc], writes=[bsq])
                    p2, bp2 = psY.next()
                    for jt in range(2):
                        P.op("pe", lambda e, p2=p2, s_q=s_q, jt=jt: e.matmul(p2[:], lhsT=ones_bf, rhs=s_q[:, jt, :], start=(jt == 0), stop=(jt == 1)),
                             reads=[bsq, b_const], writes=[bp2])
                    r_s, brs = rs.next()
                    P.op("act", lambda e, r_s=r_s, p2=p2: e.activation(out=r_s[:], in_=p2[:], func=AF.Sqrt, scale=1.0 / 256, bias=epsT[:, 0:1]),
                         reads=[bp2, b_const], writes=[brs])
                    P.op("dve", lambda e, r_s=r_s: e.reciprocal(out=r_s[:], in_=r_s[:]), reads=[brs], writes=[brs])
                    o_n, bon = ocn.next()
                    P.op("dve", lambda e, o_n=o_n, o_c=o_c, r_s=r_s: e.tensor_tensor(out=o_n[:], in0=o_c[:], in1=r_s[:].unsqueeze(1).to_broadcast([128, 2, 512]), op=ALU.mult),
                         reads=[boc, brs], writes=[bon])
                    P.dma("pool", lambda e, o_n=o_n, t0=t0: e.dma_start(out=MIXC[:, t0:t0 + 512].rearrange("(k p) t -> p k t", p=128), in_=o_n[:]), reads=[bon])
                P.barrier()

        def phase_E(l):
            xsrc = x_in if l == 0 else OUT
            with ExitStack() as st:
                Wout = sbuf(st, "Wout", [128, 8, 1024], BF16)
                Wr = sbuf(st, "Wr", [128, 8, 16], F32)
                gffn = sbuf(st, "gffn", [128, 1024], F32)
                bW, bWr, bg = Buf(), Buf(), Buf()
                wst = Rot([sbuf(st, "wstE%d" % i, [128, 1024], F32) for i in range(2)])
                for kc in range(8):
                    w, bw = wst.next()
                    P.dma("sp", lambda e, w=w, kc=kc: e.dma_start(out=w[:], in_=w_out[l, kc * 128:(kc + 1) * 128, :]), writes=[bw])
                    P.op("dve", lambda e, w=w, kc=kc: e.tensor_scalar(out=Wout[:, kc, :], in0=w[:], scalar1=gcol[:, l, 1, kc:kc + 1],
                                                                       scalar2=None, op0=ALU.mult), reads=[bw, b_const], writes=[bW])
                P.dma("sp", lambda e: e.dma_start(out=Wr[:], in_=w_router[l].rearrange("(k p) e -> p k e", p=128)), writes=[bWr])
                P.dma("sp", lambda e: e.dma_start(out=gffn[:], in_=gffn_d[l]), writes=[bg])
                acc = Rot([sbuf(st, "accE%d" % i, [128, 3, 520], F32) for i in range(2)])
                accb = Rot([sbuf(st, "accbE%d" % i, [128, 260], F32) for i in range(2)])
                xt = Rot([sbuf(st, "xtE%d" % i, [128, 1024], F32) for i in range(2)])
                mc = Rot([sbuf(st, "mcE%d" % i, [128, 2, 128], BF16) for i in range(2)])
                sa = Rot([sbuf(st, "saE%d" % i, [128, 520], F32) for i in range(2)])
                rd = Rot([sbuf(st, "rdE%d" % i, [128, 16], F32) for i in range(2)])
                oab = Rot([sbuf(st, "oabE%d" % i, [128, 768], F32) for i in range(2)])
                junk = sbuf(st, "junkE", [128, 1024], BF16)
                bjunk = Buf()
                ssq = Rot([sbuf(st, "ssqE%d" % i, [128, 4], F32) for i in range(2)])
                mixn = Rot([sbuf(st, "mixnE%d" % i, [128, 768], BF16) for i in range(2)])
                mT = Rot([sbuf(st, "mTE%d" % i, [128, 6, 128], BF16) for i in range(2)])
                x1 = Rot([sbuf(st, "x1E%d" % i, [128, 1024], F32) for i in range(2)])
                hf = Rot([sbuf(st, "hfE%d" % i, [128, 1024], F32) for i in range(2)])
                hfb = Rot([sbuf(st, "hfbE%d" % i, [128, 1024], BF16) for i in range(2)])
                hfT = Rot([sbuf(st, "hfTE%d" % i, [128, 8, 128], F32) for i in range(2)])
                ex = Rot([sbuf(st, "exE%d" % i, [128, 16], F32) for i in range(2)])
                se = Rot([sbuf(st, "seE%d" % i, [128, 2], F32) for i in range(2)])
                aff = Rot([sbuf(st, "affE%d" % i, [128, 16], F32) for i in range(2)])
                aT = Rot([sbuf(st, "aTE%d" % i, [16, 128], F32) for i in range(2)])
                pT = Rot([psum(st, "pTE%d" % i, [128, 8, 128], BF16) for i in range(1)])
                psx = Rot([psum(st, "psxE%d" % i) for i in range(2)])
                pTf = Rot([psum(st, "pTfE%d" % i, [128, 4, 128], F32) for i in range(2)])
                psl = Rot([psum(st, "pslE%d" % i) for i in range(2)])
                for tt in range(NT):
                    t0 = tt * 128
                    a_t, ba = acc.next()
                    P.dma("sp", lambda e, a_t=a_t, t0=t0: e.dma_start(out=a_t[:], in_=ACCA[:, t0:t0 + 128, :].rearrange("c p f -> p c f")),
                          writes=[ba])
                    ab_t, bab = accb.next()
                    P.dma("sp", lambda e, ab_t=ab_t, t0=t0: e.dma_start(out=ab_t[:], in_=ACCB[t0:t0 + 128, :]), writes=[bab])
                    x_t, bx = xt.next()
                    P.dma("sp", lambda e, x_t=x_t, t0=t0: e.dma_start(out=x_t[:], in_=xsrc[t0:t0 + 128, :]), writes=[bx])
                    m_c, bmc = mc.next()
                    P.dma("sp", lambda e, m_c=m_c, t0=t0: e.dma_start(out=m_c[:], in_=MIXC[:, t0:t0 + 128].rearrange("(k p) t -> p k t", p=128)),
                          writes=[bmc])
                    s_a, bsa = sa.next()
                    P.op("dve", lambda e, s_a=s_a, a_t=a_t: e.tensor_tensor(out=s_a[:], in0=a_t[:, 0, :], in1=a_t[:, 1, :], op=ALU.add), reads=[ba], writes=[bsa])
                    P.op("dve", lambda e, s_a=s_a, a_t=a_t: e.tensor_tensor(out=s_a[:], in0=s_a[:], in1=a_t[:, 2, :], op=ALU.add), reads=[ba, bsa], writes=[bsa])
                    r_d, brd = rd.next()
                    sa3 = s_a[:].rearrange("p (h d) -> p h d", d=65)
                    ab3 = ab_t[:].rearrange("p (h d) -> p h d", d=65)
                    P.op("dve", lambda e, r_d=r_d, sa3=sa3: e.reciprocal(out=r_d[:, 0:8], in_=sa3[:, :, 64]), reads=[bsa], writes=[brd])
                    P.op("dve", lambda e, r_d=r_d, ab3=ab3: e.reciprocal(out=r_d[:, 8:12], in_=ab3[:, :, 64]), reads=[bab], writes=[brd])
                    o_t, bo = oab.next()
                    P.op("dve", lambda e, o_t=o_t, sa3=sa3, r_d=r_d: e.tensor_tensor(
                        out=o_t[:, 0:512].rearrange("p (h d) -> p h d", d=64), in0=sa3[:, :, 0:64],
                        in1=r_d[:, 0:8].unsqueeze(2).to_broadcast([128, 8, 64]), op=ALU.mult), reads=[bsa, brd], writes=[bo])
                    P.op("dve", lambda e, o_t=o_t, ab3=ab3, r_d=r_d: e.tensor_tensor(
                        out=o_t[:, 512:768].rearrange("p (h d) -> p h d", d=64), in0=ab3[:, :, 0:64],
                        in1=r_d[:, 8:12].unsqueeze(2).to_broadcast([128, 4, 64]), op=ALU.mult), reads=[bab, brd], writes=[bo])
                    s_s, bss = ssq.next()
                    P.op("act", lambda e, o_t=o_t, s_s=s_s: e.activation(out=junk[:, 0:512], in_=o_t[:, 0:512], func=AF.Square, accum_out=s_s[:, 0:1]),
                         reads=[bo], writes=[bjunk, bss])
                    P.op("act", lambda e, o_t=o_t, s_s=s_s: e.activation(out=junk[:, 0:256], in_=o_t[:, 512:768], func=AF.Square, accum_out=s_s[:, 1:2]),
                         reads=[bo], writes=[bjunk, bss])
                    P.op("act", lambda e, s_s=s_s: e.activation(out=s_s[:, 0:1], in_=s_s[:, 0:1], func=AF.Sqrt, scale=1.0 / 512, bias=epsT[:, 0:1]),
                         reads=[bss, b_const], writes=[bss])
                    P.op("act", lambda e, s_s=s_s: e.activation(out=s_s[:, 1:2], in_=s_s[:, 1:2], func=AF.Sqrt, scale=1.0 / 256, bias=epsT[:, 0:1]),
                         reads=[bss, b_const], writes=[bss])
                    P.op("dve", lambda e, s_s=s_s: e.reciprocal(out=s_s[:, 0:2], in_=s_s[:, 0:2]), reads=[bss], writes=[bss])
                    m_n, bmn = mixn.next()
                    P.op("dve", lambda e, m_n=m_n, o_t=o_t, s_s=s_s: e.tensor_scalar(out=m_n[:, 0:512], in0=o_t[:, 0:512], scalar1=s_s[:, 0:1],
                                                                                     scalar2=None, op0=ALU.mult), reads=[bo, bss], writes=[bmn])
                    P.op("act", lambda e, m_n=m_n, o_t=o_t, s_s=s_s: e.activation(out=m_n[:, 512:768], in_=o_t[:, 512:768], func=AF.Copy, scale=s_s[:, 1:2]),
                         reads=[bo, bss], writes=[bmn])
                    p_T, bpT = pT.next()
                    for kc in range(6):
                        P.op("pe", lambda e, p_T=p_T, m_n=m_n, kc=kc: e.transpose(out=p_T[:, kc, :], in_=m_n[:, kc * 128:(kc + 1) * 128], identity=ident_bf),
                             reads=[bmn, b_const], writes=[bpT])
                    m_T, bmT = mT.next()
                    P.op("act", lambda e, m_T=m_T, p_T=p_T: e.activation(out=m_T[:], in_=p_T[:, 0:6, :], func=AF.Copy), reads=[bpT], writes=[bmT])
                    x_1, bx1 = x1.next()
                    for half in range(2):
                        px, bpx = psx.next()
                        for kc in range(8):
                            if kc < 6:
                                P.op("pe", lambda e, px=px, m_T=m_T, kc=kc, half=half: e.matmul(px[:], lhsT=m_T[:, kc, :], rhs=Wout[:, kc, half * 512:(half + 1) * 512],
                                                                                                 start=(kc == 0), stop=False), reads=[bmT, bW], writes=[bpx])
                            else:
                                P.op("pe", lambda e, px=px, m_c=m_c, kc=kc, half=half: e.matmul(px[:], lhsT=m_c[:, kc - 6, :], rhs=Wout[:, kc, half * 512:(half + 1) * 512],
                                                                                                 start=False, stop=(kc == 7)), reads=[bmc, bW], writes=[bpx])
                        P.op("dve", lambda e, x_1=x_1, px=px, x_t=x_t, half=half: e.tensor_tensor(out=x_1[:, half * 512:(half + 1) * 512], in0=px[:],
                                                                                                 in1=x_t[:, half * 512:(half + 1) * 512], op=ALU.add),
                             reads=[bpx, bx], writes=[bx1])
                    P.dma("pool", lambda e, x_1=x_1, t0=t0: e.dma_start(out=OUT[t0:t0 + 128, :], in_=x_1[:]), reads=[bx1])
                    P.op("act", lambda e, x_1=x_1, s_s=s_s: e.activation(out=junk[:], in_=x_1[:], func=AF.Square, accum_out=s_s[:, 2:3]),
                         reads=[bx1], writes=[bjunk, bss])
                    P.op("act", lambda e, s_s=s_s: e.activation(out=s_s[:, 2:3], in_=s_s[:, 2:3], func=AF.Sqrt, scale=1.0 / D, bias=epsT[:, 0:1]),
                         reads=[bss, b_const], writes=[bss])
                    P.op("dve", lambda e, s_s=s_s: e.reciprocal(out=s_s[:, 2:3], in_=s_s[:, 2:3]), reads=[bss], writes=[bss])
                    h_f, bhf = hf.next()
                    P.op("dve", lambda e, h_f=h_f, x_1=x_1, s_s=s_s: e.scalar_tensor_tensor(out=h_f[:], in0=x_1[:], scalar=s_s[:, 2:3], in1=gffn[:],
                                                                                           op0=ALU.mult, op1=ALU.mult), reads=[bx1, bss, bg], writes=[bhf])
                    h_b, bhb = hfb.next()
                    P.op("act", lambda e, h_b=h_b, h_f=h_f: e.activation(out=h_b[:], in_=h_f[:], func=AF.Copy), reads=[bhf], writes=[bhb])
                    P.dma("pool", lambda e, h_b=h_b, t0=t0: e.dma_start(out=HF[t0:t0 + 128, :], in_=h_b[:]), reads=[bhb], writes=[])
                    h_T, bhT = hfT.next()
                    for q4 in range(2):
                        pf, bpf = pTf.next()
                        for k4 in range(4):
                            kc = q4 * 4 + k4
                            P.op("pe", lambda e, pf=pf, h_f=h_f, kc=kc, k4=k4: e.transpose(out=pf[:, k4, :], in_=h_f[:, kc * 128:(kc + 1) * 128], identity=identf[:]),
                                 reads=[bhf, b_const], writes=[bpf])
                        P.op("act", lambda e, h_T=h_T, pf=pf, q4=q4: e.activation(out=h_T[:, q4 * 4:(q4 + 1) * 4, :], in_=pf[:], func=AF.Copy),
                             reads=[bpf], writes=[bhT])
                    pl, bpl = psl.next()
                    for kc in range(8):
                        P.op("pe", lambda e, pl=pl, h_T=h_T, kc=kc: e.matmul(pl[:, 0:16], lhsT=h_T[:, kc, :], rhs=Wr[:, kc, :], start=(kc == 0), stop=(kc == 7)),
                             reads=[bhT, bWr], writes=[bpl])
                    e_x, bex = ex.next()
                    s_e, bse = se.next()
                    P.op("act", lambda e, e_x=e_x, pl=pl, s_e=s_e: e.activation(out=e_x[:], in_=pl[:, 0:16], func=AF.Exp, accum_out=s_e[:, 0:1]),
                         reads=[bpl], writes=[bex, bse])
                    P.op("dve", lambda e, s_e=s_e: e.reciprocal(out=s_e[:, 1:2], in_=s_e[:, 0:1]), reads=[bse], writes=[bse])
                    a_f, baf = aff.next()
                    P.op("dve", lambda e, a_f=a_f, e_x=e_x, s_e=s_e: e.tensor_scalar(out=a_f[:], in0=e_x[:], scalar1=s_e[:, 1:2], scalar2=None, op0=ALU.mult),
                         reads=[bex, bse], writes=[baf])
                    P.dma("pool", lambda e, a_f=a_f, t0=t0: e.dma_start(out=AFFD[t0:t0 + 128, :], in_=a_f[:]), reads=[baf], writes=[])
                    pl2, bpl2 = psl.next()
                    P.op("pe", lambda e, pl2=pl2, a_f=a_f: e.transpose(out=pl2[0:16, 0:128], in_=a_f[:, 0:16], identity=identf[:]),
                         reads=[baf, b_const], writes=[bpl2])
                    a_T, baT = aT.next()
                    P.op("act", lambda e, pl2=pl2, a_T=a_T: e.activation(out=a_T[:], in_=pl2[0:16, 0:128], func=AF.Copy),
                         reads=[bpl2], writes=[baT])
                    P.dma("pool", lambda e, a_T=a_T, t0=t0: e.dma_start(out=AFFTD[:, t0:t0 + 128], in_=a_T[:]), reads=[baT])
                P.barrier()

        def phase_F(l):
            with ExitStack() as st:
                with ExitStack() as s1:
                    work = sbuf(s1, "workF", [16, S], F32)
                    mask = sbuf(s1, "maskF", [16, S], F32)
                    posf = sbuf(s1, "posF", [16, S], F32)
                    m8 = sbuf(s1, "m8F", [16, 8], F32)
                    ecst = sbuf(s1, "ecstF", [128, 18], F32)
                    bwk, bmk, bps_, bm8, bec = Buf(), Buf(), Buf(), Buf(), Buf()
                    P.dma("sp", lambda e: e.dma_start(out=ecst[:], in_=ecst_d[:, :]), writes=[bec])
                    affc = sbuf(s1, "affcF", [16, S], F32)
                    b_AFFT = Buf()
                    P.dma("sp", lambda e: e.dma_start(out=affc[:], in_=AFFTD[:, :]), writes=[b_AFFT])
                    bs = sbuf(s1, "bsF", [16, 8], F32)
                    bbs = Buf()
                    P.op("dve", lambda e: e.memset(bs[:], 0.0), writes=[bbs])
                    P.op("dve", lambda e: e.memset(bs[:, 1:2], 1.0), reads=[bbs], writes=[bbs])
                    for it in range(36):
                        P.op("dve", lambda e: e.tensor_tensor(out=bs[:, 2:3], in0=bs[:, 0:1], in1=bs[:, 1:2], op=ALU.add), reads=[bbs], writes=[bbs])
                        P.op("dve", lambda e: e.tensor_scalar(out=bs[:, 2:3], in0=bs[:, 2:3], scalar1=0.5, scalar2=None, op0=ALU.mult), reads=[bbs], writes=[bbs])
                        P.op("dve", lambda e: e.tensor_scalar(out=mask[:], in0=affc[:], scalar1=bs[:, 2:3], scalar2=None, op0=ALU.is_ge),
                             reads=[b_AFFT, bbs], writes=[bmk])
                        P.op("act", lambda e: e.activation(out=work[:], in_=mask[:], func=AF.Copy, accum_out=bs[:, 3:4]), reads=[bmk], writes=[bwk, bbs])
                        P.op("dve", lambda e: e.tensor_scalar(out=bs[:, 4:5], in0=bs[:, 3:4], scalar1=float(CAP) - 0.5, scalar2=None, op0=ALU.is_ge),
                             reads=[bbs], writes=[bbs])
                        P.op("dve", lambda e: e.scalar_tensor_tensor(out=bs[:, 0:1], in0=bs[:, 2:3], scalar=bs[:, 4:5], in1=bs[:, 0:1], op0=ALU.mult, op1=ALU.max),
                             reads=[bbs], writes=[bbs])
                        P.op("dve", lambda e: e.scalar_tensor_tensor(out=bs[:, 5:6], in0=bs[:, 4:5], scalar=2.0, in1=bs[:, 2:3], op0=ALU.mult, op1=ALU.add),
                             reads=[bbs], writes=[bbs])
                        P.op("dve", lambda e: e.tensor_tensor(out=bs[:, 1:2], in0=bs[:, 1:2], in1=bs[:, 5:6], op=ALU.min), reads=[bbs], writes=[bbs])
                    bm8 = bbs
                    P.op("dve", lambda e: e.tensor_scalar(out=mask[:], in0=affc[:], scalar1=bs[:, 0:1], scalar2=None, op0=ALU.is_ge),
                         reads=[b_AFFT, bm8], writes=[bmk])
                    P.op("pool", lambda e: e.memset(work[:], 1.0), reads=[bm8], writes=[bwk])
                    P.op("dve", lambda e: e.tensor_tensor_scan(out=posf[:], data0=work[:], data1=mask[:], initial=0.0, op0=ALU.mult, op1=ALU.add),
                         reads=[bwk, bmk], writes=[bps_])
                    afl = Rot([sbuf(s1, "aflF%d" % i, [128, 16], F32) for i in range(3)])
                    dst = Rot([sbuf(s1, "dstF%d" % i, [128, 16], F32) for i in range(3)])
                    dsi = Rot([sbuf(s1, "dsiF%d" % i, [128, 16], I32) for i in range(3)])
                    pair = Rot([sbuf(s1, "pairF%d" % i, [128, 16, 2], F32) for i in range(3)])
                    ptr = Rot([psum(s1, "ptrF%d" % i) for i in range(2)])
                    for tt in range(NT):
                        t0 = tt * 128
                        pt_, bpt_ = ptr.next()
                        P.op("pe", lambda e, pt_=pt_, t0=t0: e.transpose(out=pt_[:, 0:16], in_=mask[:, t0:t0 + 128], identity=identf[0:16, 0:16]),
                             reads=[bmk, b_const], writes=[bpt_])
                        P.op("pe", lambda e, pt_=pt_, t0=t0: e.transpose(out=pt_[:, 16:32], in_=posf[:, t0:t0 + 128], identity=identf[0:16, 0:16]),
                             reads=[bps_, b_const], writes=[bpt_])
                        a_l, bal = afl.next()
                        P.dma("sp", lambda e, a_l=a_l, t0=t0: e.dma_start(out=a_l[:], in_=AFFD[t0:t0 + 128, :]), writes=[bal])
                        d_t, bdt = dst.next()
                        P.op("dve", lambda e, d_t=d_t, pt_=pt_: e.tensor_scalar(out=d_t[:], in0=pt_[:, 16:32], scalar1=ecst[:, 16:17], scalar2=None, op0=ALU.subtract),
                             reads=[bpt_, bec], writes=[bdt])
                        P.op("dve", lambda e, d_t=d_t, pt_=pt_: e.tensor_tensor(out=d_t[:], in0=d_t[:], in1=pt_[:, 0:16], op=ALU.mult),
                             reads=[bpt_, bdt], writes=[bdt])
                        P.op("dve", lambda e, d_t=d_t: e.tensor_tensor(out=d_t[:], in0=d_t[:], in1=ecst[:, 0:16], op=ALU.add), reads=[bdt, bec], writes=[bdt])
                        d_i, bdi = dsi.next()
                        P.op("dve", lambda e, d_i=d_i, d_t=d_t: e.tensor_copy(out=d_i[:], in_=d_t[:]), reads=[bdt], writes=[bdi])
                        p_r, bpr = pair.next()
                        P.op("dve", lambda e, p_r=p_r, t0=t0: e.tensor_scalar(out=p_r[:, :, 0], in0=ecst[:, 17:18].to_broadcast([128, 16]), scalar1=float(t0),
                                                                               scalar2=None, op0=ALU.add), reads=[bec], writes=[bpr])
                        P.op("act", lambda e, p_r=p_r, a_l=a_l: e.activation(out=p_r[:, :, 1], in_=a_l[:], func=AF.Copy), reads=[bal], writes=[bpr])
                        for ex_ in range(NEXP):
                            P.dma("pool", lambda e, p_r=p_r, d_i=d_i, ex_=ex_: e.indirect_dma_start(
                                out=LIST[:, :], out_offset=bass.IndirectOffsetOnAxis(ap=d_i[:, ex_:ex_ + 1], axis=0),
                                in_=p_r[:, ex_, :], in_offset=None), reads=[bpr, bdi], writes=[])
                    P.barrier()
                Wg = sbuf(st, "WgF", [128, 8, 2048], BF16)
                Wu = sbuf(st, "WuF", [128, 8, 2048], BF16)
                Wd = sbuf(st, "WdF", [128, 16, 1024], BF16)
                bWg, bWu, bWd = Buf(), Buf(), Buf()
                wst = Rot([sbuf(st, "wstF%d" % i, [128, 2048], F32) for i in range(2)])
                li = sbuf(st, "liF", [128, 8, 2], F32)
                lii = sbuf(st, "liiF", [128, 8], I32)
                lif = sbuf(st, "lifF", [128, 8], F32)
                bli, blii, blif = Buf(), Buf(), Buf()
                xe = Rot([sbuf(st, "xeF%d" % i, [128, 1024], BF16) for i in range(2)])
                xeT = sbuf(st, "xeTF", [128, 8, 1024], BF16)
                bxeT = Buf()
                hid = sbuf(st, "hidF", [128, 16, 1024], BF16)
                bhid = Buf()
                sg = Rot([sbuf(st, "sgF%d" % i, [128, 512], F32) for i in range(2)])
                ys = Rot([sbuf(st, "ysF%d" % i, [128, 1024], F32) for i in range(2)])
                xr = Rot([sbuf(st, "xrF%d" % i, [128, 1024], F32) for i in range(2)])
                pT = Rot([psum(st, "pTF%d" % i, [128, 8, 128], BF16) for i in range(2)])
                psg = Rot([psum(st, "psgF%d" % i) for i in range(2)])
                psu = Rot([psum(st, "psuF%d" % i) for i in range(2)])
                psy = Rot([psum(st, "psyF%d" % i) for i in range(2)])
                cvt = [0]

                def convert(dst_ap, src_ap, rd, wr):
                    k = cvt[0] % 2
                    cvt[0] += 1
                    if k == 0:
                        P.op("act", lambda e: e.activation(out=dst_ap, in_=src_ap, func=AF.Copy), reads=rd, writes=wr)
                    else:
                        P.op("dve", lambda e: e.tensor_copy(out=dst_ap, in_=src_ap), reads=rd, writes=wr)

                for ex_ in range(NEXP):
                    for kc in range(8):
                        w, bw = wst.next()
                        P.dma("sp", lambda e, w=w, kc=kc, ex_=ex_: e.dma_start(out=w[:], in_=w_gate[l][ex_, kc * 128:(kc + 1) * 128, :]), writes=[bw])
                        convert(Wg[:, kc, :], w[:], [bw], [bWg])
                        w, bw = wst.next()
                        P.dma("sp", lambda e, w=w, kc=kc, ex_=ex_: e.dma_start(out=w[:], in_=w_up[l][ex_, kc * 128:(kc + 1) * 128, :]), writes=[bw])
                        convert(Wu[:, kc, :], w[:], [bw], [bWu])
                    for fc2 in range(8):
                        w, bw = wst.next()
                        P.dma("sp", lambda e, w=w, fc2=fc2, ex_=ex_: e.dma_start(
                            out=w[:].rearrange("p (a d) -> p a d", a=2), in_=w_down[l][ex_, fc2 * 256:(fc2 + 1) * 256, :].rearrange("(a p) d -> p a d", p=128)),
                            writes=[bw])
                        convert(Wd[:, fc2 * 2:fc2 * 2 + 2, :], w[:].rearrange("p (a d) -> p a d", a=2), [bw], [bWd])
                    P.dma("sp", lambda e, ex_=ex_: e.dma_start(out=li[:], in_=LIST[ex_ * ROWS:ex_ * ROWS + CAP, :].rearrange("(j p) c -> p j c", p=128)),
                          writes=[bli])
                    P.op("dve", lambda e: e.tensor_scalar(out=lif[:], in0=li[:, :, 0], scalar1=0.0, scalar2=float(S - 1), op0=ALU.max, op1=ALU.min), reads=[bli], writes=[blif])
                    P.op("dve", lambda e: e.tensor_copy(out=lii[:], in_=lif[:]), reads=[blif], writes=[blii])
                    for j in range(8):
                        x_e, bxe = xe.next()
                        P.dma("pool", lambda e, x_e=x_e, j=j: e.indirect_dma_start(out=x_e[:], out_offset=None, in_=HF[:, :],
                                                                                    in_offset=bass.IndirectOffsetOnAxis(ap=lii[:, j:j + 1], axis=0)),
                              reads=[blii], writes=[bxe])
                        p_T, bpT = pT.next()
                        for kc in range(8):
                            P.op("pe", lambda e, p_T=p_T, x_e=x_e, kc=kc: e.transpose(out=p_T[:, kc, :], in_=x_e[:, kc * 128:(kc + 1) * 128], identity=ident_bf),
                                 reads=[bxe, b_const], writes=[bpT])
                        P.op("act" if j % 2 == 0 else "dve", (lambda e, p_T=p_T, j=j: e.activation(out=xeT[:, :, j * 128:(j + 1) * 128], in_=p_T[:], func=AF.Copy)) if j % 2 == 0
                             else (lambda e, p_T=p_T, j=j: e.tensor_copy(out=xeT[:, :, j * 128:(j + 1) * 128], in_=p_T[:])), reads=[bpT], writes=[bxeT])
                    for tb in range(2):
                        for fc in range(16):
                            pg, bpg = psg.next()
                            pu, bpu = psu.next()
                            for kc in range(8):
                                P.op("pe", lambda e, pg=pg, kc=kc, fc=fc, tb=tb: e.matmul(pg[:], lhsT=Wg[:, kc, fc * 128:(fc + 1) * 128], rhs=xeT[:, kc, tb * 512:(tb + 1) * 512],
                                                                                          start=(kc == 0), stop=(kc == 7)), reads=[bWg, bxeT], writes=[bpg])
                            for kc in range(8):
                                P.op("pe", lambda e, pu=pu, kc=kc, fc=fc, tb=tb: e.matmul(pu[:], lhsT=Wu[:, kc, fc * 128:(fc + 1) * 128], rhs=xeT[:, kc, tb * 512:(tb + 1) * 512],
                                                                                          start=(kc == 0), stop=(kc == 7)), reads=[bWu, bxeT], writes=[bpu])
                            s_g, bsg = sg.next()
                            P.op("act", lambda e, s_g=s_g, pg=pg: e.activation(out=s_g[:], in_=pg[:], func=AF.Silu), reads=[bpg], writes=[bsg])
                            P.op("dve", lambda e, s_g=s_g, pu=pu, fc=fc, tb=tb: e.tensor_tensor(out=hid[:, fc, tb * 512:(tb + 1) * 512], in0=pu[:], in1=s_g[:], op=ALU.mult),
                                 reads=[bpu, bsg], writes=[bhid])
                    for j in range(8):
                        y_s, bys = ys.next()
                        for half in range(2):
                            py, bpy = psy.next()
                            for fc in range(16):
                                P.op("pe", lambda e, py=py, fc=fc, j=j, half=half: e.matmul(py[:], lhsT=hid[:, fc, j * 128:(j + 1) * 128], rhs=Wd[:, fc, half * 512:(half + 1) * 512],
                                                                                            start=(fc == 0), stop=(fc == 15)), reads=[bhid, bWd], writes=[bpy])
                            P.op("act", lambda e, y_s=y_s, py=py, half=half, j=j: e.activation(out=y_s[:, half * 512:(half + 1) * 512], in_=py[:], func=AF.Copy,
                                                                                                 scale=li[:, j, 1:2]), reads=[bpy, bli], writes=[bys])
                        x_r, bxr = xr.next()
                        P.dma("pool", lambda e, x_r=x_r, j=j: e.indirect_dma_start(out=x_r[:], out_offset=None, in_=OUT[:, :],
                                                                                    in_offset=bass.IndirectOffsetOnAxis(ap=lii[:, j:j + 1], axis=0)),
                              reads=[blii, b_X], writes=[bxr])
                        P.op("dve", lambda e, x_r=x_r, y_s=y_s: e.tensor_tensor(out=x_r[:], in0=x_r[:], in1=y_s[:], op=ALU.add), reads=[bxr, bys], writes=[bxr])
                        P.dma("pool", lambda e, x_r=x_r, j=j: e.indirect_dma_start(out=OUT[:, :], out_offset=bass.IndirectOffsetOnAxis(ap=lii[:, j:j + 1], axis=0),
                                                                                    in_=x_r[:], in_offset=None), reads=[bxr, blii], writes=[b_X])
                P.barrier()

        PHASES = {}
        PHASES["A"] = phase_A
        PHASES["B"] = phase_B
        PHASES["C"] = phase_C
        PHASES["D"] = phase_D
        PHASES["E"] = phase_E
        PHASES["F"] = phase_F
        order = "ABCDEF"
        stop = False
        for l in range(depth):
            if l > 0:
                P.new_epoch()
            for ph in order:
                if ph in PHASES and (only is None or ph in only):
                    PHASES[ph](l)
                if done(l, ph):
                    stop = True
                    break
            if stop:
                break
        P.finish_wait_all("sp")
        P.emit()
    return nc


def _consts():
    c = {}
    bf = ml_dtypes.bfloat16
    cm = np.zeros((6, 128, 128), np.float32)
    i = np.arange(128)
    cm[0] = np.eye(128)
    cm[1] = (i[:, None] // 64 == i[None, :] // 64)
    cm[2] = 1.0
    cm[3] = (i[:, None] < i[None, :])
    cm[4] = np.eye(128)[::-1]
    c["cmat"] = cm.astype(bf)
    c["identf"] = np.eye(128, dtype=np.float32)
    slopes = np.array([2.0 ** (-8.0 * (h + 1) / 8) for h in range(8)], np.float64)
    db = np.zeros((3, 8, 128, 256), np.float64)
    k = np.arange(128)[:, None]
    q = np.arange(256)[None, :]
    rel = k - q + 64
    valid = np.abs(rel) <= 64
    for ci, dil in enumerate((1, 4, 16)):
        for h in range(8):
            db[ci, h] = np.where(valid, -slopes[h] * dil * np.abs(rel) * 8.0, NEGB)
    c["dbias"] = db.astype(np.float32).astype(bf)
    variants = _na_variants()
    nm = np.zeros((21, 128, 128), np.float32)
    kk = np.arange(128)
    krl, kc = kk // 64, kk % 64
    qrl, qc = kk // 64, kk % 64
    cs = np.clip(qc - 8, 0, 48)
    colv = (kc[:, None] >= cs[None, :]) & (kc[:, None] < cs[None, :] + 16)
    for vi, (a, jp) in enumerate(variants):
        krow = 2 * (a + jp) + krl
        qrow = 2 * a + qrl
        rs = np.clip(qrow - 4, 0, 120)
        rowv = (krow[:, None] >= rs[None, :]) & (krow[:, None] < rs[None, :] + 8)
        nm[vi] = np.where(colv & rowv, 0.0, NEGB)
    c["namask"] = nm.astype(bf)
    p = np.arange(128)
    c["maskB"] = (p[:, None] // 16 == np.arange(8)[None, :]).astype(np.float32)
    mc = np.zeros((128, 4, 8), np.float32)
    for pl in range(4):
        for g8 in range(8):
            mc[:, pl, g8] = (g8 == 2 * pl + p // 64)
    c["maskC"] = mc
    ec = np.zeros((128, 18), np.float32)
    ec[:, 0:16] = np.arange(16)[None, :] * ROWS + CAP + p[:, None]
    ec[:, 16] = CAP + p + 1
    ec[:, 17] = p
    c["ecst"] = ec
    return c


def _na_variants():
    v = [(10, jp) for jp in (-2, -1, 0, 1, 2)]
    v += [(0, jp) for jp in (0, 1, 2, 3)]
    v += [(1, jp) for jp in (-1, 0, 1, 2)]
    v += [(62, jp) for jp in (-2, -1, 0, 1)]
    v += [(63, jp) for jp in (-3, -2, -1, 0)]
    return v


def _na_block_tiles(a):
    if a == 0:
        return [(j, 5 + j, j) for j in range(4)]
    if a == 1:
        return [(a + jp, 9 + (jp + 1), jp) for jp in (-1, 0, 1, 2)]
    if a == 62:
        return [(a + jp, 13 + (jp + 2), jp) for jp in (-2, -1, 0, 1)]
    if a == 63:
        return [(a + jp, 17 + (jp + 3), jp) for jp in (-3, -2, -1, 0)]
    return [(a + jp, jp + 2, jp) for jp in (-2, -1, 0, 1, 2)]


def _prep_shared(inp):
    L = DEPTH
    f = lambda a: np.ascontiguousarray(np.asarray(a, dtype=np.float32))
    sh = {}
    for k in ("w_in", "w_out", "w_glu", "w_router", "w_gate", "w_up", "w_down"):
        sh[k] = f(inp[k])
    onorm = np.concatenate([inp["out_norm_a"], inp["out_norm_b"], inp["out_norm_c"]], axis=1)
    g3 = np.stack([inp["attn_norm"], onorm, inp["ffn_norm"]], axis=1)
    sh["gcol"] = f(g3.reshape(L, 3, 8, 128).transpose(3, 0, 1, 2))
    gq = np.stack([inp["q_norm_a"], inp["k_norm_a"], inp["q_norm_b"], inp["k_norm_b"]], axis=1)
    sh["gqk"] = f(np.concatenate([gq, gq], axis=2).transpose(2, 0, 1))
    sh["gffn"] = f(np.broadcast_to(np.asarray(inp["ffn_norm"])[:, None, :], (L, 128, D)))
    rp = np.asarray(inp["rel_pos_bias"], np.float32)
    kk = np.arange(128)
    krl, kc = kk // 64, kk % 64
    nabt = np.zeros((L, 7, 4, 128, 128), np.float32)
    dc = np.clip(kc[:, None] - kc[None, :] + 15, 0, 30)
    for jp in range(-3, 4):
        dr = np.clip(2 * jp + krl[:, None] - krl[None, :] + 7, 0, 14)
        nabt[:, jp + 3] = rp[:, :, dr, dc]
    sh["nab"] = nabt
    are = np.asarray(inp["s5_a_re"], np.float32)
    aim = np.asarray(inp["s5_a_im"], np.float32)
    ldt = np.broadcast_to(np.asarray(inp["s5_log_dt"], np.float32)[..., None], are.shape)
    p3 = np.stack([are, aim, ldt], axis=1)
    t = p3.reshape(L, 3, 2, 8, 2, 64)
    sh["s5lam"] = f(t.transpose(4, 5, 0, 1, 2, 3).reshape(128, L, 3, 16))
    bre = np.asarray(inp["s5_b_re"], np.float32)
    bim = np.asarray(inp["s5_b_im"], np.float32)
    rep = lambda a: np.broadcast_to(a[..., None], a.shape + (16,))
    q5 = np.stack([bre, bim, rep(are), rep(aim), rep(ldt)], axis=0)
    q5 = q5.reshape(5, L, 2, 2, 8, 64, 16)
    sh["s5bT"] = f(q5.transpose(4, 6, 1, 2, 3, 0, 5).reshape(128, L, 4, 5, 64))
    cre = np.asarray(inp["s5_c_re"], np.float32)
    cim = np.asarray(inp["s5_c_im"], np.float32)
    c2 = np.stack([cre, cim], axis=0).reshape(2, L, 2, 8, 2, 16, 64)
    sh["s5cT"] = f(c2.transpose(4, 6, 1, 0, 2, 3, 5).reshape(128, L, 2, 16, 16))
    sh["s5dd"] = f(np.asarray(inp["s5_d"]).reshape(L, 2, 128).transpose(2, 0, 1))
    sh.update(_consts())
    return sh


_NC_CACHE = {}
_AX0 = ("w_in", "w_out", "w_glu", "w_router", "gffn", "nab")
_SPLIT = ("w_gate", "w_up", "w_down")
_AX1 = ("gcol", "gqk", "s5lam", "s5bT", "s5cT", "s5dd")
LAYERS_PER_LAUNCH = 4


def kernel(**inputs):
    x = np.asarray(inputs["x"], dtype=np.float32)
    sh = _prep_shared(inputs)
    npl = LAYERS_PER_LAUNCH
    if "nc" not in _NC_CACHE:
        _NC_CACHE["nc"] = build_program(depth=npl, nl=npl)
    nc = _NC_CACHE["nc"]
    cur = [np.ascontiguousarray(x[c]) for c in range(8)]
    for l0 in range(0, DEPTH, npl):
        shl = {}
        for k, v in sh.items():
            if k in _SPLIT:
                for i in range(npl):
                    shl["%s%d" % (k, i)] = v[l0 + i]
            elif k in _AX0:
                shl[k] = np.ascontiguousarray(v[l0:l0 + npl])
            elif k in _AX1:
                shl[k] = np.ascontiguousarray(v[:, l0:l0 + npl])
            else:
                shl[k] = v
        in_maps = []
        for c in range(8):
            m = dict(shl)
            m["x"] = cur[c]
            in_maps.append(m)
        res = run_bass_kernel_spmd(nc, in_maps, core_ids=list(range(8)))
        cur = [np.ascontiguousarray(np.asarray(r["out"], dtype=np.float32)) for r in res.results]
    return np.stack(cur, axis=0)
```

```python
import math
import numpy as np
import ml_dtypes
from contextlib import ExitStack
import concourse.bass as bass
import concourse.mybir as mybir
from concourse.bass_utils import run_bass_kernel_spmd

F32 = mybir.dt.float32
BF16 = mybir.dt.bfloat16
I32 = mybir.dt.int32
AF = mybir.ActivationFunctionType
ALU = mybir.AluOpType
AX = mybir.AxisListType

S = 8192
D = 1024
DEPTH = 4
NT = S // 128
EPS = 1e-6
PADR = 1024
NEGB = -30000.0
NEXP = 16
CAP = 1024
ROWS = CAP + 128
SEG = 2048


class Buf:
    __slots__ = ("name", "lw", "rd")

    def __init__(self, name=""):
        self.name = name
        self.lw = None
        self.rd = {}


class Prog:
    ENGS = ("pe", "act", "dve", "pool", "sp")

    def __init__(self, nc, es, ndma_sems=16):
        self.nc = nc
        self.es = es
        self.q = {e: [] for e in self.ENGS}
        self.seq = {}
        self.sem = {}
        self.cur = {}
        self.epoch = 0
        for e in ("pe", "act", "dve", "pool"):
            self.cur[e] = e + "#0"
            self.sem[self.cur[e]] = es.enter_context(nc.semaphore("prog_" + e + "_0"))
            self.seq[self.cur[e]] = 0
        self.waited = {e: {} for e in self.ENGS}
        self.dpool = {}
        self.dval = {}
        self.drr = {}
        for qn in ("sp", "pool", "act"):
            self.dpool[qn] = [es.enter_context(nc.semaphore("dq_%s_%d" % (qn, i))) for i in range(ndma_sems)]
            for i in range(ndma_sems):
                self.dval[(qn, i)] = 0
            self.drr[qn] = 0
        self.ninstr = 0

    def new_epoch(self):
        self.epoch += 1
        for e in ("pe", "act", "dve", "pool"):
            k = "%s#%d" % (e, self.epoch)
            self.cur[e] = k
            self.sem[k] = self.es.enter_context(self.nc.semaphore("prog_%s_%d" % (e, self.epoch)))
            self.seq[k] = 0

    def _semobj(self, key):
        if isinstance(key, str):
            return self.sem[key]
        return self.dpool[key[0]][key[1]]

    def _wait(self, eng, key, val):
        if val <= 0:
            return
        w = self.waited[eng]
        if w.get(key, 0) >= val:
            return
        w[key] = val
        self.q[eng].append(("w", key, val))

    def _deps(self, eng, reads, writes):
        deps = {}
        for b in reads:
            if b.lw is not None and deps.get(b.lw[0], 0) < b.lw[1]:
                deps[b.lw[0]] = b.lw[1]
        for b in writes:
            if b.lw is not None and deps.get(b.lw[0], 0) < b.lw[1]:
                deps[b.lw[0]] = b.lw[1]
            for k, v in b.rd.items():
                if deps.get(k, 0) < v:
                    deps[k] = v
        for k, v in deps.items():
            if eng == "pe" and isinstance(k, str) and k.startswith("pe#"):
                continue
            self._wait(eng, k, v)

    def op(self, eng, fn, reads=(), writes=()):
        self._deps(eng, reads, writes)
        key = self.cur[eng]
        self.seq[key] += 1
        v = self.seq[key]
        self.q[eng].append(("i", fn, key, 1))
        for b in writes:
            b.lw = (key, v)
            b.rd = {}
        for b in reads:
            if b.rd.get(key, 0) < v:
                b.rd[key] = v
        self.ninstr += 1

    def dma(self, qn, fn, reads=(), writes=()):
        self._deps(qn, reads, writes)
        i = self.drr[qn]
        self.drr[qn] = (i + 1) % len(self.dpool[qn])
        key = (qn, i)
        self._wait(qn, key, self.dval[key])
        self.dval[key] += 16
        v = self.dval[key]
        self.q[qn].append(("i", fn, key, 16))
        for b in writes:
            b.lw = (key, v)
            b.rd = {}
        for b in reads:
            b.rd[key] = v
        self.ninstr += 1

    def barrier(self):
        for eng in self.ENGS:
            for e in ("pe", "act", "dve", "pool"):
                self._wait(eng, self.cur[e], self.seq[self.cur[e]])
            for key, v in self.dval.items():
                self._wait(eng, key, v)

    def finish_wait_all(self, eng="sp"):
        for e in ("pe", "act", "dve", "pool"):
            self._wait(eng, self.cur[e], self.seq[self.cur[e]])
        for key, v in self.dval.items():
            self._wait(eng, key, v)

    def emit(self):
        nc = self.nc
        engmap = {"pe": "tensor", "act": "scalar", "dve": "vector", "pool": "gpsimd", "sp": "sync"}
        with nc.Block() as block:
            for e in self.ENGS:
                items = self.q[e]

                def body(engobj, items=items):
                    for it in items:
                        if it[0] == "w":
                            engobj.wait_ge(self._semobj(it[1]), it[2])
                        else:
                            it[1](engobj).then_inc(self._semobj(it[2]), it[3])
                getattr(block, engmap[e])(body)
        self.q = {e: [] for e in self.ENGS}


class Rot:
    def __init__(self, tiles):
        self.t = [(t, Buf()) for t in tiles]
        self.i = 0

    def next(self):
        r = self.t[self.i]
        self.i = (self.i + 1) % len(self.t)
        return r


def build_program(depth=DEPTH, stop_after=None, dbg=(), only=None, feed=(), nl=DEPTH):
    nc = bass.Bass("TRN2", target_bir_lowering=False)

    def din(name, shape, dt=F32):
        return nc.dram_tensor(name, list(shape), dt, kind="ExternalInput").ap()

    def dscr(name, shape, dt):
        kind = "ExternalOutput" if name in dbg else ("ExternalInput" if name in feed else "Internal")
        return nc.dram_tensor(name, list(shape), dt, kind=kind).ap()

    L = nl
    x_in = din("x", [S, D])
    OUT = nc.dram_tensor("out", [S, D], F32, kind="ExternalOutput").ap()
    w_in = din("w_in", [L, D, 2560])
    w_out = din("w_out", [L, D, D])
    w_glu = din("w_glu", [L, 256, 256])
    w_router = din("w_router", [L, D, NEXP])
    if only is None or "F" in only:
        w_gate = [din("w_gate%d" % i, [NEXP, D, 2048]) for i in range(L)]
        w_up = [din("w_up%d" % i, [NEXP, D, 2048]) for i in range(L)]
        w_down = [din("w_down%d" % i, [NEXP, 2048, D]) for i in range(L)]
    gcol_d = din("gcol", [128, L, 3, 8])
    gqk_d = din("gqk", [128, L, 4])
    gffn_d = din("gffn", [L, 128, D])
    nab_d = din("nab", [L, 7, 4, 128, 128])
    dbias_d = din("dbias", [3, 8, 128, 256], BF16)
    namask_d = din("namask", [21, 128, 128], BF16)
    cmat_d = din("cmat", [6, 128, 128], BF16)
    identf_d = din("identf", [128, 128])
    s5lam_d = din("s5lam", [128, L, 3, 16])
    s5bT_d = din("s5bT", [128, L, 4, 5, 64])
    s5cT_d = din("s5cT", [128, L, 2, 16, 16])
    s5d_d = din("s5dd", [128, L, 2])
    maskB_d = din("maskB", [128, 8])
    maskC_d = din("maskC", [128, 4, 8])
    ecst_d = din("ecst", [128, 18])

    QTa = dscr("QTa", [512, S], BF16)
    KTa = dscr("KTa", [512, S], BF16)
    Va = dscr("Va", [S + 2 * PADR, 8 * 128], BF16)
    QTb = dscr("QTb", [256, S], BF16)
    KTb = dscr("KTb", [256, S], BF16)
    Vb = dscr("Vb", [S, 4 * 128], BF16)
    UT = dscr("UT", [256, S], BF16)
    ACCA = dscr("ACCA", [3, S, 8 * 65], F32)
    ACCB = dscr("ACCB", [S, 4 * 65], F32)
    MIXC = dscr("MIXC", [256, S], BF16)
    HF = dscr("HF", [S, D], BF16)
    LIST = dscr("LIST", [NEXP * ROWS, 2], F32)
    AFFD = dscr("AFFD", [S, NEXP], F32)
    AFFTD = dscr("AFFTD", [NEXP, S], F32)

    es = ExitStack()
    with es:
        P = Prog(nc, es)

        uid = [0]

        def sbuf(stack, name, shape, dt):
            uid[0] += 1
            return stack.enter_context(nc.sbuf_tensor("s%d_%s" % (uid[0], name), list(shape), dt))

        def psum(stack, name, shape=(128, 512), dt=F32):
            uid[0] += 1
            return stack.enter_context(nc.psum_tensor("p%d_%s" % (uid[0], name), list(shape), dt))

        cmat = sbuf(es, "cmat", [128, 6, 128], BF16)
        identf = sbuf(es, "identf", [128, 128], F32)
        gcol = sbuf(es, "gcol", [128, L, 3, 8], F32)
        gqk = sbuf(es, "gqk", [128, L, 4], F32)
        epsT = sbuf(es, "epsT", [128, 1], F32)
        b_const = Buf("const")
        P.dma("sp", lambda e: e.dma_start(out=cmat[:], in_=cmat_d.rearrange("k p f -> p k f")), writes=[b_const])
        P.dma("sp", lambda e: e.dma_start(out=identf[:], in_=identf_d[:, :]), writes=[b_const])
        P.dma("sp", lambda e: e.dma_start(out=gcol[:], in_=gcol_d[:, :, :, :]), writes=[b_const])
        P.dma("sp", lambda e: e.dma_start(out=gqk[:], in_=gqk_d[:, :, :]), writes=[b_const])
        P.op("pool", lambda e: e.memset(epsT[:], EPS), writes=[b_const])
        ident_bf = cmat[:, 0, :]
        blockones = cmat[:, 1, :]
        ones_bf = cmat[:, 2, :]
        b_X = Buf("X")
        b_QK = Buf("QK")
        b_ACCA = Buf("ACCA")
        b_ACCB = Buf("ACCB")
        b_MIXC = Buf("MIXC")
        b_HF = Buf("HF")
        b_LIST = Buf("LIST")
        b_AFF = Buf("AFF")

        with ExitStack() as s0:
            z = sbuf(s0, "zpad", [128, 8, 1024], BF16)
            bz = Buf()
            P.op("pool", lambda e: e.memset(z[:], 0.0), writes=[bz])
            for r0 in (0, PADR + S):
                P.dma("sp", lambda e, r0=r0: e.dma_start(out=Va[r0:r0 + PADR, :].rearrange("(p a) c -> p a c", a=8), in_=z[:]),
                      reads=[bz], writes=[])
            P.barrier()

        def done(l, ph):
            return stop_after is not None and (l, ph) == tuple(stop_after)

        def phase_A(l):
            xsrc = x_in if l == 0 else OUT
            with ExitStack() as st:
                Win = sbuf(st, "Win", [128, 8, 2560], BF16)
                wst = Rot([sbuf(st, "wst%d" % i, [128, 2560], F32) for i in range(2)])
                xt = Rot([sbuf(st, "xt%d" % i, [128, 4, 1024], F32) for i in range(2)])
                hn = Rot([sbuf(st, "hn%d" % i, [128, 1024], BF16) for i in range(2)])
                hT = Rot([sbuf(st, "hT%d" % i, [128, 8, 512], BF16) for i in range(2)])
                junk = sbuf(st, "junkA", [128, 1024], BF16)
                ss = Rot([sbuf(st, "ssA%d" % i, [128, 4], F32) for i in range(2)])
                sq = Rot([sbuf(st, "sqA%d" % i, [128, 512], BF16) for i in range(2)])
                rr = Rot([sbuf(st, "rrA%d" % i, [128, 512], F32) for i in range(2)])
                qn = Rot([sbuf(st, "qnA%d" % i, [128, 512], BF16) for i in range(3)])
                vst = Rot([sbuf(st, "vstA%d" % i, [128, 8, 128], BF16) for i in range(2)])
                vbst = Rot([sbuf(st, "vbstA%d" % i, [128, 4, 128], BF16) for i in range(2)])
                pT = Rot([psum(st, "pTA%d" % i, [128, 8, 128], BF16) for i in range(2)])
                psA = Rot([psum(st, "psA%d" % i) for i in range(2)])
                ps2 = Rot([psum(st, "ps2A%d" % i) for i in range(1)])
                psv = Rot([psum(st, "psvA%d" % i) for i in range(2)])
                bWin = Buf()
                bjunk = Buf()
                for (t, b) in vst.t + vbst.t:
                    P.op("pool", lambda e, t=t: e.memset(t[:, :, 64:128], 1.0), writes=[b])
                for kc in range(8):
                    w, bw = wst.next()
                    P.dma("sp", lambda e, w=w, kc=kc: e.dma_start(out=w[:], in_=w_in[l, kc * 128:(kc + 1) * 128, :]), writes=[bw])
                    if kc % 2 == 0:
                        P.op("dve", lambda e, w=w, kc=kc: e.tensor_scalar(out=Win[:, kc, :], in0=w[:], scalar1=gcol[:, l, 0, kc:kc + 1],
                                                                           scalar2=None, op0=ALU.mult), reads=[bw, b_const], writes=[bWin])
                    else:
                        P.op("act", lambda e, w=w, kc=kc: e.activation(out=Win[:, kc, :], in_=w[:], func=AF.Copy,
                                                                        scale=gcol[:, l, 0, kc:kc + 1]), reads=[bw, b_const], writes=[bWin])
                fm_tiles = [(f0, QTa, f0, 0) for f0 in range(0, 512, 128)] + \
                           [(512 + f0, KTa, f0, 1) for f0 in range(0, 512, 128)] + \
                           [(1536 + f0, QTb, f0, 2) for f0 in range(0, 256, 128)] + \
                           [(1792 + f0, KTb, f0, 3) for f0 in range(0, 256, 128)] + \
                           [(2304 + f0, UT, f0, None) for f0 in range(0, 256, 128)]
                for blk in range(S // 512):
                    t0 = blk * 512
                    x_t, bx = xt.next()
                    P.dma("sp", lambda e, x_t=x_t, t0=t0: e.dma_start(out=x_t[:], in_=xsrc[t0:t0 + 512, :].rearrange("(j p) d -> p j d", p=128)),
                          reads=[b_X], writes=[bx])
                    s_t, bs = ss.next()
                    for j in range(4):
                        P.op("act", lambda e, x_t=x_t, s_t=s_t, j=j: e.activation(out=junk[:], in_=x_t[:, j, :], func=AF.Square,
                                                                                   accum_out=s_t[:, j:j + 1]), reads=[bx], writes=[bjunk, bs])
                    P.op("act", lambda e, s_t=s_t: e.activation(out=s_t[:], in_=s_t[:], func=AF.Sqrt, scale=1.0 / D, bias=epsT[:, 0:1]),
                         reads=[bs, b_const], writes=[bs])
                    P.op("dve", lambda e, s_t=s_t: e.reciprocal(out=s_t[:], in_=s_t[:]), reads=[bs], writes=[bs])
                    h_T, bhT = hT.next()
                    for j in range(4):
                        h_n, bhn = hn.next()
                        P.op("dve", lambda e, h_n=h_n, x_t=x_t, s_t=s_t, j=j: e.tensor_scalar(out=h_n[:], in0=x_t[:, j, :], scalar1=s_t[:, j:j + 1],
                                                                                               scalar2=None, op0=ALU.mult), reads=[bx, bs], writes=[bhn])
                        p_T, bpT = pT.next()
                        for kc in range(8):
                            P.op("pe", lambda e, p_T=p_T, h_n=h_n, kc=kc: e.transpose(out=p_T[:, kc, :], in_=h_n[:, kc * 128:(kc + 1) * 128],
                                                                                      identity=ident_bf), reads=[bhn, b_const], writes=[bpT])
                        P.op("act", lambda e, p_T=p_T, h_T=h_T, j=j: e.activation(out=h_T[:, :, j * 128:(j + 1) * 128], in_=p_T[:], func=AF.Copy),
                             reads=[bpT], writes=[bhT])
                    for (f0, dst, r0, gi) in fm_tiles:
                        ps, bps = psA.next()
                        for kc in range(8):
                            P.op("pe", lambda e, ps=ps, h_T=h_T, kc=kc, f0=f0: e.matmul(ps[:], lhsT=Win[:, kc, f0:f0 + 128], rhs=h_T[:, kc, :],
                                                                                        start=(kc == 0), stop=(kc == 7)), reads=[bWin, bhT], writes=[bps])
                        q_n, bqn = qn.next()
                        if gi is None:
                            P.op("act", lambda e, q_n=q_n, ps=ps: e.activation(out=q_n[:], in_=ps[:], func=AF.Copy), reads=[bps], writes=[bqn])
                        else:
                            s_q, bsq = sq.next()
                            P.op("act", lambda e, s_q=s_q, ps=ps: e.activation(out=s_q[:], in_=ps[:], func=AF.Square), reads=[bps], writes=[bsq])
                            p2, bp2 = ps2.next()
                            P.op("pe", lambda e, p2=p2, s_q=s_q: e.matmul(p2[:], lhsT=blockones, rhs=s_q[:], start=True, stop=True),
                                 reads=[bsq, b_const], writes=[bp2])
                            r_t, brt = rr.next()
                            P.op("act", lambda e, r_t=r_t, p2=p2: e.activation(out=r_t[:], in_=p2[:], func=AF.Sqrt, scale=1.0 / 64, bias=epsT[:, 0:1]),
                                 reads=[bp2, b_const], writes=[brt])
                            P.op("dve", lambda e, r_t=r_t: e.reciprocal(out=r_t[:], in_=r_t[:]), reads=[brt], writes=[brt])
                            P.op("dve", lambda e, q_n=q_n, ps=ps, r_t=r_t, gi=gi: e.scalar_tensor_tensor(out=q_n[:], in0=ps[:], scalar=gqk[:, l, gi:gi + 1],
                                                                                                         in1=r_t[:], op0=ALU.mult, op1=ALU.mult),
                                 reads=[bps, brt, b_const], writes=[bqn])
                        P.dma("pool", lambda e, q_n=q_n, dst=dst, r0=r0, t0=t0: e.dma_start(out=dst[r0:r0 + 128, t0:t0 + 512], in_=q_n[:]),
                              reads=[bqn], writes=[])
                    for j in range(4):
                        tk = t0 + j * 128
                        pv, bpv = psv.next()
                        for kc in range(8):
                            P.op("pe", lambda e, pv=pv, h_T=h_T, kc=kc, j=j: e.matmul(pv[:], lhsT=h_T[:, kc, j * 128:(j + 1) * 128], rhs=Win[:, kc, 1024:1536],
                                                                                      start=(kc == 0), stop=(kc == 7)), reads=[bWin, bhT], writes=[bpv])
                        v_s, bvs = vst.next()
                        P.op("act", lambda e, v_s=v_s, pv=pv: e.activation(out=v_s[:, :, 0:64], in_=pv[:].rearrange("p (h d) -> p h d", d=64), func=AF.Copy),
                             reads=[bpv], writes=[bvs])
                        P.dma("pool", lambda e, v_s=v_s, tk=tk: e.dma_start(out=Va[PADR + tk:PADR + tk + 128, :], in_=v_s[:].rearrange("p h d -> p (h d)")),
                              reads=[bvs], writes=[])
                        pw, bpw = psv.next()
                        for kc in range(8):
                            P.op("pe", lambda e, pw=pw, h_T=h_T, kc=kc, j=j: e.matmul(pw[:, 0:256], lhsT=h_T[:, kc, j * 128:(j + 1) * 128], rhs=Win[:, kc, 2048:2304],
                                                                                      start=(kc == 0), stop=(kc == 7)), reads=[bWin, bhT], writes=[bpw])
                        vb_s, bvbs = vbst.next()
                        P.op("dve", lambda e, vb_s=vb_s, pw=pw: e.tensor_copy(out=vb_s[:, :, 0:64], in_=pw[:, 0:256].rearrange("p (h d) -> p h d", d=64)),
                             reads=[bpw], writes=[bvbs])
                        P.dma("pool", lambda e, vb_s=vb_s, tk=tk: e.dma_start(out=Vb[tk:tk + 128, :], in_=vb_s[:].rearrange("p h d -> p (h d)")),
                              reads=[bvbs], writes=[])
                P.barrier()

        def phase_B(l):
            with ExitStack() as st:
                dbias = sbuf(st, "dbias", [128, 24, 256], BF16)
                bdb = Buf()
                P.dma("sp", lambda e: e.dma_start(out=dbias[:], in_=dbias_d.rearrange("c h p f -> p (c h) f")), writes=[bdb])
                QT = sbuf(st, "QTB", [128, S], BF16)
                KT = sbuf(st, "KTB", [128, S + 2 * PADR], BF16)
                bQT, bKT = Buf(), Buf()
                P.op("pool", lambda e: e.memset(KT[:, 0:PADR], 0.0), writes=[bKT])
                P.op("pool", lambda e: e.memset(KT[:, PADR + S:], 0.0), writes=[bKT])
                vt = Rot([sbuf(st, "vtB%d" % i, [128, 256], BF16) for i in range(8)])
                pt = Rot([sbuf(st, "ptB%d" % i, [128, 256], BF16) for i in range(8)])
                ost = Rot([sbuf(st, "ostB%d" % i, [128, 65], F32) for i in range(8)])
                psS = Rot([psum(st, "psSB%d" % i) for i in range(4)])
                pso = [[(psum(st, "psoB%d_%d" % (hh, par)), Buf()) for par in range(2)] for hh in range(2)]
                LOOK = 3

                def run_pipe(items):
                    n = len(items)
                    for i in range(n + LOOK):
                        if i < n:
                            items[i][0]()
                        if i - LOOK >= 0:
                            items[i - LOOK][1]()

                for hp in range(4):
                    P.dma("sp", lambda e, hp=hp: e.dma_start(out=QT[:], in_=QTa[hp * 128:(hp + 1) * 128, :]), writes=[bQT])
                    P.dma("sp", lambda e, hp=hp: e.dma_start(out=KT[:, PADR:PADR + S], in_=KTa[hp * 128:(hp + 1) * 128, :]), writes=[bKT])
                    items = []
                    for c, dil in enumerate((1, 4, 16)):
                        nq = S // dil // 128
                        for r in range(dil):
                            for m in range(nq + 1):
                                shared = {}
                                for hh in range(2):
                                    st_ = {}

                                    def s1(c=c, dil=dil, nq=nq, r=r, m=m, hh=hh, shared=shared, st_=st_, hp=hp):
                                        base = r + dil * (128 * m - 64)
                                        if hh == 0:
                                            v_t, bvt = vt.next()
                                            P.dma("sp", lambda e: e.dma_start(
                                                out=v_t[:], in_=Va[PADR + base:PADR + base + 127 * dil + 1:dil, hp * 256:(hp + 1) * 256]), writes=[bvt])
                                            shared["v"] = (v_t, bvt)
                                        clo = 128 if m == 0 else 0
                                        chi = 128 if m == nq else 256
                                        q0 = r + dil * (128 * (m - 1) + clo)
                                        ncol = chi - clo
                                        h = 2 * hp + hh
                                        pb = 64 * hh
                                        ps, bps = psS.next()
                                        P.op("pe", lambda e: e.matmul(
                                            ps[:, clo:chi], lhsT=KT[pb:pb + 64, PADR + base:PADR + base + 127 * dil + 1:dil],
                                            rhs=QT[pb:pb + 64, q0:q0 + (ncol - 1) * dil + 1:dil], start=True, stop=False),
                                            reads=[bKT, bQT], writes=[bps])
                                        P.op("pe", lambda e: e.matmul(
                                            ps[:, clo:chi], lhsT=ident_bf, rhs=dbias[:, c * 8 + h, clo:chi], start=False, stop=True),
                                            reads=[bdb, b_const], writes=[bps])
                                        p_t, bpt = pt.next()
                                        P.op("act", lambda e: e.activation(out=p_t[:, clo:chi], in_=ps[:, clo:chi], func=AF.Exp, scale=0.125),
                                             reads=[bps], writes=[bpt])
                                        st_["p"] = (p_t, bpt)

                                    def s2(c=c, dil=dil, nq=nq, r=r, m=m, hh=hh, shared=shared, st_=st_, hp=hp):
                                        v_t, bvt = shared["v"]
                                        p_t, bpt = st_["p"]
                                        h = 2 * hp + hh
                                        for j in (m - 1, m):
                                            if j < 0 or j >= nq:
                                                continue
                                            co = (j - (m - 1)) * 128
                                            po, bpo = pso[hh][j % 2]
                                            first = (j == m)
                                            P.op("pe", lambda e, po=po, co=co, first=first: e.matmul(
                                                po[:, 0:128], lhsT=p_t[:, co:co + 128], rhs=v_t[:, hh * 128:(hh + 1) * 128], start=first, stop=(not first)),
                                                reads=[bpt, bvt], writes=[bpo])
                                            if not first:
                                                o_s, bos = ost.next()
                                                P.op("dve", lambda e, o_s=o_s, po=po: e.tensor_copy(out=o_s[:], in_=po[:, 0:65]), reads=[bpo], writes=[bos])
                                                tok0 = r + dil * 128 * j
                                                P.dma("pool", lambda e, o_s=o_s, tok0=tok0: e.dma_start(
                                                    out=ACCA[c, tok0:tok0 + 127 * dil + 1:dil, h * 65:(h + 1) * 65], in_=o_s[:]),
                                                    reads=[bos], writes=[])
                                    items.append((s1, s2))
                    run_pipe(items)
                P.barrier()

        def phase_C(l):
            with ExitStack() as st:
                namask = sbuf(st, "namask", [128, 21, 128], BF16)
                nab = sbuf(st, "nab", [128, 28, 128], BF16)
                nst = Rot([sbuf(st, "nabst%d" % i, [128, 4, 128], F32) for i in range(2)])
                bnm, bnab = Buf(), Buf()
                P.dma("sp", lambda e: e.dma_start(out=namask[:], in_=namask_d.rearrange("v p f -> p v f")), writes=[bnm])
                for jj in range(7):
                    n_s, bns = nst.next()
                    P.dma("sp", lambda e, n_s=n_s, jj=jj: e.dma_start(out=n_s[:], in_=nab_d[l, jj].rearrange("h p f -> p h f")), writes=[bns])
                    P.op("act", lambda e, n_s=n_s, jj=jj: e.activation(out=nab[:, jj * 4:(jj + 1) * 4, :], in_=n_s[:], func=AF.Copy, scale=8.0),
                         reads=[bns], writes=[bnab])
                QT = sbuf(st, "QTC", [128, S], BF16)
                KT = sbuf(st, "KTC", [128, S], BF16)
                bQT, bKT = Buf(), Buf()
                vt = Rot([sbuf(st, "vtC%d" % i, [128, 256], BF16) for i in range(4)])
                pt = Rot([sbuf(st, "ptC%d" % i, [128, 128], BF16) for i in range(4)])
                ost = Rot([sbuf(st, "ostC%d" % i, [128, 65], F32) for i in range(4)])
                psS = Rot([psum(st, "psSC%d" % i) for i in range(3)])
                pso = [Rot([psum(st, "psoC%d_%d" % (hh, i)) for i in range(2)]) for hh in range(2)]
                for hp in range(2):
                    P.dma("sp", lambda e, hp=hp: e.dma_start(out=QT[:], in_=QTb[hp * 128:(hp + 1) * 128, :]), writes=[bQT])
                    P.dma("sp", lambda e, hp=hp: e.dma_start(out=KT[:], in_=KTb[hp * 128:(hp + 1) * 128, :]), writes=[bKT])
                    for a in range(64):
                        tiles = _na_block_tiles(a)
                        pos = [pso[hh].next() for hh in range(2)]
                        for ti, (kt, vi, jp) in enumerate(tiles):
                            v_t, bvt = vt.next()
                            P.dma("sp", lambda e, v_t=v_t, kt=kt, hp=hp: e.dma_start(out=v_t[:], in_=Vb[kt * 128:(kt + 1) * 128, hp * 256:(hp + 1) * 256]),
                                  writes=[bvt])
                            for hh in range(2):
                                h = 2 * hp + hh
                                pb = 64 * hh
                                ps, bps = psS.next()
                                P.op("pe", lambda e, ps=ps, pb=pb, kt=kt, a=a: e.matmul(ps[:, 0:128], lhsT=KT[pb:pb + 64, kt * 128:(kt + 1) * 128],
                                                                                         rhs=QT[pb:pb + 64, a * 128:(a + 1) * 128], start=True, stop=False),
                                     reads=[bKT, bQT], writes=[bps])
                                P.op("pe", lambda e, ps=ps, jp=jp, h=h: e.matmul(ps[:, 0:128], lhsT=ident_bf, rhs=nab[:, (jp + 3) * 4 + h, :], start=False, stop=False),
                                     reads=[bnab, b_const], writes=[bps])
                                P.op("pe", lambda e, ps=ps, vi=vi: e.matmul(ps[:, 0:128], lhsT=ident_bf, rhs=namask[:, vi, :], start=False, stop=True),
                                     reads=[bnm, b_const], writes=[bps])
                                p_t, bpt = pt.next()
                                P.op("act", lambda e, p_t=p_t, ps=ps: e.activation(out=p_t[:], in_=ps[:, 0:128], func=AF.Exp, scale=0.125),
                                     reads=[bps], writes=[bpt])
                                po, bpo = pos[hh]
                                P.op("pe", lambda e, po=po, p_t=p_t, v_t=v_t, hh=hh, ti=ti, nt=len(tiles): e.matmul(
                                    po[:, 0:128], lhsT=p_t[:], rhs=v_t[:, hh * 128:(hh + 1) * 128], start=(ti == 0), stop=(ti == nt - 1)),
                                    reads=[bpt, bvt], writes=[bpo])
                        for hh in range(2):
                            h = 2 * hp + hh
                            po, bpo = pos[hh]
                            o_s, bos = ost.next()
                            P.op("dve", lambda e, o_s=o_s, po=po: e.tensor_copy(out=o_s[:], in_=po[:, 0:65]), reads=[bpo], writes=[bos])
                            P.dma("pool", lambda e, o_s=o_s, a=a, h=h: e.dma_start(out=ACCB[a * 128:(a + 1) * 128, h * 65:(h + 1) * 65], in_=o_s[:]),
                                  reads=[bos], writes=[])
                P.barrier()

        def phase_D(l):
            PI = math.pi
            with ExitStack() as st:
                lamraw = sbuf(st, "lamraw", [128, 3, 16], F32)
                bTraw = sbuf(st, "bTraw", [128, 4, 5, 64], F32)
                cT = sbuf(st, "cTD", [128, 2, 16, 16], F32)
                maskB = sbuf(st, "maskB", [128, 8], F32)
                maskC = sbuf(st, "maskC", [128, 2, 4, 8], F32)
                dcol = sbuf(st, "dcolD", [128, 2], F32)
                wgst = sbuf(st, "wgst", [128, 2, 256], F32)
                Wglu = sbuf(st, "WgluD", [128, 2, 256], BF16)
                bprm = Buf()
                P.dma("sp", lambda e: e.dma_start(out=lamraw[:], in_=s5lam_d[:, l]), writes=[bprm])
                P.dma("sp", lambda e: e.dma_start(out=bTraw[:], in_=s5bT_d[:, l]), writes=[bprm])
                P.dma("sp", lambda e: e.dma_start(out=cT[:], in_=s5cT_d[:, l]), writes=[bprm])
                P.dma("sp", lambda e: e.dma_start(out=maskB[:], in_=maskB_d[:, :]), writes=[bprm])
                P.dma("sp", lambda e: e.dma_start(out=maskC[:, 0], in_=maskC_d[:, :, :]), writes=[bprm])
                P.dma("sp", lambda e: e.dma_start(out=dcol[:], in_=s5d_d[:, l, :]), writes=[bprm])
                P.dma("sp", lambda e: e.dma_start(out=wgst[:], in_=w_glu[l].rearrange("(k p) j -> p k j", p=128)), writes=[bprm])
                P.op("dve", lambda e: e.tensor_copy(out=Wglu[:], in_=wgst[:]), reads=[bprm], writes=[bprm])
                P.op("dve", lambda e: e.tensor_scalar(out=maskC[:, 1], in0=maskC[:, 0], scalar1=-1.0, scalar2=None, op0=ALU.mult), reads=[bprm], writes=[bprm])

                def disc(A, W, LD, F, full, tag):
                    t = {}
                    for nm in ("dt", "a", "mag", "ang", "y", "sn", "cs", "abr", "abi", "t1", "t2", "zr", "gr", "gi"):
                        t[nm] = sbuf(st, tag + nm, [128, F], F32)
                    b = bprm
                    P.op("act", lambda e: e.activation(out=t["dt"][:], in_=LD, func=AF.Exp), reads=[b], writes=[b])
                    P.op("dve", lambda e: e.tensor_scalar(out=t["a"][:], in0=A, scalar1=-1e-4, scalar2=None, op0=ALU.min), reads=[b], writes=[b])
                    P.op("dve", lambda e: e.tensor_tensor(out=t["mag"][:], in0=t["a"][:], in1=t["dt"][:], op=ALU.mult), reads=[b], writes=[b])
                    P.op("act", lambda e: e.activation(out=t["mag"][:], in_=t["mag"][:], func=AF.Exp), reads=[b], writes=[b])
                    P.op("dve", lambda e: e.tensor_tensor(out=t["ang"][:], in0=W, in1=t["dt"][:], op=ALU.mult), reads=[b], writes=[b])
                    ki = sbuf(st, tag + "ki", [128, F], I32)
                    for (dst, sh) in (("sn", 0.0), ("cs", 0.5 * PI)):
                        P.op("dve", lambda e, sh=sh: e.tensor_scalar(out=t["y"][:], in0=t["ang"][:], scalar1=sh, scalar2=None, op0=ALU.add), reads=[b], writes=[b])
                        P.op("dve", lambda e: e.tensor_scalar(out=t["t1"][:], in0=t["y"][:], scalar1=1.0 / (2.0 * PI), scalar2=None, op0=ALU.mult), reads=[b], writes=[b])
                        P.op("dve", lambda e: e.tensor_copy(out=ki[:], in_=t["t1"][:]), reads=[b], writes=[b])
                        P.op("dve", lambda e: e.tensor_copy(out=t["t1"][:], in_=ki[:]), reads=[b], writes=[b])
                        P.op("dve", lambda e: e.tensor_scalar(out=t["t1"][:], in0=t["t1"][:], scalar1=-2.0 * PI, scalar2=None, op0=ALU.mult), reads=[b], writes=[b])
                        P.op("dve", lambda e: e.tensor_tensor(out=t["y"][:], in0=t["y"][:], in1=t["t1"][:], op=ALU.add), reads=[b], writes=[b])
                        P.op("dve", lambda e: e.tensor_scalar(out=t["y"][:], in0=t["y"][:], scalar1=-PI, scalar2=PI, op0=ALU.max, op1=ALU.min), reads=[b], writes=[b])
                        P.op("act", lambda e, dst=dst: e.activation(out=t[dst][:], in_=t["y"][:], func=AF.Sin), reads=[b], writes=[b])
                    P.op("dve", lambda e: e.tensor_tensor(out=t["abr"][:], in0=t["mag"][:], in1=t["cs"][:], op=ALU.mult), reads=[b], writes=[b])
                    P.op("dve", lambda e: e.tensor_tensor(out=t["abi"][:], in0=t["mag"][:], in1=t["sn"][:], op=ALU.mult), reads=[b], writes=[b])
                    if full:
                        tt_ = lambda o, i0, i1, op: P.op("dve", lambda e: e.tensor_tensor(out=o, in0=i0, in1=i1, op=op), reads=[b], writes=[b])
                        tt_(t["t1"][:], t["a"][:], t["a"][:], ALU.mult)
                        tt_(t["t2"][:], W, W, ALU.mult)
                        tt_(t["t1"][:], t["t1"][:], t["t2"][:], ALU.add)
                        P.op("dve", lambda e: e.reciprocal(out=t["t1"][:], in_=t["t1"][:]), reads=[b], writes=[b])
                        P.op("dve", lambda e: e.tensor_scalar(out=t["zr"][:], in0=t["abr"][:], scalar1=-1.0, scalar2=None, op0=ALU.add), reads=[b], writes=[b])
                        tt_(t["gr"][:], t["zr"][:], t["a"][:], ALU.mult)
                        tt_(t["t2"][:], t["abi"][:], W, ALU.mult)
                        tt_(t["gr"][:], t["gr"][:], t["t2"][:], ALU.add)
                        tt_(t["gr"][:], t["gr"][:], t["t1"][:], ALU.mult)
                        tt_(t["gi"][:], t["abi"][:], t["a"][:], ALU.mult)
                        tt_(t["t2"][:], t["zr"][:], W, ALU.mult)
                        tt_(t["gi"][:], t["gi"][:], t["t2"][:], ALU.subtract)
                        tt_(t["gi"][:], t["gi"][:], t["t1"][:], ALU.mult)
                    return t

                NK = 11
                tl = disc(lamraw[:, 0, :], lamraw[:, 1, :], lamraw[:, 2, :], 16, False, "dl_")
                lamP = sbuf(st, "lamP", [128, 16, NK, 3], F32)
                tq = sbuf(st, "tqD", [128, 2, 16], F32)
                b = bprm
                P.op("dve", lambda e: e.tensor_copy(out=lamP[:, :, 0, 0], in_=tl["abr"][:]), reads=[b], writes=[b])
                P.op("dve", lambda e: e.tensor_copy(out=lamP[:, :, 0, 1], in_=tl["abi"][:]), reads=[b], writes=[b])
                for k in range(NK):
                    if k > 0:
                        P.op("dve", lambda e, k=k: e.tensor_tensor(out=tq[:, 0, :], in0=lamP[:, :, k - 1, 0], in1=lamP[:, :, k - 1, 0], op=ALU.mult), reads=[b], writes=[b])
                        P.op("dve", lambda e, k=k: e.tensor_tensor(out=tq[:, 1, :], in0=lamP[:, :, k - 1, 1], in1=lamP[:, :, k - 1, 1], op=ALU.mult), reads=[b], writes=[b])
                        P.op("dve", lambda e, k=k: e.tensor_tensor(out=lamP[:, :, k, 0], in0=tq[:, 0, :], in1=tq[:, 1, :], op=ALU.subtract), reads=[b], writes=[b])
                        P.op("dve", lambda e, k=k: e.scalar_tensor_tensor(out=lamP[:, :, k, 1], in0=lamP[:, :, k - 1, 0], scalar=2.0, in1=lamP[:, :, k - 1, 1],
                                                                          op0=ALU.mult, op1=ALU.mult), reads=[b], writes=[b])
                    P.op("dve", lambda e, k=k: e.tensor_scalar(out=lamP[:, :, k, 2], in0=lamP[:, :, k, 1], scalar1=-1.0, scalar2=None, op0=ALU.mult), reads=[b], writes=[b])
                A3 = sbuf(st, "A3D", [128, 4, 64], F32)
                W3 = sbuf(st, "W3D", [128, 4, 64], F32)
                L3 = sbuf(st, "L3D", [128, 4, 64], F32)
                Br3 = sbuf(st, "Br3D", [128, 4, 64], F32)
                Bi3 = sbuf(st, "Bi3D", [128, 4, 64], F32)
                for (dst, idx) in ((Br3, 0), (Bi3, 1), (A3, 2), (W3, 3), (L3, 4)):
                    P.op("dve", lambda e, dst=dst, idx=idx: e.tensor_copy(out=dst[:], in_=bTraw[:, :, idx, :]), reads=[b], writes=[b])
                fl = lambda t_: t_[:].rearrange("p a n -> p (a n)")
                tb = disc(fl(A3), fl(W3), fl(L3), 256, True, "db_")
                Bb = sbuf(st, "BbD", [128, 2, 256], F32)
                tt_ = lambda o, i0, i1, op: P.op("dve", lambda e: e.tensor_tensor(out=o, in0=i0, in1=i1, op=op), reads=[b], writes=[b])
                tt_(Bb[:, 0, :], tb["gr"][:], fl(Br3), ALU.mult)
                tt_(tb["t2"][:], tb["gi"][:], fl(Bi3), ALU.mult)
                tt_(Bb[:, 0, :], Bb[:, 0, :], tb["t2"][:], ALU.subtract)
                tt_(Bb[:, 1, :], tb["gr"][:], fl(Bi3), ALU.mult)
                tt_(tb["t2"][:], tb["gi"][:], fl(Br3), ALU.mult)
                tt_(Bb[:, 1, :], Bb[:, 1, :], tb["t2"][:], ALU.add)
                Bblk = sbuf(st, "BblkD", [128, 4, 2, 512], BF16)
                for dc in range(4):
                    for ri in range(2):
                        P.op("dve", lambda e, dc=dc, ri=ri: e.tensor_tensor(
                            out=Bblk[:, dc, ri, :].rearrange("p (g n) -> p g n", n=64),
                            in0=Bb[:, ri, dc * 64:(dc + 1) * 64].unsqueeze(1).to_broadcast([128, 8, 64]),
                            in1=maskB[:, :].unsqueeze(2).to_broadcast([128, 8, 64]), op=ALU.mult), reads=[b], writes=[b])
                Cblk = sbuf(st, "CblkD", [128, 16, 2, 128], F32)
                for dp in range(16):
                    pl = dp % 4
                    for k in range(2):
                        P.op("dve", lambda e, dp=dp, pl=pl, k=k: e.tensor_tensor(
                            out=Cblk[:, dp, k, :].rearrange("p (g o) -> p g o", o=16),
                            in0=cT[:, k, dp, :].unsqueeze(1).to_broadcast([128, 8, 16]),
                            in1=maskC[:, k, pl, :].unsqueeze(2).to_broadcast([128, 8, 16]), op=ALU.mult), reads=[b], writes=[b])

                G = sbuf(st, "GD", [128, 2, S], BF16)
                bG = Buf()
                UTs = sbuf(st, "UTsD", [128, S], BF16)
                Yacc = sbuf(st, "YaccD", [128, S], F32)
                bUT, bY = Buf(), Buf()
                X = [[(sbuf(st, "XD%d%d" % (i, j), [128, SEG], F32), Buf()) for j in range(2)] for i in range(2)]
                endst = sbuf(st, "endstD", [128, 2], F32)
                ptmp = sbuf(st, "ptmpD", [128, SEG], F32)
                bptmp = Buf()
                bend = Buf()
                psr = Rot([psum(st, "psrD%d" % i) for i in range(2)])
                psi = Rot([psum(st, "psiD%d" % i) for i in range(2)])
                psY = Rot([psum(st, "psYD%d" % i) for i in range(2)])

                def stt(eng, out, in0, scalar, in1, rd, wr):
                    P.op(eng, lambda e: e.scalar_tensor_tensor(out=out, in0=in0, scalar=scalar, in1=in1, op0=ALU.mult, op1=ALU.add), reads=rd + [bprm], writes=wr)

                for ct in range(2):
                    P.dma("sp", lambda e, ct=ct: e.dma_start(out=UTs[:], in_=UT[ct * 128:(ct + 1) * 128, :]), writes=[bUT])
                    for q4 in range(4):
                        P.op("act", lambda e, q4=q4, ct=ct: e.activation(out=Yacc[:, q4 * 2048:(q4 + 1) * 2048], in_=UTs[:, q4 * 2048:(q4 + 1) * 2048], func=AF.Copy,
                                                                         scale=dcol[:, ct:ct + 1]), reads=[bUT, bprm], writes=[bY])
                    for d in range(2):
                        for pl in range(4):
                            P8 = ct * 4 + pl
                            dp = d * 8 + P8
                            dc = d * 2 + ct
                            segs = list(range(S // SEG))
                            if d == 1:
                                segs = segs[::-1]
                            for si, sg_ in enumerate(segs):
                                c0 = sg_ * SEG
                                (Ar, bAr), (Ai, bAi) = X[0]
                                for b4 in range(SEG // 512):
                                    pr, bpr = psr.next()
                                    pi_, bpi = psi.next()
                                    P.op("pe", lambda e, pr=pr, dc=dc, pl=pl, c0=c0, b4=b4: e.matmul(pr[:], lhsT=Bblk[:, dc, 0, pl * 128:(pl + 1) * 128],
                                                                                                    rhs=UTs[:, c0 + b4 * 512:c0 + (b4 + 1) * 512], start=True, stop=True),
                                         reads=[bprm, bUT], writes=[bpr])
                                    P.op("pe", lambda e, pi_=pi_, dc=dc, pl=pl, c0=c0, b4=b4: e.matmul(pi_[:], lhsT=Bblk[:, dc, 1, pl * 128:(pl + 1) * 128],
                                                                                                      rhs=UTs[:, c0 + b4 * 512:c0 + (b4 + 1) * 512], start=True, stop=True),
                                         reads=[bprm, bUT], writes=[bpi])
                                    P.op("act", lambda e, pr=pr, Ar=Ar, b4=b4: e.activation(out=Ar[:, b4 * 512:(b4 + 1) * 512], in_=pr[:], func=AF.Copy), reads=[bpr], writes=[bAr])
                                    P.op("act", lambda e, pi_=pi_, Ai=Ai, b4=b4: e.activation(out=Ai[:, b4 * 512:(b4 + 1) * 512], in_=pi_[:], func=AF.Copy), reads=[bpi], writes=[bAi])
                                if si > 0:
                                    col = 0 if d == 0 else SEG - 1
                                    cs_ = slice(col, col + 1)
                                    stt("dve", Ar[:, cs_], endst[:, 0:1], lamP[:, dp, 0, 0:1], Ar[:, cs_], [bend, bAr], [bAr])
                                    stt("dve", Ar[:, cs_], endst[:, 1:2], lamP[:, dp, 0, 2:3], Ar[:, cs_], [bend, bAr], [bAr])
                                    stt("dve", Ai[:, cs_], endst[:, 1:2], lamP[:, dp, 0, 0:1], Ai[:, cs_], [bend, bAi], [bAi])
                                    stt("dve", Ai[:, cs_], endst[:, 0:1], lamP[:, dp, 0, 1:2], Ai[:, cs_], [bend, bAi], [bAi])
                                cur = 0
                                for k in range(NK):
                                    dd = 1 << k
                                    (Sr, bSr), (Si, bSi) = X[cur]
                                    (Dr, bDr), (Di, bDi) = X[1 - cur]
                                    if d == 0:
                                        o, i_, hd = slice(dd, SEG), slice(0, SEG - dd), slice(0, dd)
                                    else:
                                        o, i_, hd = slice(0, SEG - dd), slice(dd, SEG), slice(SEG - dd, SEG)
                                    stt("dve", Dr[:, o], Sr[:, i_], lamP[:, dp, k, 0:1], Sr[:, o], [bSr], [bDr])
                                    stt("dve", Dr[:, o], Si[:, i_], lamP[:, dp, k, 2:3], Dr[:, o], [bSi, bDr], [bDr])
                                    stt("dve", Di[:, o], Si[:, i_], lamP[:, dp, k, 0:1], Si[:, o], [bSi], [bDi])
                                    stt("dve", Di[:, o], Sr[:, i_], lamP[:, dp, k, 1:2], Di[:, o], [bSr, bDi], [bDi])
                                    P.op("act", lambda e, Dr=Dr, Sr=Sr, hd=hd: e.activation(out=Dr[:, hd], in_=Sr[:, hd], func=AF.Copy), reads=[bSr], writes=[bDr])
                                    P.op("act", lambda e, Di=Di, Si=Si, hd=hd: e.activation(out=Di[:, hd], in_=Si[:, hd], func=AF.Copy), reads=[bSi], writes=[bDi])
                                    cur = 1 - cur
                                (Fr, bFr), (Fi, bFi) = X[cur]
                                lc = SEG - 1 if d == 0 else 0
                                P.op("act", lambda e, Fr=Fr, lc=lc: e.activation(out=endst[:, 0:1], in_=Fr[:, lc:lc + 1], func=AF.Copy), reads=[bFr], writes=[bend])
                                P.op("act", lambda e, Fi=Fi, lc=lc: e.activation(out=endst[:, 1:2], in_=Fi[:, lc:lc + 1], func=AF.Copy), reads=[bFi], writes=[bend])
                                for b4 in range(SEG // 512):
                                    py, bpy = psY.next()
                                    P.op("pe", lambda e, py=py, Fr=Fr, dp=dp, b4=b4: e.matmul(py[:], lhsT=Cblk[:, dp, 0, :], rhs=Fr[:, b4 * 512:(b4 + 1) * 512], start=True, stop=False),
                                         reads=[bprm, bFr], writes=[bpy])
                                    P.op("pe", lambda e, py=py, Fi=Fi, dp=dp, b4=b4: e.matmul(py[:], lhsT=Cblk[:, dp, 1, :], rhs=Fi[:, b4 * 512:(b4 + 1) * 512], start=False, stop=True),
                                         reads=[bprm, bFi], writes=[bpy])
                                    P.op("dve", lambda e, py=py, c0=c0, b4=b4: e.tensor_tensor(out=Yacc[:, c0 + b4 * 512:c0 + (b4 + 1) * 512], in0=py[:],
                                                                                              in1=Yacc[:, c0 + b4 * 512:c0 + (b4 + 1) * 512], op=ALU.add), reads=[bpy, bY], writes=[bY])
                    for q4 in range(4):
                        P.op("act", lambda e, q4=q4, ct=ct: e.activation(out=G[:, ct, q4 * 2048:(q4 + 1) * 2048], in_=Yacc[:, q4 * 2048:(q4 + 1) * 2048], func=AF.Gelu),
                             reads=[bY], writes=[bG])
                sig = Rot([sbuf(st, "sigD%d" % i, [128, 512], F32) for i in range(2)])
                oc = Rot([sbuf(st, "ocD%d" % i, [128, 2, 512], F32) for i in range(2)])
                sq = Rot([sbuf(st, "sqD%d" % i, [128, 2, 512], BF16) for i in range(2)])
                rs = Rot([sbuf(st, "rsD%d" % i, [128, 512], F32) for i in range(2)])
                ocn = Rot([sbuf(st, "ocnD%d" % i, [128, 2, 512], BF16) for i in range(2)])
                for blk in range(S // 512):
                    t0 = blk * 512
                    o_c, boc = oc.next()
                    s_q, bsq = sq.next()
                    for jt in range(2):
                        pz, bpz = psr.next()
                        for kt in range(2):
                            P.op("pe", lambda e, pz=pz, kt=kt, jt=jt, t0=t0: e.matmul(pz[:], lhsT=Wglu[:, kt, jt * 128:(jt + 1) * 128], rhs=G[:, kt, t0:t0 + 512],
                                                                                      start=(kt == 0), stop=(kt == 1)), reads=[bprm, bG], writes=[bpz])
                        s_g, bsg = sig.next()
                        P.op("act", lambda e, s_g=s_g, pz=pz: e.activation(out=s_g[:], in_=pz[:], func=AF.Sigmoid), reads=[bpz], writes=[bsg])
                        P.op("dve", lambda e, o_c=o_c, s_g=s_g, jt=jt, t0=t0: e.tensor_tensor(out=o_c[:, jt, :], in0=G[:, jt, t0:t0 + 512], in1=s_g[:], op=ALU.mult),
                             reads=[bG, bsg], writes=[boc])
                        P.op("act", lambda e, o_c=o_c, s_q=s_q, jt=jt: e.activation(out=s_q[:, jt, :], in_=o_c[:, jt, :], func=AF.Square),
                             reads=[boc], writes=[bsq])
                    p2, bp2 = psY.next()
                    for jt in range(2):
                        P.op("pe", lambda e, p2=p2, s_q=s_q, jt=jt: e.matmul(p2[:], lhsT=ones_bf, rhs=s_q[:, jt, :], start=(jt == 0), stop=(jt == 1)),
                             reads=[bsq, b_const], writes=[bp2])
                    r_s, brs = rs.next()
                    P.op("act", lambda e, r_s=r_s, p2=p2: e.activation(out=r_s[:], in_=p2[:], func=AF.Sqrt, scale=1.0 / 256, bias=epsT[:, 0:1]),
                         reads=[bp2, b_const], writes=[brs])
                    P.op("dve", lambda e, r_s=r_s: e.reciprocal(out=r_s[:], in_=r_s[:]), reads=[brs], writes=[brs])
                    o_n, bon = ocn.next()
                    P.op("dve", lambda e, o_n=o_n, o_c=o_c, r_s=r_s: e.tensor_tensor(out=o_n[:], in0=o_c[:], in1=r_s[:].unsqueeze(1).to_broadcast([128, 2, 512]), op=ALU.mult),
                         reads=[boc, brs], writes=[bon])
                    P.dma("pool", lambda e, o_n=o_n, t0=t0: e.dma_start(out=MIXC[:, t0:t0 + 512].rearrange("(k p) t -> p k t", p=128), in_=o_n[:]), reads=[bon])
                P.barrier()

        def phase_E(l):
            xsrc = x_in if l == 0 else OUT
            with ExitStack() as st:
                Wout = sbuf(st, "Wout", [128, 8, 1024], BF16)
                Wr = sbuf(st, "Wr", [128, 8, 16], F32)
                gffn = sbuf(st, "gffn", [128, 1024], F32)
                bW, bWr, bg = Buf(), Buf(), Buf()
                wst = Rot([sbuf(st, "wstE%d" % i, [128, 1024], F32) for i in range(2)])
                for kc in range(8):
                    w, bw = wst.next()
                    P.dma("sp", lambda e, w=w, kc=kc: e.dma_start(out=w[:], in_=w_out[l, kc * 128:(kc + 1) * 128, :]), writes=[bw])
                    P.op("dve", lambda e, w=w, kc=kc: e.tensor_scalar(out=Wout[:, kc, :], in0=w[:], scalar1=gcol[:, l, 1, kc:kc + 1],
                                                                       scalar2=None, op0=ALU.mult), reads=[bw, b_const], writes=[bW])
                P.dma("sp", lambda e: e.dma_start(out=Wr[:], in_=w_router[l].rearrange("(k p) e -> p k e", p=128)), writes=[bWr])
                P.dma("sp", lambda e: e.dma_start(out=gffn[:], in_=gffn_d[l]), writes=[bg])
                acc = Rot([sbuf(st, "accE%d" % i, [128, 3, 520], F32) for i in range(2)])
                accb = Rot([sbuf(st, "accbE%d" % i, [128, 260], F32) for i in range(2)])
                xt = Rot([sbuf(st, "xtE%d" % i, [128, 1024], F32) for i in range(2)])
                mc = Rot([sbuf(st, "mcE%d" % i, [128, 2, 128], BF16) for i in range(2)])
                sa = Rot([sbuf(st, "saE%d" % i, [128, 520], F32) for i in range(2)])
                rd = Rot([sbuf(st, "rdE%d" % i, [128, 16], F32) for i in range(2)])
                oab = Rot([sbuf(st, "oabE%d" % i, [128, 768], F32) for i in range(2)])
                junk = sbuf(st, "junkE", [128, 1024], BF16)
                bjunk = Buf()
                ssq = Rot([sbuf(st, "ssqE%d" % i, [128, 4], F32) for i in range(2)])
                mixn = Rot([sbuf(st, "mixnE%d" % i, [128, 768], BF16) for i in range(2)])
                mT = Rot([sbuf(st, "mTE%d" % i, [128, 6, 128], BF16) for i in range(2)])
                x1 = Rot([sbuf(st, "x1E%d" % i, [128, 1024], F32) for i in range(2)])
                hf = Rot([sbuf(st, "hfE%d" % i, [128, 1024], F32) for i in range(2)])
                hfb = Rot([sbuf(st, "hfbE%d" % i, [128, 1024], BF16) for i in range(2)])
                hfT = Rot([sbuf(st, "hfTE%d" % i, [128, 8, 128], F32) for i in range(2)])
                ex = Rot([sbuf(st, "exE%d" % i, [128, 16], F32) for i in range(2)])
                se = Rot([sbuf(st, "seE%d" % i, [128, 2], F32) for i in range(2)])
                aff = Rot([sbuf(st, "affE%d" % i, [128, 16], F32) for i in range(2)])
                aT = Rot([sbuf(st, "aTE%d" % i, [16, 128], F32) for i in range(2)])
                pT = Rot([psum(st, "pTE%d" % i, [128, 8, 128], BF16) for i in range(1)])
                psx = Rot([psum(st, "psxE%d" % i) for i in range(2)])
                pTf = Rot([psum(st, "pTfE%d" % i, [128, 4, 128], F32) for i in range(2)])
                psl = Rot([psum(st, "pslE%d" % i) for i in range(2)])
                for tt in range(NT):
                    t0 = tt * 128
                    a_t, ba = acc.next()
                    P.dma("sp", lambda e, a_t=a_t, t0=t0: e.dma_start(out=a_t[:], in_=ACCA[:, t0:t0 + 128, :].rearrange("c p f -> p c f")),
                          writes=[ba])
                    ab_t, bab = accb.next()
                    P.dma("sp", lambda e, ab_t=ab_t, t0=t0: e.dma_start(out=ab_t[:], in_=ACCB[t0:t0 + 128, :]), writes=[bab])
                    x_t, bx = xt.next()
                    P.dma("sp", lambda e, x_t=x_t, t0=t0: e.dma_start(out=x_t[:], in_=xsrc[t0:t0 + 128, :]), writes=[bx])
                    m_c, bmc = mc.next()
                    P.dma("sp", lambda e, m_c=m_c, t0=t0: e.dma_start(out=m_c[:], in_=MIXC[:, t0:t0 + 128].rearrange("(k p) t -> p k t", p=128)),
                          writes=[bmc])
                    s_a, bsa = sa.next()
                    P.op("dve", lambda e, s_a=s_a, a_t=a_t: e.tensor_tensor(out=s_a[:], in0=a_t[:, 0, :], in1=a_t[:, 1, :], op=ALU.add), reads=[ba], writes=[bsa])
                    P.op("dve", lambda e, s_a=s_a, a_t=a_t: e.tensor_tensor(out=s_a[:], in0=s_a[:], in1=a_t[:, 2, :], op=ALU.add), reads=[ba, bsa], writes=[bsa])
                    r_d, brd = rd.next()
                    sa3 = s_a[:].rearrange("p (h d) -> p h d", d=65)
                    ab3 = ab_t[:].rearrange("p (h d) -> p h d", d=65)
                    P.op("dve", lambda e, r_d=r_d, sa3=sa3: e.reciprocal(out=r_d[:, 0:8], in_=sa3[:, :, 64]), reads=[bsa], writes=[brd])
                    P.op("dve", lambda e, r_d=r_d, ab3=ab3: e.reciprocal(out=r_d[:, 8:12], in_=ab3[:, :, 64]), reads=[bab], writes=[brd])
                    o_t, bo = oab.next()
                    P.op("dve", lambda e, o_t=o_t, sa3=sa3, r_d=r_d: e.tensor_tensor(
                        out=o_t[:, 0:512].rearrange("p (h d) -> p h d", d=64), in0=sa3[:, :, 0:64],
                        in1=r_d[:, 0:8].unsqueeze(2).to_broadcast([128, 8, 64]), op=ALU.mult), reads=[bsa, brd], writes=[bo])
                    P.op("dve", lambda e, o_t=o_t, ab3=ab3, r_d=r_d: e.tensor_tensor(
                        out=o_t[:, 512:768].rearrange("p (h d) -> p h d", d=64), in0=ab3[:, :, 0:64],
                        in1=r_d[:, 8:12].unsqueeze(2).to_broadcast([128, 4, 64]), op=ALU.mult), reads=[bab, brd], writes=[bo])
                    s_s, bss = ssq.next()
                    P.op("act", lambda e, o_t=o_t, s_s=s_s: e.activation(out=junk[:, 0:512], in_=o_t[:, 0:512], func=AF.Square, accum_out=s_s[:, 0:1]),
                         reads=[bo], writes=[bjunk, bss])
                    P.op("act", lambda e, o_t=o_t, s_s=s_s: e.activation(out=junk[:, 0:256], in_=o_t[:, 512:768], func=AF.Square, accum_out=s_s[:, 1:2]),
                         reads=[bo], writes=[bjunk, bss])
                    P.op("act", lambda e, s_s=s_s: e.activation(out=s_s[:, 0:1], in_=s_s[:, 0:1], func=AF.Sqrt, scale=1.0 / 512, bias=epsT[:, 0:1]),
                         reads=[bss, b_const], writes=[bss])
                    P.op("act", lambda e, s_s=s_s: e.activation(out=s_s[:, 1:2], in_=s_s[:, 1:2], func=AF.Sqrt, scale=1.0 / 256, bias=epsT[:, 0:1]),
                         reads=[bss, b_const], writes=[bss])
                    P.op("dve", lambda e, s_s=s_s: e.reciprocal(out=s_s[:, 0:2], in_=s_s[:, 0:2]), reads=[bss], writes=[bss])
                    m_n, bmn = mixn.next()
                    P.op("dve", lambda e, m_n=m_n, o_t=o_t, s_s=s_s: e.tensor_scalar(out=m_n[:, 0:512], in0=o_t[:, 0:512], scalar1=s_s[:, 0:1],
                                                                                     scalar2=None, op0=ALU.mult), reads=[bo, bss], writes=[bmn])
                    P.op("act", lambda e, m_n=m_n, o_t=o_t, s_s=s_s: e.activation(out=m_n[:, 512:768], in_=o_t[:, 512:768], func=AF.Copy, scale=s_s[:, 1:2]),
                         reads=[bo, bss], writes=[bmn])
                    p_T, bpT = pT.next()
                    for kc in range(6):
                        P.op("pe", lambda e, p_T=p_T, m_n=m_n, kc=kc: e.transpose(out=p_T[:, kc, :], in_=m_n[:, kc * 128:(kc + 1) * 128], identity=ident_bf),
                             reads=[bmn, b_const], writes=[bpT])
                    m_T, bmT = mT.next()
                    P.op("act", lambda e, m_T=m_T, p_T=p_T: e.activation(out=m_T[:], in_=p_T[:, 0:6, :], func=AF.Copy), reads=[bpT], writes=[bmT])
                    x_1, bx1 = x1.next()
                    for half in range(2):
                        px, bpx = psx.next()
                        for kc in range(8):
                            if kc < 6:
                                P.op("pe", lambda e, px=px, m_T=m_T, kc=kc, half=half: e.matmul(px[:], lhsT=m_T[:, kc, :], rhs=Wout[:, kc, half * 512:(half + 1) * 512],
                                                                                                 start=(kc == 0), stop=False), reads=[bmT, bW], writes=[bpx])
                            else:
                                P.op("pe", lambda e, px=px, m_c=m_c, kc=kc, half=half: e.matmul(px[:], lhsT=m_c[:, kc - 6, :], rhs=Wout[:, kc, half * 512:(half + 1) * 512],
                                                                                                 start=False, stop=(kc == 7)), reads=[bmc, bW], writes=[bpx])
                        P.op("dve", lambda e, x_1=x_1, px=px, x_t=x_t, half=half: e.tensor_tensor(out=x_1[:, half * 512:(half + 1) * 512], in0=px[:],
                                                                                                 in1=x_t[:, half * 512:(half + 1) * 512], op=ALU.add),
                             reads=[bpx, bx], writes=[bx1])
                    P.dma("pool", lambda e, x_1=x_1, t0=t0: e.dma_start(out=OUT[t0:t0 + 128, :], in_=x_1[:]), reads=[bx1])
                    P.op("act", lambda e, x_1=x_1, s_s=s_s: e.activation(out=junk[:], in_=x_1[:], func=AF.Square, accum_out=s_s[:, 2:3]),
                         reads=[bx1], writes=[bjunk, bss])
                    P.op("act", lambda e, s_s=s_s: e.activation(out=s_s[:, 2:3], in_=s_s[:, 2:3], func=AF.Sqrt, scale=1.0 / D, bias=epsT[:, 0:1]),
                         reads=[bss, b_const], writes=[bss])
                    P.op("dve", lambda e, s_s=s_s: e.reciprocal(out=s_s[:, 2:3], in_=s_s[:, 2:3]), reads=[bss], writes=[bss])
                    h_f, bhf = hf.next()
                    P.op("dve", lambda e, h_f=h_f, x_1=x_1, s_s=s_s: e.scalar_tensor_tensor(out=h_f[:], in0=x_1[:], scalar=s_s[:, 2:3], in1=gffn[:],
                                                                                           op0=ALU.mult, op1=ALU.mult), reads=[bx1, bss, bg], writes=[bhf])
                    h_b, bhb = hfb.next()
                    P.op("act", lambda e, h_b=h_b, h_f=h_f: e.activation(out=h_b[:], in_=h_f[:], func=AF.Copy), reads=[bhf], writes=[bhb])
                    P.dma("pool", lambda e, h_b=h_b, t0=t0: e.dma_start(out=HF[t0:t0 + 128, :], in_=h_b[:]), reads=[bhb], writes=[])
                    h_T, bhT = hfT.next()
                    for q4 in range(2):
                        pf, bpf = pTf.next()
                        for k4 in range(4):
                            kc = q4 * 4 + k4
                            P.op("pe", lambda e, pf=pf, h_f=h_f, kc=kc, k4=k4: e.transpose(out=pf[:, k4, :], in_=h_f[:, kc * 128:(kc + 1) * 128], identity=identf[:]),
                                 reads=[bhf, b_const], writes=[bpf])
                        P.op("act", lambda e, h_T=h_T, pf=pf, q4=q4: e.activation(out=h_T[:, q4 * 4:(q4 + 1) * 4, :], in_=pf[:], func=AF.Copy),
                             reads=[bpf], writes=[bhT])
                    pl, bpl = psl.next()
                    for kc in range(8):
                        P.op("pe", lambda e, pl=pl, h_T=h_T, kc=kc: e.matmul(pl[:, 0:16], lhsT=h_T[:, kc, :], rhs=Wr[:, kc, :], start=(kc == 0), stop=(kc == 7)),
                             reads=[bhT, bWr], writes=[bpl])
                    e_x, bex = ex.next()
                    s_e, bse = se.next()
                    P.op("act", lambda e, e_x=e_x, pl=pl, s_e=s_e: e.activation(out=e_x[:], in_=pl[:, 0:16], func=AF.Exp, accum_out=s_e[:, 0:1]),
                         reads=[bpl], writes=[bex, bse])
                    P.op("dve", lambda e, s_e=s_e: e.reciprocal(out=s_e[:, 1:2], in_=s_e[:, 0:1]), reads=[bse], writes=[bse])
                    a_f, baf = aff.next()
                    P.op("dve", lambda e, a_f=a_f, e_x=e_x, s_e=s_e: e.tensor_scalar(out=a_f[:], in0=e_x[:], scalar1=s_e[:, 1:2], scalar2=None, op0=ALU.mult),
                         reads=[bex, bse], writes=[baf])
                    P.dma("pool", lambda e, a_f=a_f, t0=t0: e.dma_start(out=AFFD[t0:t0 + 128, :], in_=a_f[:]), reads=[baf], writes=[])
                    pl2, bpl2 = psl.next()
                    P.op("pe", lambda e, pl2=pl2, a_f=a_f: e.transpose(out=pl2[0:16, 0:128], in_=a_f[:, 0:16], identity=identf[:]),
                         reads=[baf, b_const], writes=[bpl2])
                    a_T, baT = aT.next()
                    P.op("act", lambda e, pl2=pl2, a_T=a_T: e.activation(out=a_T[:], in_=pl2[0:16, 0:128], func=AF.Copy),
                         reads=[bpl2], writes=[baT])
                    P.dma("pool", lambda e, a_T=a_T, t0=t0: e.dma_start(out=AFFTD[:, t0:t0 + 128], in_=a_T[:]), reads=[baT])
                P.barrier()

        def phase_F(l):
            with ExitStack() as st:
                with ExitStack() as s1:
                    work = sbuf(s1, "workF", [16, S], F32)
                    mask = sbuf(s1, "maskF", [16, S], F32)
                    posf = sbuf(s1, "posF", [16, S], F32)
                    m8 = sbuf(s1, "m8F", [16, 8], F32)
                    ecst = sbuf(s1, "ecstF", [128, 18], F32)
                    bwk, bmk, bps_, bm8, bec = Buf(), Buf(), Buf(), Buf(), Buf()
                    P.dma("sp", lambda e: e.dma_start(out=ecst[:], in_=ecst_d[:, :]), writes=[bec])
                    affc = sbuf(s1, "affcF", [16, S], F32)
                    b_AFFT = Buf()
                    P.dma("sp", lambda e: e.dma_start(out=affc[:], in_=AFFTD[:, :]), writes=[b_AFFT])
                    bs = sbuf(s1, "bsF", [16, 8], F32)
                    bbs = Buf()
                    P.op("dve", lambda e: e.memset(bs[:], 0.0), writes=[bbs])
                    P.op("dve", lambda e: e.memset(bs[:, 1:2], 1.0), reads=[bbs], writes=[bbs])
                    for it in range(36):
                        P.op("dve", lambda e: e.tensor_tensor(out=bs[:, 2:3], in0=bs[:, 0:1], in1=bs[:, 1:2], op=ALU.add), reads=[bbs], writes=[bbs])
                        P.op("dve", lambda e: e.tensor_scalar(out=bs[:, 2:3], in0=bs[:, 2:3], scalar1=0.5, scalar2=None, op0=ALU.mult), reads=[bbs], writes=[bbs])
                        P.op("dve", lambda e: e.tensor_scalar(out=mask[:], in0=affc[:], scalar1=bs[:, 2:3], scalar2=None, op0=ALU.is_ge),
                             reads=[b_AFFT, bbs], writes=[bmk])
                        P.op("act", lambda e: e.activation(out=work[:], in_=mask[:], func=AF.Copy, accum_out=bs[:, 3:4]), reads=[bmk], writes=[bwk, bbs])
                        P.op("dve", lambda e: e.tensor_scalar(out=bs[:, 4:5], in0=bs[:, 3:4], scalar1=float(CAP) - 0.5, scalar2=None, op0=ALU.is_ge),
                             reads=[bbs], writes=[bbs])
                        P.op("dve", lambda e: e.scalar_tensor_tensor(out=bs[:, 0:1], in0=bs[:, 2:3], scalar=bs[:, 4:5], in1=bs[:, 0:1], op0=ALU.mult, op1=ALU.max),
                             reads=[bbs], writes=[bbs])
                        P.op("dve", lambda e: e.scalar_tensor_tensor(out=bs[:, 5:6], in0=bs[:, 4:5], scalar=2.0, in1=bs[:, 2:3], op0=ALU.mult, op1=ALU.add),
                             reads=[bbs], writes=[bbs])
                        P.op("dve", lambda e: e.tensor_tensor(out=bs[:, 1:2], in0=bs[:, 1:2], in1=bs[:, 5:6], op=ALU.min), reads=[bbs], writes=[bbs])
                    bm8 = bbs
                    P.op("dve", lambda e: e.tensor_scalar(out=mask[:], in0=affc[:], scalar1=bs[:, 0:1], scalar2=None, op0=ALU.is_ge),
                         reads=[b_AFFT, bm8], writes=[bmk])
                    P.op("pool", lambda e: e.memset(work[:], 1.0), reads=[bm8], writes=[bwk])
                    P.op("dve", lambda e: e.tensor_tensor_scan(out=posf[:], data0=work[:], data1=mask[:], initial=0.0, op0=ALU.mult, op1=ALU.add),
                         reads=[bwk, bmk], writes=[bps_])
                    afl = Rot([sbuf(s1, "aflF%d" % i, [128, 16], F32) for i in range(3)])
                    dst = Rot([sbuf(s1, "dstF%d" % i, [128, 16], F32) for i in range(3)])
                    dsi = Rot([sbuf(s1, "dsiF%d" % i, [128, 16], I32) for i in range(3)])
                    pair = Rot([sbuf(s1, "pairF%d" % i, [128, 16, 2], F32) for i in range(3)])
                    ptr = Rot([psum(s1, "ptrF%d" % i) for i in range(2)])
                    for tt in range(NT):
                        t0 = tt * 128
                        pt_, bpt_ = ptr.next()
                        P.op("pe", lambda e, pt_=pt_, t0=t0: e.transpose(out=pt_[:, 0:16], in_=mask[:, t0:t0 + 128], identity=identf[0:16, 0:16]),
                             reads=[bmk, b_const], writes=[bpt_])
                        P.op("pe", lambda e, pt_=pt_, t0=t0: e.transpose(out=pt_[:, 16:32], in_=posf[:, t0:t0 + 128], identity=identf[0:16, 0:16]),
                             reads=[bps_, b_const], writes=[bpt_])
                        a_l, bal = afl.next()
                        P.dma("sp", lambda e, a_l=a_l, t0=t0: e.dma_start(out=a_l[:], in_=AFFD[t0:t0 + 128, :]), writes=[bal])
                        d_t, bdt = dst.next()
                        P.op("dve", lambda e, d_t=d_t, pt_=pt_: e.tensor_scalar(out=d_t[:], in0=pt_[:, 16:32], scalar1=ecst[:, 16:17], scalar2=None, op0=ALU.subtract),
                             reads=[bpt_, bec], writes=[bdt])
                        P.op("dve", lambda e, d_t=d_t, pt_=pt_: e.tensor_tensor(out=d_t[:], in0=d_t[:], in1=pt_[:, 0:16], op=ALU.mult),
                             reads=[bpt_, bdt], writes=[bdt])
                        P.op("dve", lambda e, d_t=d_t: e.tensor_tensor(out=d_t[:], in0=d_t[:], in1=ecst[:, 0:16], op=ALU.add), reads=[bdt, bec], writes=[bdt])
                        d_i, bdi = dsi.next()
                        P.op("dve", lambda e, d_i=d_i, d_t=d_t: e.tensor_copy(out=d_i[:], in_=d_t[:]), reads=[bdt], writes=[bdi])
                        p_r, bpr = pair.next()
                        P.op("dve", lambda e, p_r=p_r, t0=t0: e.tensor_scalar(out=p_r[:, :, 0], in0=ecst[:, 17:18].to_broadcast([128, 16]), scalar1=float(t0),
                                                                               scalar2=None, op0=ALU.add), reads=[bec], writes=[bpr])
                        P.op("act", lambda e, p_r=p_r, a_l=a_l: e.activation(out=p_r[:, :, 1], in_=a_l[:], func=AF.Copy), reads=[bal], writes=[bpr])
                        for ex_ in range(NEXP):
                            P.dma("pool", lambda e, p_r=p_r, d_i=d_i, ex_=ex_: e.indirect_dma_start(
                                out=LIST[:, :], out_offset=bass.IndirectOffsetOnAxis(ap=d_i[:, ex_:ex_ + 1], axis=0),
                                in_=p_r[:, ex_, :], in_offset=None), reads=[bpr, bdi], writes=[])
                    P.barrier()
                Wg = sbuf(st, "WgF", [128, 8, 2048], BF16)
                Wu = sbuf(st, "WuF", [128, 8, 2048], BF16)
                Wd = sbuf(st, "WdF", [128, 16, 1024], BF16)
                bWg, bWu, bWd = Buf(), Buf(), Buf()
                wst = Rot([sbuf(st, "wstF%d" % i, [128, 2048], F32) for i in range(2)])
                li = sbuf(st, "liF", [128, 8, 2], F32)
                lii = sbuf(st, "liiF", [128, 8], I32)
                lif = sbuf(st, "lifF", [128, 8], F32)
                bli, blii, blif = Buf(), Buf(), Buf()
                xe = Rot([sbuf(st, "xeF%d" % i, [128, 1024], BF16) for i in range(2)])
                xeT = sbuf(st, "xeTF", [128, 8, 1024], BF16)
                bxeT = Buf()
                hid = sbuf(st, "hidF", [128, 16, 1024], BF16)
                bhid = Buf()
                sg = Rot([sbuf(st, "sgF%d" % i, [128, 512], F32) for i in range(2)])
                ys = Rot([sbuf(st, "ysF%d" % i, [128, 1024], F32) for i in range(2)])
                xr = Rot([sbuf(st, "xrF%d" % i, [128, 1024], F32) for i in range(2)])
                pT = Rot([psum(st, "pTF%d" % i, [128, 8, 128], BF16) for i in range(2)])
                psg = Rot([psum(st, "psgF%d" % i) for i in range(2)])
                psu = Rot([psum(st, "psuF%d" % i) for i in range(2)])
                psy = Rot([psum(st, "psyF%d" % i) for i in range(2)])
                cvt = [0]

                def convert(dst_ap, src_ap, rd, wr):
                    k = cvt[0] % 2
                    cvt[0] += 1
                    if k == 0:
                        P.op("act", lambda e: e.activation(out=dst_ap, in_=src_ap, func=AF.Copy), reads=rd, writes=wr)
                    else:
                        P.op("dve", lambda e: e.tensor_copy(out=dst_ap, in_=src_ap), reads=rd, writes=wr)

                for ex_ in range(NEXP):
                    for kc in range(8):
                        w, bw = wst.next()
                        P.dma("sp", lambda e, w=w, kc=kc, ex_=ex_: e.dma_start(out=w[:], in_=w_gate[l][ex_, kc * 128:(kc + 1) * 128, :]), writes=[bw])
                        convert(Wg[:, kc, :], w[:], [bw], [bWg])
                        w, bw = wst.next()
                        P.dma("sp", lambda e, w=w, kc=kc, ex_=ex_: e.dma_start(out=w[:], in_=w_up[l][ex_, kc * 128:(kc + 1) * 128, :]), writes=[bw])
                        convert(Wu[:, kc, :], w[:], [bw], [bWu])
                    for fc2 in range(8):
                        w, bw = wst.next()
                        P.dma("sp", lambda e, w=w, fc2=fc2, ex_=ex_: e.dma_start(
                            out=w[:].rearrange("p (a d) -> p a d", a=2), in_=w_down[l][ex_, fc2 * 256:(fc2 + 1) * 256, :].rearrange("(a p) d -> p a d", p=128)),
                            writes=[bw])
                        convert(Wd[:, fc2 * 2:fc2 * 2 + 2, :], w[:].rearrange("p (a d) -> p a d", a=2), [bw], [bWd])
                    P.dma("sp", lambda e, ex_=ex_: e.dma_start(out=li[:], in_=LIST[ex_ * ROWS:ex_ * ROWS + CAP, :].rearrange("(j p) c -> p j c", p=128)),
                          writes=[bli])
                    P.op("dve", lambda e: e.tensor_scalar(out=lif[:], in0=li[:, :, 0], scalar1=0.0, scalar2=float(S - 1), op0=ALU.max, op1=ALU.min), reads=[bli], writes=[blif])
                    P.op("dve", lambda e: e.tensor_copy(out=lii[:], in_=lif[:]), reads=[blif], writes=[blii])
                    for j in range(8):
                        x_e, bxe = xe.next()
                        P.dma("pool", lambda e, x_e=x_e, j=j: e.indirect_dma_start(out=x_e[:], out_offset=None, in_=HF[:, :],
                                                                                    in_offset=bass.IndirectOffsetOnAxis(ap=lii[:, j:j + 1], axis=0)),
                              reads=[blii], writes=[bxe])
                        p_T, bpT = pT.next()
                        for kc in range(8):
                            P.op("pe", lambda e, p_T=p_T, x_e=x_e, kc=kc: e.transpose(out=p_T[:, kc, :], in_=x_e[:, kc * 128:(kc + 1) * 128], identity=ident_bf),
                                 reads=[bxe, b_const], writes=[bpT])
                        P.op("act" if j % 2 == 0 else "dve", (lambda e, p_T=p_T, j=j: e.activation(out=xeT[:, :, j * 128:(j + 1) * 128], in_=p_T[:], func=AF.Copy)) if j % 2 == 0
                             else (lambda e, p_T=p_T, j=j: e.tensor_copy(out=xeT[:, :, j * 128:(j + 1) * 128], in_=p_T[:])), reads=[bpT], writes=[bxeT])
                    for tb in range(2):
                        for fc in range(16):
                            pg, bpg = psg.next()
                            pu, bpu = psu.next()
                            for kc in range(8):
                                P.op("pe", lambda e, pg=pg, kc=kc, fc=fc, tb=tb: e.matmul(pg[:], lhsT=Wg[:, kc, fc * 128:(fc + 1) * 128], rhs=xeT[:, kc, tb * 512:(tb + 1) * 512],
                                                                                          start=(kc == 0), stop=(kc == 7)), reads=[bWg, bxeT], writes=[bpg])
                            for kc in range(8):
                                P.op("pe", lambda e, pu=pu, kc=kc, fc=fc, tb=tb: e.matmul(pu[:], lhsT=Wu[:, kc, fc * 128:(fc + 1) * 128], rhs=xeT[:, kc, tb * 512:(tb + 1) * 512],
                                                                                          start=(kc == 0), stop=(kc == 7)), reads=[bWu, bxeT], writes=[bpu])
                            s_g, bsg = sg.next()
                            P.op("act", lambda e, s_g=s_g, pg=pg: e.activation(out=s_g[:], in_=pg[:], func=AF.Silu), reads=[bpg], writes=[bsg])
                            P.op("dve", lambda e, s_g=s_g, pu=pu, fc=fc, tb=tb: e.tensor_tensor(out=hid[:, fc, tb * 512:(tb + 1) * 512], in0=pu[:], in1=s_g[:], op=ALU.mult),
                                 reads=[bpu, bsg], writes=[bhid])
                    for j in range(8):
                        y_s, bys = ys.next()
                        for half in range(2):
                            py, bpy = psy.next()
                            for fc in range(16):
                                P.op("pe", lambda e, py=py, fc=fc, j=j, half=half: e.matmul(py[:], lhsT=hid[:, fc, j * 128:(j + 1) * 128], rhs=Wd[:, fc, half * 512:(half + 1) * 512],
                                                                                            start=(fc == 0), stop=(fc == 15)), reads=[bhid, bWd], writes=[bpy])
                            P.op("act", lambda e, y_s=y_s, py=py, half=half, j=j: e.activation(out=y_s[:, half * 512:(half + 1) * 512], in_=py[:], func=AF.Copy,
                                                                                                 scale=li[:, j, 1:2]), reads=[bpy, bli], writes=[bys])
                        x_r, bxr = xr.next()
                        P.dma("pool", lambda e, x_r=x_r, j=j: e.indirect_dma_start(out=x_r[:], out_offset=None, in_=OUT[:, :],
                                                                                    in_offset=bass.IndirectOffsetOnAxis(ap=lii[:, j:j + 1], axis=0)),
                              reads=[blii, b_X], writes=[bxr])
                        P.op("dve", lambda e, x_r=x_r, y_s=y_s: e.tensor_tensor(out=x_r[:], in0=x_r[:], in1=y_s[:], op=ALU.add), reads=[bxr, bys], writes=[bxr])
                        P.dma("pool", lambda e, x_r=x_r, j=j: e.indirect_dma_start(out=OUT[:, :], out_offset=bass.IndirectOffsetOnAxis(ap=lii[:, j:j + 1], axis=0),
                                                                                    in_=x_r[:], in_offset=None), reads=[bxr, blii], writes=[b_X])
                P.barrier()

        PHASES = {}
        PHASES["A"] = phase_A
        PHASES["B"] = phase_B
        PHASES["C"] = phase_C
        PHASES["D"] = phase_D
        PHASES["E"] = phase_E
        PHASES["F"] = phase_F
        order = "ABCDEF"
        stop = False
        for l in range(depth):
            if l > 0:
                P.new_epoch()
            for ph in order:
                if ph in PHASES and (only is None or ph in only):
                    PHASES[ph](l)
                if done(l, ph):
                    stop = True
                    break
            if stop:
                break
        P.finish_wait_all("sp")
        P.emit()
    return nc


def _consts():
    c = {}
    bf = ml_dtypes.bfloat16
    cm = np.zeros((6, 128, 128), np.float32)
    i = np.arange(128)
    cm[0] = np.eye(128)
    cm[1] = (i[:, None] // 64 == i[None, :] // 64)
    cm[2] = 1.0
    cm[3] = (i[:, None] < i[None, :])
    cm[4] = np.eye(128)[::-1]
    c["cmat"] = cm.astype(bf)
    c["identf"] = np.eye(128, dtype=np.float32)
    slopes = np.array([2.0 ** (-8.0 * (h + 1) / 8) for h in range(8)], np.float64)
    db = np.zeros((3, 8, 128, 256), np.float64)
    k = np.arange(128)[:, None]
    q = np.arange(256)[None, :]
    rel = k - q + 64
    valid = np.abs(rel) <= 64
    for ci, dil in enumerate((1, 4, 16)):
        for h in range(8):
            db[ci, h] = np.where(valid, -slopes[h] * dil * np.abs(rel) * 8.0, NEGB)
    c["dbias"] = db.astype(np.float32).astype(bf)
    variants = _na_variants()
    nm = np.zeros((21, 128, 128), np.float32)
    kk = np.arange(128)
    krl, kc = kk // 64, kk % 64
    qrl, qc = kk // 64, kk % 64
    cs = np.clip(qc - 8, 0, 48)
    colv = (kc[:, None] >= cs[None, :]) & (kc[:, None] < cs[None, :] + 16)
    for vi, (a, jp) in enumerate(variants):
        krow = 2 * (a + jp) + krl
        qrow = 2 * a + qrl
        rs = np.clip(qrow - 4, 0, 120)
        rowv = (krow[:, None] >= rs[None, :]) & (krow[:, None] < rs[None, :] + 8)
        nm[vi] = np.where(colv & rowv, 0.0, NEGB)
    c["namask"] = nm.astype(bf)
    p = np.arange(128)
    c["maskB"] = (p[:, None] // 16 == np.arange(8)[None, :]).astype(np.float32)
    mc = np.zeros((128, 4, 8), np.float32)
    for pl in range(4):
        for g8 in range(8):
            mc[:, pl, g8] = (g8 == 2 * pl + p // 64)
    c["maskC"] = mc
    ec = np.zeros((128, 18), np.float32)
    ec[:, 0:16] = np.arange(16)[None, :] * ROWS + CAP + p[:, None]
    ec[:, 16] = CAP + p + 1
    ec[:, 17] = p
    c["ecst"] = ec
    return c


def _na_variants():
    v = [(10, jp) for jp in (-2, -1, 0, 1, 2)]
    v += [(0, jp) for jp in (0, 1, 2, 3)]
    v += [(1, jp) for jp in (-1, 0, 1, 2)]
    v += [(62, jp) for jp in (-2, -1, 0, 1)]
    v += [(63, jp) for jp in (-3, -2, -1, 0)]
    return v


def _na_block_tiles(a):
    if a == 0:
        return [(j, 5 + j, j) for j in range(4)]
    if a == 1:
        return [(a + jp, 9 + (jp + 1), jp) for jp in (-1, 0, 1, 2)]
    if a == 62:
        return [(a + jp, 13 + (jp + 2), jp) for jp in (-2, -1, 0, 1)]
    if a == 63:
        return [(a + jp, 17 + (jp + 3), jp) for jp in (-3, -2, -1, 0)]
    return [(a + jp, jp + 2, jp) for jp in (-2, -1, 0, 1, 2)]


def _prep_shared(inp):
    L = DEPTH
    f = lambda a: np.ascontiguousarray(np.asarray(a, dtype=np.float32))
    sh = {}
    for k in ("w_in", "w_out", "w_glu", "w_router", "w_gate", "w_up", "w_down"):
        sh[k] = f(inp[k])
    onorm = np.concatenate([inp["out_norm_a"], inp["out_norm_b"], inp["out_norm_c"]], axis=1)
    g3 = np.stack([inp["attn_norm"], onorm, inp["ffn_norm"]], axis=1)
    sh["gcol"] = f(g3.reshape(L, 3, 8, 128).transpose(3, 0, 1, 2))
    gq = np.stack([inp["q_norm_a"], inp["k_norm_a"], inp["q_norm_b"], inp["k_norm_b"]], axis=1)
    sh["gqk"] = f(np.concatenate([gq, gq], axis=2).transpose(2, 0, 1))
    sh["gffn"] = f(np.broadcast_to(np.asarray(inp["ffn_norm"])[:, None, :], (L, 128, D)))
    rp = np.asarray(inp["rel_pos_bias"], np.float32)
    kk = np.arange(128)
    krl, kc = kk // 64, kk % 64
    nabt = np.zeros((L, 7, 4, 128, 128), np.float32)
    dc = np.clip(kc[:, None] - kc[None, :] + 15, 0, 30)
    for jp in range(-3, 4):
        dr = np.clip(2 * jp + krl[:, None] - krl[None, :] + 7, 0, 14)
        nabt[:, jp + 3] = rp[:, :, dr, dc]
    sh["nab"] = nabt
    are = np.asarray(inp["s5_a_re"], np.float32)
    aim = np.asarray(inp["s5_a_im"], np.float32)
    ldt = np.broadcast_to(np.asarray(inp["s5_log_dt"], np.float32)[..., None], are.shape)
    p3 = np.stack([are, aim, ldt], axis=1)
    t = p3.reshape(L, 3, 2, 8, 2, 64)
    sh["s5lam"] = f(t.transpose(4, 5, 0, 1, 2, 3).reshape(128, L, 3, 16))
    bre = np.asarray(inp["s5_b_re"], np.float32)
    bim = np.asarray(inp["s5_b_im"], np.float32)
    rep = lambda a: np.broadcast_to(a[..., None], a.shape + (16,))
    q5 = np.stack([bre, bim, rep(are), rep(aim), rep(ldt)], axis=0)
    q5 = q5.reshape(5, L, 2, 2, 8, 64, 16)
    sh["s5bT"] = f(q5.transpose(4, 6, 1, 2, 3, 0, 5).reshape(128, L, 4, 5, 64))
    cre = np.asarray(inp["s5_c_re"], np.float32)
    cim = np.asarray(inp["s5_c_im"], np.float32)
    c2 = np.stack([cre, cim], axis=0).reshape(2, L, 2, 8, 2, 16, 64)
    sh["s5cT"] = f(c2.transpose(4, 6, 1, 0, 2, 3, 5).reshape(128, L, 2, 16, 16))
    sh["s5dd"] = f(np.asarray(inp["s5_d"]).reshape(L, 2, 128).transpose(2, 0, 1))
    sh.update(_consts())
    return sh


_NC_CACHE = {}
_AX0 = ("w_in", "w_out", "w_glu", "w_router", "gffn", "nab")
_SPLIT = ("w_gate", "w_up", "w_down")
_AX1 = ("gcol", "gqk", "s5lam", "s5bT", "s5cT", "s5dd")
LAYERS_PER_LAUNCH = 4


def kernel(**inputs):
    x = np.asarray(inputs["x"], dtype=np.float32)
    sh = _prep_shared(inputs)
    npl = LAYERS_PER_LAUNCH
    if "nc" not in _NC_CACHE:
        _NC_CACHE["nc"] = build_program(depth=npl, nl=npl)
    nc = _NC_CACHE["nc"]
    cur = [np.ascontiguousarray(x[c]) for c in range(8)]
    for l0 in range(0, DEPTH, npl):
        shl = {}
        for k, v in sh.items():
            if k in _SPLIT:
                for i in range(npl):
                    shl["%s%d" % (k, i)] = v[l0 + i]
            elif k in _AX0:
                shl[k] = np.ascontiguousarray(v[l0:l0 + npl])
            elif k in _AX1:
                shl[k] = np.ascontiguousarray(v[:, l0:l0 + npl])
            else:
                shl[k] = v
        in_maps = []
        for c in range(8):
            m = dict(shl)
            m["x"] = cur[c]
            in_maps.append(m)
        res = run_bass_kernel_spmd(nc, in_maps, core_ids=list(range(8)))
        cur = [np.ascontiguousarray(np.asarray(r["out"], dtype=np.float32)) for r in res.results]
    return np.stack(cur, axis=0)
```
